# Optimizing a Trainium2 kernel written in Bass

```python
import math
import jax, jax.numpy as jnp
from jax import lax
import numpy as np

D_MODEL = 1024
BATCH = 4
SEQ = 8192
DEPTH = 2

N_HEADS = 8
HEAD_DIM = 64
ATTN_WIDTH = N_HEADS * HEAD_DIM
N_IDX_HEADS = 8
IDX_DIM = 64
TOPK_MAX = 256
Q_BLOCK = 128
CONV_CH = D_MODEL // 2
CONV_WIDTH = 31
D_FF = ((-(-8 * D_MODEL // 3)) + 255) // 256 * 256
N_BUCKETS = 32
MAX_DISTANCE = 128
EPS = 1e-6
SPLIT_SIZES = (ATTN_WIDTH, ATTN_WIDTH, ATTN_WIDTH,
               N_IDX_HEADS * IDX_DIM, IDX_DIM, N_IDX_HEADS,
               2 * CONV_CH,
               D_MODEL, D_MODEL)
N_IN = sum(SPLIT_SIZES)

kernel_name = "dsa_conformer_gated_hybrid"


def rmsnorm(x, g):
    xf = x.astype(jnp.float32)
    y = xf * lax.rsqrt(jnp.mean(xf * xf, axis=-1, keepdims=True) + EPS)
    return (y * g.astype(jnp.float32)).astype(x.dtype)


def layernorm(x, g, b):
    xf = x.astype(jnp.float32)
    mu = jnp.mean(xf, axis=-1, keepdims=True)
    var = jnp.mean(jnp.square(xf - mu), axis=-1, keepdims=True)
    y = (xf - mu) * lax.rsqrt(var + EPS)
    return (y * g.astype(jnp.float32) + b.astype(jnp.float32)).astype(x.dtype)


def t5_bucket(dist):
    n = jnp.maximum(dist, 0)
    max_exact = N_BUCKETS // 2
    nf = jnp.maximum(n, 1).astype(jnp.float32)
    large = max_exact + (jnp.log(nf / max_exact) / math.log(MAX_DISTANCE / max_exact)
                         * (N_BUCKETS - max_exact)).astype(jnp.int32)
    large = jnp.minimum(large, N_BUCKETS - 1)
    return jnp.where(n < max_exact, n, large)


def dsa_attention(q, k, v, q_idx, k_idx, w_idx, rel_bias):
    B, S = q.shape[0], q.shape[1]
    topk = min(TOPK_MAX, S // 4)
    nb = S // Q_BLOCK
    k_idx_f = k_idx.astype(jnp.float32)
    bias_tab = rel_bias.astype(jnp.float32)
    key_pos = jnp.arange(S, dtype=jnp.int32)
    gather = jax.vmap(lambda a, i: a[i])

    def to_blocks(a):
        return a.reshape((B, nb, Q_BLOCK) + a.shape[2:]).swapaxes(0, 1)

    def block(args):
        bi, qb, qib, wb = args
        t = bi * Q_BLOCK + jnp.arange(Q_BLOCK, dtype=jnp.int32)
        causal = key_pos[None, :] <= t[:, None]
        idx_logits = jnp.einsum('bqhd,bsd->bqhs', qib.astype(jnp.float32), k_idx_f) * (IDX_DIM ** -0.5)
        w = wb.astype(jnp.float32) * (N_IDX_HEADS ** -0.5)
        score = jnp.einsum('bqhs,bqh->bqs', jax.nn.relu(idx_logits), w)
        score = jnp.where(causal[None], score, -jnp.inf)
        _, sel = lax.top_k(score, topk)
        k_sel = gather(k, sel)
        v_sel = gather(v, sel)
        logits = jnp.einsum('bqhd,bqkhd->bhqk', qb, k_sel).astype(jnp.float32) * (HEAD_DIM ** -0.5)
        dist = t[None, :, None] - sel
        bias = bias_tab[t5_bucket(dist)]
        logits = logits + bias.transpose(0, 3, 1, 2)
        logits = jnp.where((dist >= 0)[:, None], logits, -jnp.inf)
        p = jax.nn.softmax(logits, axis=-1).astype(v.dtype)
        return jnp.einsum('bhqk,bqkhd->bqhd', p, v_sel)

    out = lax.map(block, (jnp.arange(nb, dtype=jnp.int32), to_blocks(q), to_blocks(q_idx), to_blocks(w_idx)))
    return out.swapaxes(0, 1).reshape(B, S, ATTN_WIDTH)


def conformer_conv(u, dw_kernel, dw_bias, g, b):
    a, gate = jnp.split(u, 2, axis=-1)
    h = a * jax.nn.sigmoid(gate)
    h = lax.conv_general_dilated(h, dw_kernel[:, None, :].astype(h.dtype), window_strides=(1,),
                                 padding=[(CONV_WIDTH - 1, 0)],
                                 dimension_numbers=('NWC', 'WIO', 'NWC'),
                                 feature_group_count=CONV_CH) + dw_bias.astype(h.dtype)
    h = layernorm(h, g, b)
    return jax.nn.silu(h)


def setup_inputs(seed: int = 0) -> dict:
    key = jax.random.key(seed)
    ks = jax.random.split(key, 16)
    f32 = jnp.float32

    def w(k, shape, fan_in):
        return jax.random.normal(k, shape, f32) * (fan_in ** -0.5)

    def gain(k, shape):
        return 1.0 + 0.05 * jax.random.normal(k, shape, f32)

    return {
        "x": jax.random.normal(ks[0], (BATCH, SEQ, D_MODEL), f32),
        "rel_bias": 0.5 * jax.random.normal(ks[1], (N_BUCKETS, N_HEADS), f32),
        "mix_norm": gain(ks[2], (DEPTH, D_MODEL)),
        "w_in": w(ks[3], (DEPTH, D_MODEL, N_IN), D_MODEL),
        "w_attn_out": w(ks[4], (DEPTH, ATTN_WIDTH, D_MODEL), ATTN_WIDTH),
        "dw_kernel": w(ks[5], (DEPTH, CONV_WIDTH, CONV_CH), CONV_WIDTH),
        "dw_bias": 0.02 * jax.random.normal(ks[6], (DEPTH, CONV_CH), f32),
        "conv_norm_g": gain(ks[7], (DEPTH, CONV_CH)),
        "conv_norm_b": 0.02 * jax.random.normal(ks[8], (DEPTH, CONV_CH), f32),
        "w_conv_out": w(ks[9], (DEPTH, CONV_CH, D_MODEL), CONV_CH),
        "w_mix_out": w(ks[10], (DEPTH, D_MODEL, D_MODEL), D_MODEL),
        "ffn_norm": gain(ks[11], (DEPTH, D_MODEL)),
        "w_ffn_in": w(ks[12], (DEPTH, D_MODEL, 2 * D_FF), D_MODEL),
        "w_ffn_out": w(ks[13], (DEPTH, D_FF, D_MODEL), D_FF),
        "final_norm": gain(ks[14], (D_MODEL,)),
    }


def reference(x, rel_bias, mix_norm, w_in, w_attn_out, dw_kernel, dw_bias, conv_norm_g, conv_norm_b,
              w_conv_out, w_mix_out, ffn_norm, w_ffn_in, w_ffn_out, final_norm):
    B, S, _ = x.shape
    split_points = list(np.cumsum(SPLIT_SIZES)[:-1])
    for l in range(DEPTH):
        h = rmsnorm(x, mix_norm[l])
        z = h @ w_in[l]
        q, k, v, qi, ki, wi, u_conv, g_a, g_b = jnp.split(z, split_points, axis=-1)
        q = q.reshape(B, S, N_HEADS, HEAD_DIM)
        k = k.reshape(B, S, N_HEADS, HEAD_DIM)
        v = v.reshape(B, S, N_HEADS, HEAD_DIM)
        qi = qi.reshape(B, S, N_IDX_HEADS, IDX_DIM)
        y_a = dsa_attention(q, k, v, qi, ki, wi, rel_bias) @ w_attn_out[l]
        y_b = conformer_conv(u_conv, dw_kernel[l], dw_bias[l], conv_norm_g[l], conv_norm_b[l]) @ w_conv_out[l]
        merged = jax.nn.sigmoid(g_a) * y_a + jax.nn.sigmoid(g_b) * y_b
        x = x + merged @ w_mix_out[l]
        h2 = rmsnorm(x, ffn_norm[l])
        gate, up = jnp.split(h2 @ w_ffn_in[l], 2, axis=-1)
        x = x + (jax.nn.silu(gate) * up) @ w_ffn_out[l]
    return rmsnorm(x, final_norm)
```

```python
import math
from contextlib import ExitStack

import numpy as np
import concourse.bass as bass
import concourse.mybir as mybir
from concourse.bass_utils import run_bass_kernel_spmd

F32 = mybir.dt.float32
BF16 = mybir.dt.bfloat16
AF = mybir.ActivationFunctionType
ALU = mybir.AluOpType
AX = mybir.AxisListType

D = 1024
NH = 8
DH = 64
AW = 512
CC = 512
CW = 31
DFF = 2816
NIN = 5192
TOPK = 256
EPS = 1e-6
NIT = 20
SAME_ENGINE_SYNC = True
WIDE_DUP = False

C_Q, C_K, C_V, C_QI, C_KI, C_WI, C_UA, C_UG, C_GA, C_GB = 0, 512, 1024, 1536, 2048, 2112, 2120, 2632, 3144, 4168

P_MIXG, P_FFNG, P_DW, P_DWB, P_CNG, P_CNB = 0, 8, 16, 140, 144, 148
P_LSZ = 152


class Prog:
    ENG = ("sp", "act", "dve", "pool", "pe")

    def __init__(self, nc):
        self.nc = nc
        self.eng_sem = {e: [nc.alloc_semaphore(name=f"es_{e}"), 0] for e in ("act", "dve", "pool", "pe")}
        self.dma = {}
        self.buf = {}
        self.q = {e: [] for e in self.ENG}
        self.waited = {e: {} for e in self.ENG}
        self.nblk = 0

    def _dma(self, key):
        d = self.dma.get(key)
        if d is None:
            d = [self.nc.alloc_semaphore(name=f"ds_{len(self.dma)}"), 0]
            self.dma[key] = d
        return d

    limit = None
    nops = 0

    def op(self, eng, name, kw, r=(), w=(), dma=None):
        self.nops += 1
        if self.limit is not None and self.nops > self.limit:
            return
        fn = (name, kw)
        deps = {}

        def add(t):
            if t is None:
                return
            k = id(t[0])
            if k not in deps or deps[k][1] < t[1]:
                deps[k] = t

        for b in r:
            st = self.buf.get(b)
            if st:
                add(st[0])
        for b in w:
            st = self.buf.get(b)
            if st:
                add(st[0])
                for t in st[1].values():
                    add(t)
        if dma is None:
            es = self.eng_sem[eng]
            es[1] += 1
            ticket = (es[0], es[1], eng)
            inc = 1
        else:
            d = self._dma(dma)
            d[1] += 16
            ticket = (d[0], d[1], None)
            inc = 16
        waits = []
        for k, (sem, val, src) in deps.items():
            if src == eng and (eng == "pe" or not SAME_ENGINE_SYNC):
                continue
            if self.waited[eng].get(k, 0) >= val:
                continue
            self.waited[eng][k] = val
            waits.append((sem, val))
        self.q[eng].append((waits, fn, ticket[0], inc))
        for b in r:
            st = self.buf.setdefault(b, [None, {}])
            st[1][id(ticket[0])] = ticket
        for b in w:
            self.buf[b] = [ticket, {}]

    def flush(self):
        nc = self.nc
        waits = []
        for d in self.dma.values():
            if d[1] > 0 and self.waited["sp"].get(id(d[0]), 0) < d[1]:
                self.waited["sp"][id(d[0])] = d[1]
                waits.append((d[0], d[1]))
        self.q["sp"].append((waits, None, None, 0))
        self.nblk += 1
        import os
        if os.environ.get('KDBG'):
            print('FLUSH', self.nblk, 'nops', self.nops, flush=True)
        with nc.Block() as block:
            for e, deco in (("sp", block.sync), ("act", block.scalar), ("dve", block.vector),
                            ("pool", block.gpsimd), ("pe", block.tensor)):
                items = self.q[e]

                def body(engine, items=items):
                    for waits, fn, sem, inc in items:
                        for (s, v) in waits:
                            engine.wait_ge(s, v)
                        if fn is not None:
                            getattr(engine, fn[0])(**fn[1]).then_inc(sem, inc)

                deco(body)
        self.q = {e: [] for e in self.ENG}
        self.buf = {}


def build_program(S, NL, stop=None, debug=False, limit=None):
    assert S % 512 == 0
    NT = S // 128
    nc = bass.Bass("TRN2", target_bir_lowering=False)
    dt_in = lambda name, shape: nc.dram_tensor(name, list(shape), F32, kind="ExternalInput").ap()
    x_in = dt_in("x", [S, D])
    w_in_d = dt_in("w_in", [NL, D, NIN])
    w_ao_d = dt_in("w_attn_out", [NL, AW, D])
    w_co_d = dt_in("w_conv_out", [NL, CC, D])
    w_mo_d = dt_in("w_mix_out", [NL, D, D])
    w_fi_d = dt_in("w_ffn_in", [NL, D, 2 * DFF])
    w_fo_d = dt_in("w_ffn_out", [NL, DFF, D])
    NPAR = NL * P_LSZ + 8
    par_d = dt_in("params", [128, NPAR])
    gfin_d = dt_in("gfin", [128, D])
    bias_d = dt_in("biasT", [128, NH, 3, 256])
    ident_d = dt_in("ident", [128, 128])
    tri_d = dt_in("tri", [128, 128])
    pow2_d = dt_in("pow2", [128, NIT + 1])
    out_d = nc.dram_tensor("out", [S, D], F32, kind="ExternalOutput").ap()

    scr = lambda name, shape, dt: nc.dram_tensor(name, list(shape), dt, kind=("ExternalOutput" if debug else "Internal")).ap()
    d_xa = scr("d_xa", [S, D], F32)
    d_xb = scr("d_xb", [S, D], F32)
    d_qT = scr("d_qT", [4, 128, S], BF16)
    d_kT = scr("d_kT", [4, 128, S], BF16)
    d_qiT = scr("d_qiT", [4, 128, S], BF16)
    d_V = scr("d_V", [NT, 128, 768], BF16)
    d_hg = scr("d_hg", [4, 128, S], BF16)
    d_sga = scr("d_sga", [8, 128, S], BF16)
    d_sgb = scr("d_sgb", [8, 128, S], BF16)
    d_oT = scr("d_oT", [4, 128, S], BF16)

    P = Prog(nc)
    P.limit = limit
    with ExitStack() as top:
        uid = [0]

        def sb(name, shape, dt, stack=top):
            uid[0] += 1
            return stack.enter_context(nc.sbuf_tensor(f"s{uid[0]}_{name}", list(shape), dt))

        _pa = [top.enter_context(nc.psum_tensor(f"psb{i}", [128, 512], F32)) for i in range(4)]
        Lt = top.enter_context(nc.psum_tensor("psL", [128, 1024], F32))
        _pb = [top.enter_context(nc.psum_tensor(f"psb{i}", [128, 512], F32)) for i in (6, 7)]
        psb = [t[:] for t in _pa] + [Lt[:, 0:512], Lt[:, 512:1024]] + [t[:] for t in _pb]
        par = sb("par", [128, NPAR], F32)
        identF = sb("identF", [128, 128], F32)
        identB = sb("identB", [128, 128], BF16)
        tri = sb("tri", [128, 128], F32)
        pow2 = sb("pow2", [128, NIT + 1], F32)
        onesF = sb("onesF", [128, 128], F32)
        biasadj = sb("biasadj", [128, NH, 3, 256], BF16)
        kidxT = sb("kidxT", [128, S], BF16)
        wi_all = sb("wi_all", [128, NT, 8], F32)
        absw = sb("absw", [128, NT, 8], F32)
        sgnw = sb("sgnw", [128, NT, 8], F32)
        B31 = NL * P_LSZ

        with ExitStack() as ph:
            bstg = sb("bstg", [128, NH, 3, 256], F32, ph)
            P.op("sp", "dma_start", dict(out=par[:], in_=par_d[:, :]), w=["par"], dma="l0")
            P.op("sp", "dma_start", dict(out=identF[:], in_=ident_d[:, :]), w=["identF"], dma="l1")
            P.op("pool", "dma_start", dict(out=identB[:], in_=ident_d[:, :]), w=["identB"], dma="l2")
            P.op("sp", "dma_start", dict(out=tri[:], in_=tri_d[:, :]), w=["tri"], dma="l3")
            P.op("sp", "dma_start", dict(out=pow2[:], in_=pow2_d[:, :]), w=["pow2"], dma="l4")
            P.op("sp", "dma_start", dict(out=bstg[:], in_=bias_d[:]), w=["bstg"], dma="l5")
            P.op("dve", "memset", dict(ap=onesF[:], constant=1.0 / CC), w=["onesF"])
            for h in range(NH):
                P.op("dve", "tensor_scalar", dict(out=biasadj[:, h, :, :], in0=bstg[:, h, :, :],
                                                            scalar1=par[:, B31 + h:B31 + h + 1], scalar2=None,
                                                            op0=ALU.subtract),
                     r=["bstg", "par"], w=[("biasadj", h)])
            P.flush()
        if stop == "setup":
            return nc

        x_src = x_in
        for l in range(NL):
            PB = l * P_LSZ
            with ExitStack() as ph:
                Win = sb("Win", [128, 8, NIN], BF16, ph)
                xt = sb("xtA", [128, 4, D], F32, ph)
                hb = sb("hbA", [128, 4, D], BF16, ph)
                hT = sb("hTA", [128, 8, 512], BF16, ph)
                ss = sb("ssA", [128, 4], F32, ph)
                rstd = sb("rstdA", [128, 4], F32, ph)
                NSL = 8
                ob = [sb(f"obA{i}", [128, 512], BF16, ph) for i in range(NSL)]
                tmpF = [sb(f"tmpFA{i}", [128, 512], F32, ph) for i in range(2)]
                vst = sb("vstA", [128, 4, 768], BF16, ph)
                for k in range(8):
                    P.op("pool", "dma_start", dict(out=Win[:, k, :], in_=w_in_d[l, k * 128:(k + 1) * 128, :]),
                         w=[("Win", k)], dma=("w", k))
                P.op("dve", "memset", dict(ap=vst[:], constant=1.0), w=[("vst", j) for j in range(4)])
                rot = {"ps": 0, "ob": 0, "tp": 0, "tf": 0, "ev": 0}

                def nxt(name, n):
                    v = rot[name]
                    rot[name] = (v + 1) % n
                    return v

                WinK = [("Win", k) for k in range(8)]
                hTK = [("hT", k) for k in range(8)]
                for c in range(S // 512):
                    t0 = c * 512
                    P.op("sp", "dma_start", dict(
                        out=xt[:], in_=x_src[t0:t0 + 512, :].rearrange("(j p) d -> p j d", p=128)),
                        w=["xt"], dma="xt")
                    for j in range(4):
                        P.op("act", "activation", dict(out=hb[:, j, :], in_=xt[:, j, :], func=AF.Square,
                                                                 accum_out=ss[:, j:j + 1]),
                             r=["xt"], w=[("hb", j), ("ss", j)])
                    P.op("dve", "tensor_scalar", dict(out=rstd[:], in0=ss[:], scalar1=1.0 / D, scalar2=EPS,
                                                          op0=ALU.mult, op1=ALU.add),
                         r=[("ss", j) for j in range(4)], w=["rstd"])
                    P.op("act", "activation", dict(out=rstd[:], in_=rstd[:], func=AF.Sqrt), r=["rstd"], w=["rstd"])
                    P.op("dve", "reciprocal", dict(out=rstd[:], in_=rstd[:]), r=["rstd"], w=["rstd"])
                    for j in range(4):
                        P.op("dve", "tensor_scalar", dict(out=hb[:, j, :], in0=xt[:, j, :],
                                                                    scalar1=rstd[:, j:j + 1], scalar2=None, op0=ALU.mult),
                             r=["xt", "rstd"], w=[("hb", j)])
                    for k in range(8):
                        tb = 6 + nxt("tp", 2)
                        pT = psb[tb][:].bitcast(BF16)
                        for j in range(4):
                            P.op("pe", "transpose", dict(
                                out=pT[:, j * 128:(j + 1) * 128], in_=hb[:, j, k * 128:(k + 1) * 128], identity=identB[:]),
                                r=[("hb", j), "identB"], w=[("ps", tb)])
                        if k % 2 == 0:
                            P.op("dve", "tensor_scalar", dict(
                                out=hT[:, k, :], in0=pT[:, 0:512], scalar1=par[:, PB + P_MIXG + k:PB + P_MIXG + k + 1],
                                scalar2=None, op0=ALU.mult), r=[("ps", tb)], w=[("hT", k)])
                        else:
                            P.op("act", "activation", dict(
                                out=hT[:, k, :], in_=pT[:, 0:512], func=AF.Identity,
                                scale=par[:, PB + P_MIXG + k:PB + P_MIXG + k + 1]), r=[("ps", tb)], w=[("hT", k)])

                    def fm_group(col0, M=128):
                        b = nxt("ps", 6)
                        for k in range(8):
                            P.op("pe", "matmul", dict(out=psb[b][0:M, :], lhsT=Win[:, k, col0:col0 + M],
                                                                     rhs=hT[:, k, :], start=(k == 0), stop=(k == 7)),
                                 r=[("Win", k), ("hT", k)], w=[("ps", b)])
                        return b

                    def evac_store(b, dst, func=None, scale=None):
                        s = nxt("ob", NSL)
                        use_act = func is not None or (nxt("ev", 2) == 0)
                        if use_act:
                            f = func if func is not None else AF.Copy
                            kw = {} if scale is None else {"scale": scale}
                            P.op("act", "activation", dict(out=ob[s][:], in_=psb[b][:, :], func=f, **kw),
                                 r=[("ps", b)], w=[("ob", s)])
                        else:
                            sc = 1.0 if scale is None else scale
                            P.op("dve", "tensor_scalar", dict(out=ob[s][:], in0=psb[b][:, :], scalar1=sc, scalar2=None,
                                                                  op0=ALU.mult), r=[("ps", b)], w=[("ob", s)])
                        P.op("sp", "dma_start", dict(out=dst, in_=ob[s][:]), r=[("ob", s)], w=[], dma=("ob", s))

                    for pr in range(4):
                        b = fm_group(C_Q + pr * 128)
                        evac_store(b, d_qT[pr, :, t0:t0 + 512], scale=DH ** -0.5)
                    for pr in range(4):
                        b = fm_group(C_K + pr * 128)
                        evac_store(b, d_kT[pr, :, t0:t0 + 512])
                    for pr in range(4):
                        b = fm_group(C_QI + pr * 128)
                        evac_store(b, d_qiT[pr, :, t0:t0 + 512])
                    b = fm_group(C_KI, M=64)
                    P.op("dve", "tensor_copy", dict(out=kidxT[0:64, t0:t0 + 512], in_=psb[b][0:64, :]),
                         r=[("ps", b)], w=[("kidx", c, 0)])
                    P.op("sp", "dma_start", dict(out=kidxT[64:128, t0:t0 + 512], in_=kidxT[0:64, t0:t0 + 512]),
                         r=[("kidx", c, 0)], w=[("kidx", c, 1)], dma="kidx")
                    for cc in range(4):
                        ba = fm_group(C_UA + cc * 128)
                        bg = fm_group(C_UG + cc * 128)
                        tf = nxt("tf", 2)
                        s = nxt("ob", NSL)
                        P.op("act", "activation", dict(out=tmpF[tf][:], in_=psb[bg][:, :], func=AF.Sigmoid),
                             r=[("ps", bg)], w=[("tmpF", tf)])
                        P.op("dve", "tensor_tensor", dict(out=ob[s][:], in0=psb[ba][:, :], in1=tmpF[tf][:],
                                                                                 op=ALU.mult),
                             r=[("ps", ba), ("tmpF", tf)], w=[("ob", s)])
                        P.op("sp", "dma_start", dict(out=d_hg[cc, :, t0:t0 + 512], in_=ob[s][:]),
                             r=[("ob", s)], dma=("ob", s))
                    for oc in range(8):
                        b = fm_group(C_GA + oc * 128)
                        evac_store(b, d_sga[oc, :, t0:t0 + 512], func=AF.Sigmoid)
                    for oc in range(8):
                        b = fm_group(C_GB + oc * 128)
                        evac_store(b, d_sgb[oc, :, t0:t0 + 512], func=AF.Sigmoid)
                    for j in range(4):
                        b = nxt("ps", 6)
                        for k in range(8):
                            P.op("pe", "matmul", dict(out=psb[b][:, :], lhsT=hT[:, k, j * 128:(j + 1) * 128],
                                                                          rhs=Win[:, k, C_V:C_V + 512], start=(k == 0), stop=(k == 7)),
                                 r=[("Win", k), ("hT", k)], w=[("ps", b)])
                        pv = psb[b][:, :].rearrange("p (a two d) -> p a two d", a=4, two=2)
                        vv = vst[:, j, :].rearrange("p (a x) -> p a x", a=4)
                        P.op("dve", "tensor_copy", dict(out=vv[:, :, 0:64], in_=pv[:, :, 0, :]),
                             r=[("ps", b)], w=[("vst", j)])
                        P.op("act", "activation", dict(out=vv[:, :, 128:192], in_=pv[:, :, 1, :], func=AF.Copy),
                             r=[("ps", b)], w=[("vst", j)])
                        P.op("sp", "dma_start", dict(out=d_V[c * 4 + j, :, :], in_=vst[:, j, :]),
                             r=[("vst", j)], dma=("vst", j))
                    b = nxt("ps", 6)
                    for j in range(4):
                        for k in range(8):
                            P.op("pe", "matmul", dict(out=psb[b][:, j * 8:(j + 1) * 8],
                                                                          lhsT=hT[:, k, j * 128:(j + 1) * 128],
                                                                          rhs=Win[:, k, C_WI:C_WI + 8], start=(k == 0), stop=(k == 7)),
                                 r=[("Win", k), ("hT", k)], w=[("ps", b)])
                    wv = wi_all[:, c * 4:(c + 1) * 4, :]
                    P.op("dve", "tensor_copy", dict(out=wv, in_=psb[b][:, 0:32].rearrange("p (j h) -> p j h", j=4)),
                         r=[("ps", b)], w=[("wi", c)])
                    P.op("act", "activation", dict(out=absw[:, c * 4:(c + 1) * 4, :], in_=wv, func=AF.Abs,
                                                   scale=(64 ** -0.5) * (8 ** -0.5)),
                         r=[("wi", c)], w=[("absw", c)])
                    P.op("dve", "tensor_scalar", dict(out=sgnw[:, c * 4:(c + 1) * 4, :], in0=wv, scalar1=0.0,
                                                                scalar2=2.0, op0=ALU.is_gt, op1=ALU.mult),
                         r=[("wi", c)], w=[("sgnw", c)])
                    P.op("dve", "tensor_scalar", dict(out=sgnw[:, c * 4:(c + 1) * 4, :], in0=sgnw[:, c * 4:(c + 1) * 4, :],
                                                          scalar1=-1.0, scalar2=None, op0=ALU.add),
                         r=[("sgnw", c)], w=[("sgnw", c)])
                P.flush()
            if stop == ("A", l):
                return nc

            with ExitStack() as ph:
                score = sb("score", [128, S], F32, ph)
                junk = sb("junkB", [128, S], F32, ph)
                maskA = [sb(f"maskA{i}", [128, S], BF16, ph) for i in range(2)]
                qTc = [sb(f"qTc{i}", [128, 4, 256], BF16, ph) for i in range(2)]
                qiTc = [sb(f"qiTc{i}", [128, 4, 256], BF16, ph) for i in range(2)]
                NKB = 2
                kTg = [sb(f"kTg{i}", [128, 4, 512], BF16, ph) for i in range(NKB)]
                Vg = [sb(f"Vg{i}", [128, 4, 768], BF16, ph) for i in range(NKB)]
                NR = 3
                Rb = [sb(f"Rb{i}", [128, 512], F32, ph) for i in range(NR)]
                NPT = 4
                Pt = [sb(f"Pt{i}", [128, 2, 256], BF16, ph) for i in range(NPT)]
                lo = sb("loB", [128, 2], F32, ph)
                hi = sb("hiB", [128, 2], F32, ph)
                mid = sb("midB", [128, 2], F32, ph)
                Wd = sb("WdB", [128, NIT + 1], F32, ph)
                cnt = sb("cntB", [128, 2], F32, ph)
                stp = sb("stpB", [128, 2], F32, ph)
                oacc = [sb(f"oaccB{i}", [128, 4, 512], F32, ph) for i in range(2)]
                denx = [sb(f"denxB{i}", [128, 4, 256], F32, ph) for i in range(2)]
                oTs = [sb(f"oTs{i}", [128, 4, 256], BF16, ph) for i in range(2)]
                oTo = [sb(f"oTo{i}", [128, 4, 256], BF16, ph) for i in range(2)]
                rot = {"idx": 0, "R": 0, "L": 0, "pt": 0, "mt": 0, "kg": 0}

                def nxt(name, n):
                    v = rot[name]
                    rot[name] = (v + 1) % n
                    return v

                P.op("dve", "memset", dict(ap=cnt[:], constant=0.0), w=["cnt"])
                mTb = [psb[6][:].bitcast(BF16), psb[7][:].bitcast(BF16)]
                Lbanks = [4, 5]
                for tc in range(S // 256):
                    T0 = tc * 256
                    qs = tc % 2
                    P.op("sp", "dma_start", dict(out=qTc[qs][:], in_=d_qT.rearrange("a p s -> p a s")[:, :, T0:T0 + 256]),
                         w=[("qTc", qs)], dma=("qTc", qs))
                    P.op("sp", "dma_start", dict(out=qiTc[qs][:], in_=d_qiT.rearrange("a p s -> p a s")[:, :, T0:T0 + 256]),
                         w=[("qiTc", qs)], dma=("qiTc", qs))
                    for qt in range(2):
                        Tq = T0 + 128 * qt
                        ti = Tq // 128
                        Sx = Tq + 128
                        ng = (Sx + 511) // 512
                        for g in range(ng):
                            wd = min(512, Sx - g * 512)
                            for h in range(NH):
                                b = nxt("idx", 4)
                                hp, pr = (h % 2) * 64, h // 2
                                P.op("pe", "matmul", dict(out=psb[b][:, 0:wd], lhsT=qiTc[qs][hp:hp + 64, pr, qt * 128:(qt + 1) * 128],
                                    rhs=kidxT[hp:hp + 64, g * 512:g * 512 + wd], start=True, stop=True),
                                    r=[("qiTc", qs)], w=[("ps", b)])
                                rs = nxt("R", NR)
                                P.op("act", "activation", dict(
                                    out=Rb[rs][:, 0:wd], in_=psb[b][:, 0:wd], func=AF.Relu, scale=absw[:, ti, h:h + 1]),
                                    r=[("ps", b)], w=[("R", rs)])
                                sc = score[:, g * 512:g * 512 + wd]
                                if h == 0:
                                    P.op("dve", "tensor_scalar", dict(
                                        out=sc, in0=Rb[rs][:, 0:wd], scalar1=sgnw[:, ti, 0:1], scalar2=None, op0=ALU.mult),
                                        r=[("R", rs)], w=[("score", g)])
                                else:
                                    P.op("dve", "scalar_tensor_tensor", dict(
                                        out=sc, in0=Rb[rs][:, 0:wd], scalar=sgnw[:, ti, h:h + 1], in1=sc, op0=ALU.mult, op1=ALU.add),
                                        r=[("R", rs), ("score", g)], w=[("score", g)])
                        SG = [("score", g) for g in range(ng)]
                        for col in range(2):
                            P.op("dve", "tensor_scalar", dict(out=junk[:, 0:Sx], in0=score[:, 0:Sx], scalar1=1.0, scalar2=None,
                                                              op0=ALU.mult, op1=ALU.min, accum_out=lo[:, col:col + 1]),
                                 r=SG, w=["junk", "lo"])
                        P.op("dve", "tensor_tensor", dict(out=score[:, Tq:Tq + 128], in0=score[:, Tq:Tq + 128], in1=tri[:],
                                                                     op=ALU.add), r=SG + ["tri"], w=SG)
                        for col in range(2):
                            P.op("dve", "tensor_scalar", dict(out=junk[:, 0:Sx], in0=score[:, 0:Sx], scalar1=1.0, scalar2=None,
                                                              op0=ALU.mult, op1=ALU.max, accum_out=hi[:, col:col + 1]),
                                 r=SG, w=["junk", "hi"])
                        P.op("dve", "tensor_scalar", dict(out=lo[:], in0=lo[:], scalar1=-1.0, scalar2=None, op0=ALU.add),
                             r=["lo"], w=["lo"])
                        P.op("dve", "tensor_tensor", dict(out=hi[:], in0=hi[:], in1=lo[:], op=ALU.subtract),
                             r=["lo", "hi"], w=["hi"])
                        P.op("dve", "tensor_scalar", dict(out=Wd[:], in0=pow2[:], scalar1=hi[:, 0:1], scalar2=0.5,
                                                              op0=ALU.mult, op1=ALU.mult), r=["hi", "pow2"], w=["Wd"])
                        P.op("dve", "tensor_scalar", dict(out=mid[:], in0=lo[:], scalar1=Wd[:, 0:1], scalar2=None, op0=ALU.add),
                             r=["lo", "Wd"], w=["mid"])
                        for k in range(NIT):
                            for col in range(2):
                                P.op("dve", "tensor_scalar", dict(out=junk[:, 0:Sx], in0=score[:, 0:Sx], scalar1=mid[:, 0:1],
                                                                  scalar2=None, op0=ALU.is_ge, op1=ALU.add, accum_out=cnt[:, col:col + 1]),
                                     r=SG + ["mid"], w=["junk", "cnt"])
                                if col == 0 and not WIDE_DUP:
                                    break
                            P.op("dve", "tensor_scalar", dict(out=stp[:], in0=cnt[:], scalar1=TOPK - 0.5, scalar2=Wd[:, k:k + 1],
                                                                       op0=ALU.is_gt, op1=ALU.mult), r=["cnt", "Wd"], w=["stp"])
                            P.op("dve", "scalar_tensor_tensor", dict(out=mid[:], in0=mid[:], scalar=Wd[:, k + 1:k + 2], in1=stp[:],
                                                                              op0=ALU.subtract, op1=ALU.add),
                                 r=["mid", "stp", "Wd"], w=["mid"])
                        P.op("dve", "tensor_scalar", dict(out=mid[:], in0=mid[:], scalar1=Wd[:, NIT:NIT + 1], scalar2=None, op0=ALU.subtract),
                             r=["mid", "Wd"], w=["mid"])
                        P.op("dve", "tensor_scalar", dict(out=maskA[qt][:, 0:Sx], in0=score[:, 0:Sx], scalar1=mid[:, 0:1],
                                                                            scalar2=None, op0=ALU.is_ge), r=SG + ["mid"], w=[("maskA", qt)])
                        if qt == 0:
                            P.op("dve", "memset", dict(ap=maskA[0][:, Sx:Sx + 128], constant=0.0), w=[("maskA", 0)])
                    if P.limit is not None or True:
                        import os
                        if os.environ.get('KDBG'):
                            print('ATT start tc', tc, 'nops', P.nops, flush=True)
                    nj = (T0 + 256) // 128
                    units = [(j, pr) for j in range(nj) for pr in range(4)]
                    state = {}

                    def qk(i):
                        j, pr = units[i]
                        g, jj = j // 4, j % 4
                        if pr == 0 and jj == 0:
                            kb = nxt("kg", NKB)
                            state["kb"] = kb
                            nch = min(4, nj - g * 4)
                            P.op("sp", "dma_start", dict(
                                out=kTg[kb][:, :, 0:nch * 128], in_=d_kT.rearrange("a p s -> p a s")[:, :, g * 512:g * 512 + nch * 128]),
                                w=[("kTg", kb)], dma=("kTg", kb))
                            P.op("sp", "dma_start", dict(
                                out=Vg[kb][:, 0:nch, :], in_=d_V[g * 4:g * 4 + nch, :, :].rearrange("c p x -> p c x")),
                                w=[("Vg", kb)], dma=("Vg", kb))
                        kb = state["kb"]
                        if pr == 0:
                            ms = nxt("mt", 2)
                            state["ms"] = ms
                            for qt in range(2):
                                P.op("pe", "transpose", dict(
                                    out=mTb[ms][:, qt * 128:(qt + 1) * 128],
                                    in_=maskA[qt][:, j * 128:(j + 1) * 128], identity=identB[:]),
                                    r=[("maskA", qt), "identB"], w=[("ps", 6 + ms)])
                        ms = state["ms"]
                        u = nxt("L", 2)
                        off = j * 128 - T0
                        near = off in (-128, 0, 128)
                        for hh in range(2):
                            h = 2 * pr + hh
                            Lv = Lt[:, hh * 512 + u * 256: hh * 512 + (u + 1) * 256]
                            P.op("pe", "matmul", dict(out=Lv, lhsT=kTg[kb][hh * 64:(hh + 1) * 64, pr, jj * 128:(jj + 1) * 128],
                                rhs=qTc[qs][hh * 64:(hh + 1) * 64, pr, :], start=True, stop=not near),
                                r=[("kTg", kb), ("qTc", qs)], w=[("ps", "L")])
                            if near:
                                oi = off // 128 + 1
                                P.op("pe", "matmul", dict(out=Lv, lhsT=identB[:], rhs=biasadj[:, h, oi, :],
                                                                                 start=False, stop=True),
                                     r=[("biasadj", h), "identB"], w=[("ps", "L")])
                        ps_ = nxt("pt", NPT)
                        Lin = Lt[:, :].rearrange("p (h u t) -> p h u t", h=2, u=2)[:, :, u, :]
                        P.op("act", "activation", dict(out=Pt[ps_][:], in_=Lin, func=AF.Exp), r=[("ps", "L")], w=[("Pt", ps_)])
                        P.op("dve", "tensor_tensor", dict(
                            out=Pt[ps_][:], in0=Pt[ps_][:], in1=mTb[ms][:, 0:256].unsqueeze(1).to_broadcast([128, 2, 256]),
                            op=ALU.mult), r=[("Pt", ps_), ("ps", 6 + ms)], w=[("Pt", ps_)])
                        return (ps_, kb)

                    def pv(i, info):
                        ps_, kb = info
                        j, pr = units[i]
                        jj = j % 4
                        for hh in range(2):
                            P.op("pe", "matmul", dict(out=psb[pr][:, hh * 256:(hh + 1) * 256], lhsT=Vg[kb][:, jj, pr * 192 + hh * 64: pr * 192 + hh * 64 + 128],
                                rhs=Pt[ps_][:, hh, :], start=(j == 0 and hh == 0), stop=(j == nj - 1), skip_group_check=True),
                                r=[("Vg", kb), ("Pt", ps_)], w=[("ps", pr)])

                    infos = {}
                    DEPTH = 1
                    for i in range(len(units)):
                        infos[i] = qk(i)
                        if i >= DEPTH:
                            pv(i - DEPTH, infos.pop(i - DEPTH))
                    for i in range(max(0, len(units) - DEPTH), len(units)):
                        pv(i, infos.pop(i))
                    import os
                    if os.environ.get('KDBG'):
                        print('NORM start tc', tc, 'nops', P.nops, flush=True)
                    os_ = tc % 2
                    for pr in range(4):
                        eng = "act" if pr % 2 == 0 else "dve"
                        if eng == "act":
                            P.op("act", "activation", dict(out=oacc[os_][:, pr, :], in_=psb[pr][:, :], func=AF.Copy),
                                 r=[("ps", pr)], w=[("oacc", os_, pr)])
                        else:
                            P.op("dve", "tensor_copy", dict(out=oacc[os_][:, pr, :], in_=psb[pr][:, :]),
                                 r=[("ps", pr)], w=[("oacc", os_, pr)])
                    OA = [("oacc", os_, pr) for pr in range(4)]
                    P.op("sp", "dma_start", dict(out=denx[os_][0:64, :, :], in_=oacc[os_][64:128, :, 0:256]),
                         r=OA, w=[("denx", os_, 0)], dma=("denx", os_, 0))
                    P.op("sp", "dma_start", dict(out=denx[os_][64:128, :, :], in_=oacc[os_][0:64, :, 256:512]),
                         r=OA, w=[("denx", os_, 1)], dma=("denx", os_, 1))
                    P.op("dve", "reciprocal", dict(out=denx[os_][:], in_=denx[os_][:]),
                         r=[("denx", os_, 0), ("denx", os_, 1)], w=[("denx", os_, 0), ("denx", os_, 1)])
                    P.op("dve", "tensor_tensor", dict(out=oTs[os_][:], in0=oacc[os_][:, :, 0:256], in1=denx[os_][:], op=ALU.mult),
                         r=OA + [("denx", os_, 0), ("denx", os_, 1)], w=[("oTs", os_, 0)])
                    P.op("dve", "tensor_tensor", dict(out=oTo[os_][:], in0=oacc[os_][:, :, 256:512], in1=denx[os_][:], op=ALU.mult),
                         r=OA + [("denx", os_, 0), ("denx", os_, 1)], w=[("oTs", os_, 1)])
                    dview = d_oT.rearrange("a p s -> p a s")
                    P.op("sp", "dma_start", dict(out=dview[0:64, :, T0:T0 + 256], in_=oTs[os_][0:64, :, :]),
                         r=[("oTs", os_, 0)], dma=("oTs", os_, 0))
                    P.op("sp", "dma_start", dict(out=dview[64:128, :, T0:T0 + 256], in_=oTo[os_][64:128, :, :]),
                         r=[("oTs", os_, 1)], dma=("oTs", os_, 1))
                P.flush()
            if stop == ("B", l):
                return nc

            with ExitStack() as ph:
                Wao = sb("Wao", [128, 4, D], BF16, ph)
                Wco = sb("Wco", [128, 4, D], BF16, ph)
                Wmo = sb("Wmo", [128, 8, D], BF16, ph)
                Dg = sb("Dg", [128, 4, CW, 128], BF16, ph)
                xt = sb("xtC", [128, 4, D], F32, ph)
                oTc = sb("oTc", [128, 4, 512], BF16, ph)
                hg = sb("hgC", [128, 4, 512 + CW - 1], BF16, ph)
                sga = sb("sgaC", [128, 8, 512], BF16, ph)
                sgb = sb("sgbC", [128, 8, 512], BF16, ph)
                conv = sb("convC", [128, 4, 512], F32, ph)
                sq = sb("sqC", [128, 4, 512], F32, ph)
                mean_s = sb("meanC", [128, 512], F32, ph)
                var_s = sb("varC", [128, 512], F32, ph)
                ytmp = [sb(f"ytmpC{i}", [128, 512], F32, ph) for i in range(2)]
                ycT = sb("ycT", [128, 4, 512], BF16, ph)
                merged = sb("mergedC", [128, 8, 512], BF16, ph)
                t1 = [sb(f"t1C{i}", [128, 512], F32, ph) for i in range(2)]
                t2 = [sb(f"t2C{i}", [128, 512], F32, ph) for i in range(2)]
                P.op("pool", "dma_start", dict(out=Wao[:], in_=w_ao_d[l].rearrange("(k p) n -> p k n", p=128)), w=["Wao"], dma=("w", 0))
                P.op("pool", "dma_start", dict(out=Wco[:], in_=w_co_d[l].rearrange("(k p) n -> p k n", p=128)), w=["Wco"], dma=("w", 1))
                P.op("pool", "dma_start", dict(out=Wmo[:], in_=w_mo_d[l].rearrange("(k p) n -> p k n", p=128)), w=["Wmo"], dma=("w", 2))
                for cc in range(4):
                    for k in range(CW):
                        col = PB + P_DW + cc * CW + k
                        P.op("dve", "tensor_scalar", dict(out=Dg[:, cc, k, :], in0=identF[:], scalar1=par[:, col:col + 1],
                                                                                  scalar2=None, op0=ALU.mult), w=[("Dg", cc)])
                rot = {"ps": 0, "y": 0, "t": 0}

                def nxt(name, n):
                    v = rot[name]
                    rot[name] = (v + 1) % n
                    return v

                HW_ = CW - 1
                for c in range(S // 512):
                    t0 = c * 512
                    P.op("sp", "dma_start", dict(out=xt[:], in_=x_src[t0:t0 + 512, :].rearrange("(j p) d -> p j d", p=128)),
                         w=["xt"], dma="xt")
                    P.op("sp", "dma_start", dict(out=oTc[:], in_=d_oT.rearrange("a p s -> p a s")[:, :, t0:t0 + 512]),
                         w=["oTc"], dma="oTc")
                    if c == 0:
                        P.op("dve", "memset", dict(ap=hg[:, :, 0:HW_], constant=0.0), w=["hg"])
                        P.op("sp", "dma_start", dict(out=hg[:, :, HW_:HW_ + 512], in_=d_hg.rearrange("a p s -> p a s")[:, :, 0:512]),
                             w=["hg"], dma="hg")
                    else:
                        P.op("sp", "dma_start", dict(out=hg[:], in_=d_hg.rearrange("a p s -> p a s")[:, :, t0 - HW_:t0 + 512]),
                             w=["hg"], dma="hg")
                    P.op("sp", "dma_start", dict(out=sga[:], in_=d_sga.rearrange("a p s -> p a s")[:, :, t0:t0 + 512]),
                         w=["sga"], dma="sga")
                    P.op("sp", "dma_start", dict(out=sgb[:], in_=d_sgb.rearrange("a p s -> p a s")[:, :, t0:t0 + 512]),
                         w=["sgb"], dma="sgb")
                    for cc in range(4):
                        b = nxt("ps", 4)
                        for k in range(CW):
                            P.op("pe", "matmul", dict(out=psb[b][:, :], lhsT=Dg[:, cc, k, :], rhs=hg[:, cc, k:k + 512],
                                                                           start=(k == 0), stop=(k == CW - 1)),
                                 r=[("Dg", cc), "hg"], w=[("ps", b)])
                        P.op("dve", "tensor_scalar", dict(out=conv[:, cc, :], in0=psb[b][:, :],
                                                                          scalar1=par[:, PB + P_DWB + cc:PB + P_DWB + cc + 1], scalar2=None,
                                                                          op0=ALU.add), r=[("ps", b)], w=[("conv", cc)])
                        P.op("act", "activation", dict(out=sq[:, cc, :], in_=conv[:, cc, :], func=AF.Square),
                             r=[("conv", cc)], w=[("sq", cc)])
                    bm, bq = 4, 5
                    for cc in range(4):
                        P.op("pe", "matmul", dict(out=psb[bm][:, :], lhsT=onesF[:], rhs=conv[:, cc, :], start=(cc == 0), stop=(cc == 3)),
                             r=[("conv", cc), "onesF"], w=[("ps", bm)])
                    for cc in range(4):
                        P.op("pe", "matmul", dict(out=psb[bq][:, :], lhsT=onesF[:], rhs=sq[:, cc, :], start=(cc == 0), stop=(cc == 3)),
                             r=[("sq", cc), "onesF"], w=[("ps", bq)])
                    P.op("act", "activation", dict(out=mean_s[:], in_=psb[bm][:, :], func=AF.Copy), r=[("ps", bm)], w=["mean"])
                    P.op("dve", "tensor_tensor", dict(out=var_s[:], in0=mean_s[:], in1=mean_s[:], op=ALU.mult), r=["mean"], w=["var"])
                    P.op("dve", "tensor_tensor", dict(out=var_s[:], in0=psb[bq][:, :], in1=var_s[:], op=ALU.subtract),
                         r=[("ps", bq), "var"], w=["var"])
                    P.op("dve", "tensor_scalar", dict(out=var_s[:], in0=var_s[:], scalar1=0.0, scalar2=EPS, op0=ALU.max, op1=ALU.add),
                         r=["var"], w=["var"])
                    P.op("act", "activation", dict(out=var_s[:], in_=var_s[:], func=AF.Sqrt), r=["var"], w=["var"])
                    P.op("dve", "reciprocal", dict(out=var_s[:], in_=var_s[:]), r=["var"], w=["var"])
                    for cc in range(4):
                        y = nxt("y", 2)
                        P.op("dve", "tensor_tensor", dict(out=ytmp[y][:], in0=conv[:, cc, :], in1=mean_s[:], op=ALU.subtract),
                             r=[("conv", cc), "mean"], w=[("ytmp", y)])
                        P.op("pool", "tensor_tensor", dict(out=ytmp[y][:], in0=ytmp[y][:], in1=var_s[:], op=ALU.mult),
                             r=[("ytmp", y), "var"], w=[("ytmp", y)])
                        P.op("act", "activation", dict(out=ycT[:, cc, :], in_=ytmp[y][:], func=AF.Silu,
                                                                       scale=par[:, PB + P_CNG + cc:PB + P_CNG + cc + 1],
                                                                       bias=par[:, PB + P_CNB + cc:PB + P_CNB + cc + 1]),
                             r=[("ytmp", y)], w=[("ycT", cc)])
                    for oc in range(8):
                        ba = nxt("ps", 4)
                        for pr in range(4):
                            P.op("pe", "matmul", dict(out=psb[ba][:, :], lhsT=Wao[:, pr, oc * 128:(oc + 1) * 128],
                                                                               rhs=oTc[:, pr, :], start=(pr == 0), stop=(pr == 3)),
                                 r=["Wao", "oTc"], w=[("ps", ba)])
                        bb = nxt("ps", 4)
                        for cc in range(4):
                            P.op("pe", "matmul", dict(out=psb[bb][:, :], lhsT=Wco[:, cc, oc * 128:(oc + 1) * 128],
                                                                               rhs=ycT[:, cc, :], start=(cc == 0), stop=(cc == 3)),
                                 r=["Wco", ("ycT", cc)], w=[("ps", bb)])
                        ts = nxt("t", 2)
                        P.op("dve", "tensor_tensor", dict(out=t1[ts][:], in0=psb[ba][:, :], in1=sga[:, oc, :], op=ALU.mult),
                             r=[("ps", ba), "sga"], w=[("t1", ts)])
                        P.op("dve", "tensor_tensor", dict(out=t2[ts][:], in0=psb[bb][:, :], in1=sgb[:, oc, :], op=ALU.mult),
                             r=[("ps", bb), "sgb"], w=[("t2", ts)])
                        P.op("pool", "tensor_tensor", dict(out=merged[:, oc, :], in0=t1[ts][:], in1=t2[ts][:], op=ALU.add),
                             r=[("t1", ts), ("t2", ts)], w=[("merged", oc)])
                    for j in range(4):
                        for n in range(2):
                            b = nxt("ps", 4)
                            for k in range(8):
                                P.op("pe", "matmul", dict(out=psb[b][:, :], lhsT=merged[:, k, j * 128:(j + 1) * 128],
                                                                                 rhs=Wmo[:, k, n * 512:(n + 1) * 512], start=(k == 0), stop=(k == 7)),
                                     r=[("merged", k), "Wmo"], w=[("ps", b)])
                            P.op("dve", "tensor_tensor", dict(out=xt[:, j, n * 512:(n + 1) * 512], in0=psb[b][:, :],
                                                                                in1=xt[:, j, n * 512:(n + 1) * 512], op=ALU.add),
                                 r=[("ps", b), "xt"], w=["xt"])
                    P.op("sp", "dma_start", dict(out=d_xa[t0:t0 + 512, :].rearrange("(j p) d -> p j d", p=128), in_=xt[:]),
                         r=["xt"], dma="xo")
                P.flush()
            if stop == ("C1", l):
                return nc

            last = (l == NL - 1)
            with ExitStack() as ph:
                Wfi = sb("Wfi", [128, 8, 2 * DFF], BF16, ph)
                Wfo = sb("Wfo", [128, 22, D], BF16, ph)
                xt2 = [sb(f"xtF{i}", [128, 2, D], F32, ph) for i in range(1)]
                hb = sb("hbF", [128, 2, D], BF16, ph)
                hT = sb("hTF", [128, 8, 256], BF16, ph)
                ss = sb("ssF", [128, 2], F32, ph)
                rstd = sb("rstdF", [128, 2], F32, ph)
                actT = sb("actT", [128, 22, 256], BF16, ph)
                tmpF = [sb(f"tmpFF{i}", [128, 256], F32, ph) for i in range(2)]
                if last:
                    gfin = sb("gfin", [128, D], F32, ph)
                    P.op("sp", "dma_start", dict(out=gfin[:], in_=gfin_d[:, :]), w=["gfin"], dma="gfin")
                for k in range(8):
                    P.op("pool", "dma_start", dict(out=Wfi[:, k, :], in_=w_fi_d[l, k * 128:(k + 1) * 128, :]),
                         w=[("Wfi", k)], dma=("w", k))
                for k in range(22):
                    P.op("pool", "dma_start", dict(out=Wfo[:, k, :], in_=w_fo_d[l, k * 128:(k + 1) * 128, :]),
                         w=[("Wfo", k)], dma=("w", 8 + k))
                rot = {"ps": 0, "tp": 0, "tf": 0}
                dst = out_d if last else d_xb
                for c in range(S // 256):
                    t0 = c * 256
                    xs = 0
                    xt = xt2[xs]
                    P.op("sp", "dma_start", dict(out=xt[:], in_=d_xa[t0:t0 + 256, :].rearrange("(j p) d -> p j d", p=128)),
                         w=[("xt", xs)], dma=("xt", xs))
                    for j in range(2):
                        P.op("act", "activation", dict(out=hb[:, j, :], in_=xt[:, j, :], func=AF.Square, accum_out=ss[:, j:j + 1]),
                             r=[("xt", xs)], w=[("hb", j), ("ss", j)])
                    P.op("dve", "tensor_scalar", dict(out=rstd[:], in0=ss[:], scalar1=1.0 / D, scalar2=EPS, op0=ALU.mult, op1=ALU.add),
                         r=[("ss", 0), ("ss", 1)], w=["rstd"])
                    P.op("act", "activation", dict(out=rstd[:], in_=rstd[:], func=AF.Sqrt), r=["rstd"], w=["rstd"])
                    P.op("dve", "reciprocal", dict(out=rstd[:], in_=rstd[:]), r=["rstd"], w=["rstd"])
                    for j in range(2):
                        P.op("dve", "tensor_scalar", dict(out=hb[:, j, :], in0=xt[:, j, :], scalar1=rstd[:, j:j + 1], scalar2=None,
                                                                         op0=ALU.mult), r=[("xt", xs), "rstd"], w=[("hb", j)])
                    for k in range(8):
                        tb = 6 + (rot["tp"] % 2)
                        rot["tp"] += 1
                        pT = psb[tb][:].bitcast(BF16)
                        for j in range(2):
                            P.op("pe", "transpose", dict(out=pT[:, j * 128:(j + 1) * 128], in_=hb[:, j, k * 128:(k + 1) * 128],
                                                                              identity=identB[:]), r=[("hb", j), "identB"], w=[("ps", tb)])
                        col = PB + P_FFNG + k
                        if k % 2 == 0:
                            P.op("dve", "tensor_scalar", dict(out=hT[:, k, :], in0=pT[:, 0:256], scalar1=par[:, col:col + 1],
                                                                                      scalar2=None, op0=ALU.mult), r=[("ps", tb)], w=[("hT", k)])
                        else:
                            P.op("act", "activation", dict(out=hT[:, k, :], in_=pT[:, 0:256], func=AF.Identity,
                                                                                   scale=par[:, col:col + 1]), r=[("ps", tb)], w=[("hT", k)])
                    for fc in range(22):
                        b = rot["ps"] % 4
                        rot["ps"] += 1
                        for half, c0 in ((0, fc * 128), (1, DFF + fc * 128)):
                            for k in range(8):
                                P.op("pe", "matmul", dict(out=psb[b][:, half * 256:(half + 1) * 256],
                                                                                         lhsT=Wfi[:, k, c0:c0 + 128], rhs=hT[:, k, :],
                                                                                         start=(k == 0), stop=(k == 7)),
                                     r=[("Wfi", k), ("hT", k)], w=[("ps", b)])
                        tf = rot["tf"] % 2
                        rot["tf"] += 1
                        P.op("act", "activation", dict(out=tmpF[tf][:], in_=psb[b][:, 0:256], func=AF.Silu),
                             r=[("ps", b)], w=[("tmpF", tf)])
                        P.op("dve", "tensor_tensor", dict(out=actT[:, fc, :], in0=psb[b][:, 256:512], in1=tmpF[tf][:], op=ALU.mult),
                             r=[("ps", b), ("tmpF", tf)], w=[("actT", fc)])
                    for j in range(2):
                        for n in range(2):
                            b = 4 + (rot["ps"] % 2)
                            rot["ps"] += 1
                            for fc in range(22):
                                P.op("pe", "matmul", dict(out=psb[b][:, :], lhsT=actT[:, fc, j * 128:(j + 1) * 128],
                                                                                   rhs=Wfo[:, fc, n * 512:(n + 1) * 512], start=(fc == 0), stop=(fc == 21)),
                                     r=[("actT", fc), ("Wfo", fc)], w=[("ps", b)])
                            P.op("dve", "tensor_tensor", dict(out=xt[:, j, n * 512:(n + 1) * 512], in0=psb[b][:, :],
                                                                                       in1=xt[:, j, n * 512:(n + 1) * 512], op=ALU.add),
                                 r=[("ps", b), ("xt", xs)], w=[("xt", xs)])
                    if last:
                        for j in range(2):
                            P.op("act", "activation", dict(out=hb[:, j, :], in_=xt[:, j, :], func=AF.Square, accum_out=ss[:, j:j + 1]),
                                 r=[("xt", xs)], w=[("hb", j), ("ss", j)])
                        P.op("dve", "tensor_scalar", dict(out=rstd[:], in0=ss[:], scalar1=1.0 / D, scalar2=EPS, op0=ALU.mult, op1=ALU.add),
                             r=[("ss", 0), ("ss", 1)], w=["rstd"])
                        P.op("act", "activation", dict(out=rstd[:], in_=rstd[:], func=AF.Sqrt), r=["rstd"], w=["rstd"])
                        P.op("dve", "reciprocal", dict(out=rstd[:], in_=rstd[:]), r=["rstd"], w=["rstd"])
                        for j in range(2):
                            P.op("dve", "scalar_tensor_tensor", dict(out=xt[:, j, :], in0=xt[:, j, :], scalar=rstd[:, j:j + 1], in1=gfin[:],
                                                                                    op0=ALU.mult, op1=ALU.mult),
                                 r=[("xt", xs), "rstd", "gfin"], w=[("xt", xs)])
                        P.op("sp", "dma_start", dict(out=dst[t0:t0 + 256, :].rearrange("(j p) d -> p j d", p=128), in_=xt[:]),
                             r=[("xt", xs)], dma=("xo", xs))
                    else:
                        P.op("sp", "dma_start", dict(out=dst[t0:t0 + 256, :].rearrange("(j p) d -> p j d", p=128), in_=xt[:]),
                             r=[("xt", xs)], dma=("xo", xs))
                P.flush()
            x_src = d_xb
    return nc


def t5_bucket_np(dist):
    n = np.maximum(dist, 0)
    nf = np.maximum(n, 1).astype(np.float32)
    large = 16 + (np.log(nf / np.float32(16)) / np.float32(math.log(128 / 16)) * np.float32(16)).astype(np.int32)
    large = np.minimum(large, 31)
    return np.where(n < 16, n, large)


def host_inputs(inputs, S, NL):
    f = lambda a: np.ascontiguousarray(np.asarray(a, dtype=np.float32))
    par = np.zeros((128, NL * P_LSZ + 8), np.float32)
    for l in range(NL):
        b = l * P_LSZ
        par[:, b + P_MIXG:b + P_MIXG + 8] = f(inputs["mix_norm"][l]).reshape(8, 128).T
        par[:, b + P_FFNG:b + P_FFNG + 8] = f(inputs["ffn_norm"][l]).reshape(8, 128).T
        dw = f(inputs["dw_kernel"][l])
        par[:, b + P_DW:b + P_DW + 4 * CW] = dw.T.reshape(4, 128, CW).transpose(1, 0, 2).reshape(128, 4 * CW)
        par[:, b + P_DWB:b + P_DWB + 4] = f(inputs["dw_bias"][l]).reshape(4, 128).T
        par[:, b + P_CNG:b + P_CNG + 4] = f(inputs["conv_norm_g"][l]).reshape(4, 128).T
        par[:, b + P_CNB:b + P_CNB + 4] = f(inputs["conv_norm_b"][l]).reshape(4, 128).T
    rb = f(inputs["rel_bias"])
    par[:, NL * P_LSZ:NL * P_LSZ + 8] = np.broadcast_to(rb[31:32, :], (128, 8))
    s_l = np.arange(128)[:, None]
    t_l = np.arange(256)[None, :]
    biasT = np.zeros((128, NH, 3, 256), np.float32)
    for oi, off in enumerate((-128, 0, 128)):
        bk = t5_bucket_np(t_l - s_l - off)
        biasT[:, :, oi, :] = rb[bk].transpose(0, 2, 1)
    gfin = np.ascontiguousarray(np.broadcast_to(f(inputs["final_norm"])[None, :], (128, D)))
    ident = np.eye(128, dtype=np.float32)
    tri = np.where(np.arange(128)[None, :] <= np.arange(128)[:, None], 0.0, -1e30).astype(np.float32)
    pow2 = np.broadcast_to((2.0 ** -np.arange(NIT + 1)).astype(np.float32)[None, :], (128, NIT + 1)).copy()
    shared = {
        "w_in": f(inputs["w_in"]), "w_attn_out": f(inputs["w_attn_out"]), "w_conv_out": f(inputs["w_conv_out"]),
        "w_mix_out": f(inputs["w_mix_out"]), "w_ffn_in": f(inputs["w_ffn_in"]), "w_ffn_out": f(inputs["w_ffn_out"]),
        "params": par, "gfin": gfin, "biasT": biasT, "ident": ident, "tri": tri, "pow2": pow2,
    }
    return shared


_NC_CACHE = {}


def kernel(**inputs):
    x = np.asarray(inputs["x"], dtype=np.float32)
    B, S, _ = x.shape
    NL = int(np.asarray(inputs["w_in"]).shape[0])
    key = (S, NL)
    if key not in _NC_CACHE:
        _NC_CACHE[key] = build_program(S, NL)
    nc = _NC_CACHE[key]
    shared = host_inputs(inputs, S, NL)
    in_maps = []
    for b in range(B):
        m = dict(shared)
        m["x"] = np.ascontiguousarray(x[b])
        in_maps.append(m)
    res = run_bass_kernel_spmd(nc, in_maps, core_ids=list(range(B)))
    out = np.stack([np.asarray(r["out"], dtype=np.float32) for r in res.results], axis=0)
    return out
```

```python
import math
from contextlib import ExitStack

import numpy as np
import concourse.bass as bass
import concourse.mybir as mybir
from concourse.bass_utils import run_bass_kernel_spmd

F32 = mybir.dt.float32
BF16 = mybir.dt.bfloat16
AF = mybir.ActivationFunctionType
ALU = mybir.AluOpType
AX = mybir.AxisListType

D = 1024
NH = 8
DH = 64
AW = 512
CC = 512
CW = 31
DFF = 2816
NIN = 5192
TOPK = 256
EPS = 1e-6
NIT = 20
SAME_ENGINE_SYNC = True
WIDE_DUP = False
ACT_SPLIT = 0.42

C_Q, C_K, C_V, C_QI, C_KI, C_WI, C_UA, C_UG, C_GA, C_GB = 0, 512, 1024, 1536, 2048, 2112, 2120, 2632, 3144, 4168

P_MIXG, P_FFNG, P_DW, P_DWB, P_CNG, P_CNB = 0, 8, 16, 140, 144, 148
P_LSZ = 152


class Prog:
    ENG = ("sp", "act", "dve", "pool", "pe")

    def __init__(self, nc):
        self.nc = nc
        self.eng_sem = {e: [nc.alloc_semaphore(name=f"es_{e}"), 0] for e in ("act", "dve", "pool", "pe")}
        self.dma = {}
        self.buf = {}
        self.q = {e: [] for e in self.ENG}
        self.waited = {e: {} for e in self.ENG}
        self.nblk = 0

    def _dma(self, key):
        d = self.dma.get(key)
        if d is None:
            d = [self.nc.alloc_semaphore(name=f"ds_{len(self.dma)}"), 0]
            self.dma[key] = d
        return d

    limit = None
    nops = 0

    def op(self, eng, name, kw, r=(), w=(), dma=None):
        self.nops += 1
        if self.limit is not None and self.nops > self.limit:
            return
        fn = (name, kw)
        deps = {}

        def add(t):
            if t is None:
                return
            k = id(t[0])
            if k not in deps or deps[k][1] < t[1]:
                deps[k] = t

        for b in r:
            st = self.buf.get(b)
            if st:
                add(st[0])
        for b in w:
            st = self.buf.get(b)
            if st:
                add(st[0])
                for t in st[1].values():
                    add(t)
        if dma is None:
            es = self.eng_sem[eng]
            es[1] += 1
            ticket = (es[0], es[1], eng)
            inc = 1
        else:
            d = self._dma(dma)
            d[1] += 16
            ticket = (d[0], d[1], None)
            inc = 16
        waits = []
        for k, (sem, val, src) in deps.items():
            if src == eng and (eng == "pe" or not SAME_ENGINE_SYNC):
                continue
            if self.waited[eng].get(k, 0) >= val:
                continue
            self.waited[eng][k] = val
            waits.append((sem, val))
        self.q[eng].append((waits, fn, ticket[0], inc))
        for b in r:
            st = self.buf.setdefault(b, [None, {}])
            st[1][id(ticket[0])] = ticket
        for b in w:
            self.buf[b] = [ticket, {}]

    def flush(self):
        nc = self.nc
        waits = []
        for d in self.dma.values():
            if d[1] > 0 and self.waited["sp"].get(id(d[0]), 0) < d[1]:
                self.waited["sp"][id(d[0])] = d[1]
                waits.append((d[0], d[1]))
        self.q["sp"].append((waits, None, None, 0))
        self.nblk += 1
        import os
        if os.environ.get('KDBG'):
            print('FLUSH', self.nblk, 'nops', self.nops, flush=True)
        with nc.Block() as block:
            for e, deco in (("sp", block.sync), ("act", block.scalar), ("dve", block.vector),
                            ("pool", block.gpsimd), ("pe", block.tensor)):
                items = self.q[e]

                def body(engine, items=items):
                    for waits, fn, sem, inc in items:
                        for (s, v) in waits:
                            engine.wait_ge(s, v)
                        if fn is not None:
                            getattr(engine, fn[0])(**fn[1]).then_inc(sem, inc)

                deco(body)
        self.q = {e: [] for e in self.ENG}
        self.buf = {}


def build_program(S, NL, stop=None, debug=False, limit=None):
    assert S % 512 == 0
    NT = S // 128
    nc = bass.Bass("TRN2", target_bir_lowering=False)
    dt_in = lambda name, shape: nc.dram_tensor(name, list(shape), F32, kind="ExternalInput").ap()
    x_in = dt_in("x", [S, D])
    w_in_d = dt_in("w_in", [NL, D, NIN])
    w_ao_d = dt_in("w_attn_out", [NL, AW, D])
    w_co_d = dt_in("w_conv_out", [NL, CC, D])
    w_mo_d = dt_in("w_mix_out", [NL, D, D])
    w_fi_d = dt_in("w_ffn_in", [NL, D, 2 * DFF])
    w_fo_d = dt_in("w_ffn_out", [NL, DFF, D])
    NPAR = NL * P_LSZ + 8
    par_d = dt_in("params", [128, NPAR])
    gfin_d = dt_in("gfin", [128, D])
    bias_d = dt_in("biasT", [128, NH, 3, 256])
    ident_d = dt_in("ident", [128, 128])
    tri_d = dt_in("tri", [128, 128])
    pow2_d = dt_in("pow2", [128, NIT + 1])
    out_d = nc.dram_tensor("out", [S, D], F32, kind="ExternalOutput").ap()

    scr = lambda name, shape, dt: nc.dram_tensor(name, list(shape), dt, kind=("ExternalOutput" if debug else "Internal")).ap()
    d_xa = scr("d_xa", [S, D], F32)
    d_xb = scr("d_xb", [S, D], F32)
    d_qT = scr("d_qT", [4, 128, S], BF16)
    d_kT = scr("d_kT", [4, 128, S], BF16)
    d_qiT = scr("d_qiT", [4, 128, S], BF16)
    d_V = scr("d_V", [NT, 128, 768], BF16)
    d_hg = scr("d_hg", [4, 128, S], BF16)
    d_sga = scr("d_sga", [8, 128, S], BF16)
    d_sgb = scr("d_sgb", [8, 128, S], BF16)
    d_oT = scr("d_oT", [4, 128, S], BF16)

    P = Prog(nc)
    P.limit = limit
    with ExitStack() as top:
        uid = [0]

        def sb(name, shape, dt, stack=top):
            uid[0] += 1
            return stack.enter_context(nc.sbuf_tensor(f"s{uid[0]}_{name}", list(shape), dt))

        _pa = [top.enter_context(nc.psum_tensor(f"psb{i}", [128, 512], F32)) for i in range(4)]
        Lt = top.enter_context(nc.psum_tensor("psL", [128, 1024], F32))
        _pb = [top.enter_context(nc.psum_tensor(f"psb{i}", [128, 512], F32)) for i in (6, 7)]
        psb = [t[:] for t in _pa] + [Lt[:, 0:512], Lt[:, 512:1024]] + [t[:] for t in _pb]
        par = sb("par", [128, NPAR], F32)
        identF = sb("identF", [128, 128], F32)
        identB = sb("identB", [128, 128], BF16)
        tri = sb("tri", [128, 128], F32)
        pow2 = sb("pow2", [128, NIT + 1], F32)
        onesF = sb("onesF", [128, 128], F32)
        biasadj = sb("biasadj", [128, NH, 3, 256], BF16)
        kidxT = sb("kidxT", [128, S], BF16)
        wi_all = sb("wi_all", [128, NT, 8], F32)
        absw = sb("absw", [128, NT, 8], F32)
        sgnw = sb("sgnw", [128, NT, 8], F32)
        B31 = NL * P_LSZ

        with ExitStack() as ph:
            bstg = sb("bstg", [128, NH, 3, 256], F32, ph)
            P.op("sp", "dma_start", dict(out=par[:], in_=par_d[:, :]), w=["par"], dma="l0")
            P.op("sp", "dma_start", dict(out=identF[:], in_=ident_d[:, :]), w=["identF"], dma="l1")
            P.op("pool", "dma_start", dict(out=identB[:], in_=ident_d[:, :]), w=["identB"], dma="l2")
            P.op("sp", "dma_start", dict(out=tri[:], in_=tri_d[:, :]), w=["tri"], dma="l3")
            P.op("sp", "dma_start", dict(out=pow2[:], in_=pow2_d[:, :]), w=["pow2"], dma="l4")
            P.op("sp", "dma_start", dict(out=bstg[:], in_=bias_d[:]), w=["bstg"], dma="l5")
            P.op("dve", "memset", dict(ap=onesF[:], constant=1.0 / CC), w=["onesF"])
            for h in range(NH):
                P.op("dve", "tensor_scalar", dict(out=biasadj[:, h, :, :], in0=bstg[:, h, :, :],
                                                            scalar1=par[:, B31 + h:B31 + h + 1], scalar2=None,
                                                            op0=ALU.subtract),
                     r=["bstg", "par"], w=[("biasadj", h)])
            P.flush()
        if stop == "setup":
            return nc

        x_src = x_in
        for l in range(NL):
            PB = l * P_LSZ
            with ExitStack() as ph:
                Win = sb("Win", [128, 8, NIN], BF16, ph)
                xt = sb("xtA", [128, 4, D], F32, ph)
                hb = sb("hbA", [128, 4, D], BF16, ph)
                hT = sb("hTA", [128, 8, 512], BF16, ph)
                ss = sb("ssA", [128, 4], F32, ph)
                rstd = sb("rstdA", [128, 4], F32, ph)
                NSL = 8
                ob = [sb(f"obA{i}", [128, 512], BF16, ph) for i in range(NSL)]
                tmpF = [sb(f"tmpFA{i}", [128, 512], F32, ph) for i in range(2)]
                vst = sb("vstA", [128, 4, 768], BF16, ph)
                for k in range(8):
                    P.op("pool", "dma_start", dict(out=Win[:, k, :], in_=w_in_d[l, k * 128:(k + 1) * 128, :]),
                         w=[("Win", k)], dma=("w", k))
                P.op("dve", "memset", dict(ap=vst[:], constant=1.0), w=[("vst", j) for j in range(4)])
                rot = {"ps": 0, "ob": 0, "tp": 0, "tf": 0, "ev": 0}

                def nxt(name, n):
                    v = rot[name]
                    rot[name] = (v + 1) % n
                    return v

                WinK = [("Win", k) for k in range(8)]
                hTK = [("hT", k) for k in range(8)]
                for c in range(S // 512):
                    t0 = c * 512
                    P.op("sp", "dma_start", dict(
                        out=xt[:], in_=x_src[t0:t0 + 512, :].rearrange("(j p) d -> p j d", p=128)),
                        w=["xt"], dma="xt")
                    for j in range(4):
                        P.op("act", "activation", dict(out=hb[:, j, :], in_=xt[:, j, :], func=AF.Square,
                                                                 accum_out=ss[:, j:j + 1]),
                             r=["xt"], w=[("hb", j), ("ss", j)])
                    P.op("dve", "tensor_scalar", dict(out=rstd[:], in0=ss[:], scalar1=1.0 / D, scalar2=EPS,
                                                          op0=ALU.mult, op1=ALU.add),
                         r=[("ss", j) for j in range(4)], w=["rstd"])
                    P.op("act", "activation", dict(out=rstd[:], in_=rstd[:], func=AF.Sqrt), r=["rstd"], w=["rstd"])
                    P.op("dve", "reciprocal", dict(out=rstd[:], in_=rstd[:]), r=["rstd"], w=["rstd"])
                    for j in range(4):
                        P.op("dve", "tensor_scalar", dict(out=hb[:, j, :], in0=xt[:, j, :],
                                                                    scalar1=rstd[:, j:j + 1], scalar2=None, op0=ALU.mult),
                             r=["xt", "rstd"], w=[("hb", j)])
                    for k in range(8):
                        tb = 6 + nxt("tp", 2)
                        pT = psb[tb][:].bitcast(BF16)
                        for j in range(4):
                            P.op("pe", "transpose", dict(
                                out=pT[:, j * 128:(j + 1) * 128], in_=hb[:, j, k * 128:(k + 1) * 128], identity=identB[:]),
                                r=[("hb", j), "identB"], w=[("ps", tb)])
                        if k % 2 == 0:
                            P.op("dve", "tensor_scalar", dict(
                                out=hT[:, k, :], in0=pT[:, 0:512], scalar1=par[:, PB + P_MIXG + k:PB + P_MIXG + k + 1],
                                scalar2=None, op0=ALU.mult), r=[("ps", tb)], w=[("hT", k)])
                        else:
                            P.op("act", "activation", dict(
                                out=hT[:, k, :], in_=pT[:, 0:512], func=AF.Identity,
                                scale=par[:, PB + P_MIXG + k:PB + P_MIXG + k + 1]), r=[("ps", tb)], w=[("hT", k)])

                    def fm_group(col0, M=128):
                        b = nxt("ps", 6)
                        for k in range(8):
                            P.op("pe", "matmul", dict(out=psb[b][0:M, :], lhsT=Win[:, k, col0:col0 + M],
                                                                     rhs=hT[:, k, :], start=(k == 0), stop=(k == 7)),
                                 r=[("Win", k), ("hT", k)], w=[("ps", b)])
                        return b

                    def evac_store(b, dst, func=None, scale=None):
                        s = nxt("ob", NSL)
                        use_act = func is not None or (nxt("ev", 2) == 0)
                        if use_act:
                            f = func if func is not None else AF.Copy
                            kw = {} if scale is None else {"scale": scale}
                            P.op("act", "activation", dict(out=ob[s][:], in_=psb[b][:, :], func=f, **kw),
                                 r=[("ps", b)], w=[("ob", s)])
                        else:
                            sc = 1.0 if scale is None else scale
                            P.op("dve", "tensor_scalar", dict(out=ob[s][:], in0=psb[b][:, :], scalar1=sc, scalar2=None,
                                                                  op0=ALU.mult), r=[("ps", b)], w=[("ob", s)])
                        P.op("sp", "dma_start", dict(out=dst, in_=ob[s][:]), r=[("ob", s)], w=[], dma=("ob", s))

                    for pr in range(4):
                        b = fm_group(C_Q + pr * 128)
                        evac_store(b, d_qT[pr, :, t0:t0 + 512], scale=DH ** -0.5)
                    for pr in range(4):
                        b = fm_group(C_K + pr * 128)
                        evac_store(b, d_kT[pr, :, t0:t0 + 512])
                    for pr in range(4):
                        b = fm_group(C_QI + pr * 128)
                        evac_store(b, d_qiT[pr, :, t0:t0 + 512])
                    b = fm_group(C_KI, M=64)
                    P.op("dve", "tensor_copy", dict(out=kidxT[0:64, t0:t0 + 512], in_=psb[b][0:64, :]),
                         r=[("ps", b)], w=[("kidx", c, 0)])
                    P.op("sp", "dma_start", dict(out=kidxT[64:128, t0:t0 + 512], in_=kidxT[0:64, t0:t0 + 512]),
                         r=[("kidx", c, 0)], w=[("kidx", c, 1)], dma="kidx")
                    for cc in range(4):
                        ba = fm_group(C_UA + cc * 128)
                        bg = fm_group(C_UG + cc * 128)
                        tf = nxt("tf", 2)
                        s = nxt("ob", NSL)
                        P.op("act", "activation", dict(out=tmpF[tf][:], in_=psb[bg][:, :], func=AF.Sigmoid),
                             r=[("ps", bg)], w=[("tmpF", tf)])
                        P.op("dve", "tensor_tensor", dict(out=ob[s][:], in0=psb[ba][:, :], in1=tmpF[tf][:],
                                                                                 op=ALU.mult),
                             r=[("ps", ba), ("tmpF", tf)], w=[("ob", s)])
                        P.op("sp", "dma_start", dict(out=d_hg[cc, :, t0:t0 + 512], in_=ob[s][:]),
                             r=[("ob", s)], dma=("ob", s))
                    for oc in range(8):
                        b = fm_group(C_GA + oc * 128)
                        evac_store(b, d_sga[oc, :, t0:t0 + 512], func=AF.Sigmoid)
                    for oc in range(8):
                        b = fm_group(C_GB + oc * 128)
                        evac_store(b, d_sgb[oc, :, t0:t0 + 512], func=AF.Sigmoid)
                    for j in range(4):
                        b = nxt("ps", 6)
                        for k in range(8):
                            P.op("pe", "matmul", dict(out=psb[b][:, :], lhsT=hT[:, k, j * 128:(j + 1) * 128],
                                                                          rhs=Win[:, k, C_V:C_V + 512], start=(k == 0), stop=(k == 7)),
                                 r=[("Win", k), ("hT", k)], w=[("ps", b)])
                        pv = psb[b][:, :].rearrange("p (a two d) -> p a two d", a=4, two=2)
                        vv = vst[:, j, :].rearrange("p (a x) -> p a x", a=4)
                        P.op("dve", "tensor_copy", dict(out=vv[:, :, 0:64], in_=pv[:, :, 0, :]),
                             r=[("ps", b)], w=[("vst", j)])
                        P.op("act", "activation", dict(out=vv[:, :, 128:192], in_=pv[:, :, 1, :], func=AF.Copy),
                             r=[("ps", b)], w=[("vst", j)])
                        P.op("sp", "dma_start", dict(out=d_V[c * 4 + j, :, :], in_=vst[:, j, :]),
                             r=[("vst", j)], dma=("vst", j))
                    b = nxt("ps", 6)
                    for j in range(4):
                        for k in range(8):
                            P.op("pe", "matmul", dict(out=psb[b][:, j * 8:(j + 1) * 8],
                                                                          lhsT=hT[:, k, j * 128:(j + 1) * 128],
                                                                          rhs=Win[:, k, C_WI:C_WI + 8], start=(k == 0), stop=(k == 7)),
                                 r=[("Win", k), ("hT", k)], w=[("ps", b)])
                    wv = wi_all[:, c * 4:(c + 1) * 4, :]
                    P.op("dve", "tensor_copy", dict(out=wv, in_=psb[b][:, 0:32].rearrange("p (j h) -> p j h", j=4)),
                         r=[("ps", b)], w=[("wi", c)])
                    P.op("act", "activation", dict(out=absw[:, c * 4:(c + 1) * 4, :], in_=wv, func=AF.Abs,
                                                   scale=(64 ** -0.5) * (8 ** -0.5)),
                         r=[("wi", c)], w=[("absw", c)])
                    P.op("dve", "tensor_scalar", dict(out=sgnw[:, c * 4:(c + 1) * 4, :], in0=wv, scalar1=0.0,
                                                                scalar2=2.0, op0=ALU.is_gt, op1=ALU.mult),
                         r=[("wi", c)], w=[("sgnw", c)])
                    P.op("dve", "tensor_scalar", dict(out=sgnw[:, c * 4:(c + 1) * 4, :], in0=sgnw[:, c * 4:(c + 1) * 4, :],
                                                          scalar1=-1.0, scalar2=None, op0=ALU.add),
                         r=[("sgnw", c)], w=[("sgnw", c)])
                P.flush()
            if stop == ("A", l):
                return nc

            with ExitStack() as ph:
                score = sb("score", [128, S], F32, ph)
                junk = sb("junkB", [128, S], F32, ph)
                maskA = [sb(f"maskA{i}", [128, S], BF16, ph) for i in range(2)]
                qTc = [sb(f"qTc{i}", [128, 4, 256], BF16, ph) for i in range(2)]
                qiTc = [sb(f"qiTc{i}", [128, 4, 256], BF16, ph) for i in range(2)]
                NKB = 2
                kTg = [sb(f"kTg{i}", [128, 4, 512], BF16, ph) for i in range(NKB)]
                Vg = [sb(f"Vg{i}", [128, 4, 768], BF16, ph) for i in range(NKB)]
                NR = 3
                Rb = [sb(f"Rb{i}", [128, 512], F32, ph) for i in range(NR)]
                NPT = 4
                Pt = [sb(f"Pt{i}", [128, 2, 256], BF16, ph) for i in range(NPT)]
                lo = sb("loB", [128, 2], F32, ph)
                hi = sb("hiB", [128, 2], F32, ph)
                mid = sb("midB", [128, 2], F32, ph)
                Wd = sb("WdB", [128, NIT + 1], F32, ph)
                cnt = sb("cntB", [128, 2], F32, ph)
                stp = sb("stpB", [128, 2], F32, ph)
                lo1 = sb("lo1B", [128, 2], F32, ph)
                hi1 = sb("hi1B", [128, 2], F32, ph)
                ssA = sb("ssAB", [128, 2], F32, ph)
                zeros2 = sb("zeros2B", [128, 2], F32, ph)
                oacc = [sb(f"oaccB{i}", [128, 4, 512], F32, ph) for i in range(2)]
                denx = [sb(f"denxB{i}", [128, 4, 256], F32, ph) for i in range(2)]
                oTs = [sb(f"oTs{i}", [128, 4, 256], BF16, ph) for i in range(2)]
                oTo = [sb(f"oTo{i}", [128, 4, 256], BF16, ph) for i in range(2)]
                rot = {"idx": 0, "R": 0, "L": 0, "pt": 0, "mt": 0, "kg": 0}

                def nxt(name, n):
                    v = rot[name]
                    rot[name] = (v + 1) % n
                    return v

                P.op("dve", "memset", dict(ap=cnt[:], constant=0.0), w=["cnt"])
                P.op("dve", "memset", dict(ap=ssA[:], constant=0.0), w=["ssA"])
                P.op("dve", "memset", dict(ap=zeros2[:], constant=0.0), w=["zeros2"])
                mTb = [psb[6][:].bitcast(BF16), psb[7][:].bitcast(BF16)]
                Lbanks = [4, 5]
                for tc in range(S // 256):
                    T0 = tc * 256
                    qs = tc % 2
                    P.op("sp", "dma_start", dict(out=qTc[qs][:], in_=d_qT.rearrange("a p s -> p a s")[:, :, T0:T0 + 256]),
                         w=[("qTc", qs)], dma=("qTc", qs))
                    P.op("sp", "dma_start", dict(out=qiTc[qs][:], in_=d_qiT.rearrange("a p s -> p a s")[:, :, T0:T0 + 256]),
                         w=[("qiTc", qs)], dma=("qiTc", qs))
                    for qt in range(2):
                        Tq = T0 + 128 * qt
                        ti = Tq // 128
                        Sx = Tq + 128
                        ng = (Sx + 511) // 512
                        for g in range(ng):
                            wd = min(512, Sx - g * 512)
                            for h in range(NH):
                                b = nxt("idx", 4)
                                hp, pr = (h % 2) * 64, h // 2
                                P.op("pe", "matmul", dict(out=psb[b][:, 0:wd], lhsT=qiTc[qs][hp:hp + 64, pr, qt * 128:(qt + 1) * 128],
                                    rhs=kidxT[hp:hp + 64, g * 512:g * 512 + wd], start=True, stop=True),
                                    r=[("qiTc", qs)], w=[("ps", b)])
                                rs = nxt("R", NR)
                                P.op("act", "activation", dict(
                                    out=Rb[rs][:, 0:wd], in_=psb[b][:, 0:wd], func=AF.Relu, scale=absw[:, ti, h:h + 1]),
                                    r=[("ps", b)], w=[("R", rs)])
                                sc = score[:, g * 512:g * 512 + wd]
                                if h == 0:
                                    P.op("dve", "tensor_scalar", dict(
                                        out=sc, in0=Rb[rs][:, 0:wd], scalar1=sgnw[:, ti, 0:1], scalar2=None, op0=ALU.mult),
                                        r=[("R", rs)], w=[("score", g)])
                                else:
                                    P.op("dve", "scalar_tensor_tensor", dict(
                                        out=sc, in0=Rb[rs][:, 0:wd], scalar=sgnw[:, ti, h:h + 1], in1=sc, op0=ALU.mult, op1=ALU.add),
                                        r=[("R", rs), ("score", g)], w=[("score", g)])
                        SG = [("score", g) for g in range(ng)]
                        P.op("dve", "tensor_scalar", dict(out=junk[:, 0:Sx], in0=score[:, 0:Sx], scalar1=1.0, scalar2=None,
                                                          op0=ALU.mult, op1=ALU.min, accum_out=lo1[:, 0:1]),
                             r=SG, w=["junk", "junkA", "lo1"])
                        P.op("dve", "tensor_tensor", dict(out=score[:, Tq:Tq + 128], in0=score[:, Tq:Tq + 128], in1=tri[:],
                                                                     op=ALU.add), r=SG + ["tri"], w=SG)
                        P.op("dve", "tensor_scalar", dict(out=junk[:, 0:Sx], in0=score[:, 0:Sx], scalar1=1.0, scalar2=None,
                                                          op0=ALU.mult, op1=ALU.max, accum_out=hi1[:, 0:1]),
                             r=SG, w=["junk", "junkA", "hi1"])
                        P.op("dve", "tensor_scalar", dict(out=lo[:], in0=zeros2[:], scalar1=lo1[:, 0:1], scalar2=-1.0, op0=ALU.add, op1=ALU.add),
                             r=["lo1", "zeros2"], w=["lo"])
                        P.op("dve", "tensor_scalar", dict(out=hi[:], in0=zeros2[:], scalar1=hi1[:, 0:1], scalar2=lo[:, 0:1], op0=ALU.add, op1=ALU.subtract),
                             r=["hi1", "lo", "zeros2"], w=["hi"])
                        P.op("dve", "tensor_scalar", dict(out=Wd[:], in0=pow2[:], scalar1=hi[:, 0:1], scalar2=0.5,
                                                              op0=ALU.mult, op1=ALU.mult), r=["hi", "pow2"], w=["Wd"])
                        P.op("dve", "tensor_scalar", dict(out=mid[:], in0=lo[:], scalar1=Wd[:, 0:1], scalar2=None, op0=ALU.add),
                             r=["lo", "Wd"], w=["mid"])
                        hD = min(Sx, max(128, int(round(ACT_SPLIT * Sx / 128.0)) * 128))
                        nA = Sx - hD
                        for k in range(NIT):
                            P.op("dve", "tensor_scalar", dict(out=junk[:, 0:hD], in0=score[:, 0:hD], scalar1=mid[:, 0:1],
                                                              scalar2=None, op0=ALU.is_ge, op1=ALU.add, accum_out=cnt[:, 0:1]),
                                 r=SG + ["mid"], w=["junk", "cnt"])
                            if nA > 0:
                                P.op("act", "activation", dict(out=junk[:, hD:Sx], in_=score[:, hD:Sx], func=AF.Sign, scale=-1.0,
                                                               bias=mid[:, 0:1], accum_out=ssA[:, 0:1]),
                                     r=SG + ["mid"], w=["junkA", "ssA"])
                                P.op("dve", "scalar_tensor_tensor", dict(out=cnt[:], in0=ssA[:], scalar=-0.5, in1=cnt[:], op0=ALU.mult, op1=ALU.add),
                                     r=["ssA", "cnt"], w=["cnt"])
                            P.op("dve", "tensor_scalar", dict(out=stp[:], in0=cnt[:], scalar1=TOPK - 0.5 - nA / 2.0, scalar2=Wd[:, k:k + 1],
                                                                       op0=ALU.is_gt, op1=ALU.mult), r=["cnt", "Wd"], w=["stp"])
                            P.op("dve", "scalar_tensor_tensor", dict(out=mid[:], in0=mid[:], scalar=Wd[:, k + 1:k + 2], in1=stp[:],
                                                                              op0=ALU.subtract, op1=ALU.add),
                                 r=["mid", "stp", "Wd"], w=["mid"])
                        P.op("dve", "tensor_scalar", dict(out=mid[:], in0=mid[:], scalar1=Wd[:, NIT:NIT + 1], scalar2=None, op0=ALU.subtract),
                             r=["mid", "Wd"], w=["mid"])
                        P.op("dve", "tensor_scalar", dict(out=maskA[qt][:, 0:Sx], in0=score[:, 0:Sx], scalar1=mid[:, 0:1],
                                                                            scalar2=None, op0=ALU.is_ge), r=SG + ["mid"], w=[("maskA", qt)])
                        if qt == 0:
                            P.op("dve", "memset", dict(ap=maskA[0][:, Sx:Sx + 128], constant=0.0), w=[("maskA", 0)])
                    if P.limit is not None or True:
                        import os
                        if os.environ.get('KDBG'):
                            print('ATT start tc', tc, 'nops', P.nops, flush=True)
                    nj = (T0 + 256) // 128
                    units = [(j, pr) for j in range(nj) for pr in range(4)]
                    state = {}

                    def qk(i):
                        j, pr = units[i]
                        g, jj = j // 4, j % 4
                        if pr == 0 and jj == 0:
                            kb = nxt("kg", NKB)
                            state["kb"] = kb
                            nch = min(4, nj - g * 4)
                            P.op("sp", "dma_start", dict(
                                out=kTg[kb][:, :, 0:nch * 128], in_=d_kT.rearrange("a p s -> p a s")[:, :, g * 512:g * 512 + nch * 128]),
                                w=[("kTg", kb)], dma=("kTg", kb))
                            P.op("sp", "dma_start", dict(
                                out=Vg[kb][:, 0:nch, :], in_=d_V[g * 4:g * 4 + nch, :, :].rearrange("c p x -> p c x")),
                                w=[("Vg", kb)], dma=("Vg", kb))
                        kb = state["kb"]
                        if pr == 0:
                            ms = nxt("mt", 2)
                            state["ms"] = ms
                            for qt in range(2):
                                P.op("pe", "transpose", dict(
                                    out=mTb[ms][:, qt * 128:(qt + 1) * 128],
                                    in_=maskA[qt][:, j * 128:(j + 1) * 128], identity=identB[:]),
                                    r=[("maskA", qt), "identB"], w=[("ps", 6 + ms)])
                        ms = state["ms"]
                        u = nxt("L", 2)
                        off = j * 128 - T0
                        near = off in (-128, 0, 128)
                        for hh in range(2):
                            h = 2 * pr + hh
                            Lv = Lt[:, hh * 512 + u * 256: hh * 512 + (u + 1) * 256]
                            P.op("pe", "matmul", dict(out=Lv, lhsT=kTg[kb][hh * 64:(hh + 1) * 64, pr, jj * 128:(jj + 1) * 128],
                                rhs=qTc[qs][hh * 64:(hh + 1) * 64, pr, :], start=True, stop=not near),
                                r=[("kTg", kb), ("qTc", qs)], w=[("ps", "L")])
                            if near:
                                oi = off // 128 + 1
                                P.op("pe", "matmul", dict(out=Lv, lhsT=identB[:], rhs=biasadj[:, h, oi, :],
                                                                                 start=False, stop=True),
                                     r=[("biasadj", h), "identB"], w=[("ps", "L")])
                        ps_ = nxt("pt", NPT)
                        Lin = Lt[:, :].rearrange("p (h u t) -> p h u t", h=2, u=2)[:, :, u, :]
                        P.op("act", "activation", dict(out=Pt[ps_][:], in_=Lin, func=AF.Exp), r=[("ps", "L")], w=[("Pt", ps_)])
                        P.op("dve", "tensor_tensor", dict(
                            out=Pt[ps_][:], in0=Pt[ps_][:], in1=mTb[ms][:, 0:256].unsqueeze(1).to_broadcast([128, 2, 256]),
                            op=ALU.mult), r=[("Pt", ps_), ("ps", 6 + ms)], w=[("Pt", ps_)])
                        return (ps_, kb)

                    def pv(i, info):
                        ps_, kb = info
                        j, pr = units[i]
                        jj = j % 4
                        for hh in range(2):
                            P.op("pe", "matmul", dict(out=psb[pr][:, hh * 256:(hh + 1) * 256], lhsT=Vg[kb][:, jj, pr * 192 + hh * 64: pr * 192 + hh * 64 + 128],
                                rhs=Pt[ps_][:, hh, :], start=(j == 0 and hh == 0), stop=(j == nj - 1), skip_group_check=True),
                                r=[("Vg", kb), ("Pt", ps_)], w=[("ps", pr)])

                    infos = {}
                    DEPTH = 1
                    for i in range(len(units)):
                        infos[i] = qk(i)
                        if i >= DEPTH:
                            pv(i - DEPTH, infos.pop(i - DEPTH))
                    for i in range(max(0, len(units) - DEPTH), len(units)):
                        pv(i, infos.pop(i))
                    import os
                    if os.environ.get('KDBG'):
                        print('NORM start tc', tc, 'nops', P.nops, flush=True)
                    os_ = tc % 2
                    for pr in range(4):
                        eng = "act" if pr % 2 == 0 else "dve"
                        if eng == "act":
                            P.op("act", "activation", dict(out=oacc[os_][:, pr, :], in_=psb[pr][:, :], func=AF.Copy),
                                 r=[("ps", pr)], w=[("oacc", os_, pr)])
                        else:
                            P.op("dve", "tensor_copy", dict(out=oacc[os_][:, pr, :], in_=psb[pr][:, :]),
                                 r=[("ps", pr)], w=[("oacc", os_, pr)])
                    OA = [("oacc", os_, pr) for pr in range(4)]
                    P.op("sp", "dma_start", dict(out=denx[os_][0:64, :, :], in_=oacc[os_][64:128, :, 0:256]),
                         r=OA, w=[("denx", os_, 0)], dma=("denx", os_, 0))
                    P.op("sp", "dma_start", dict(out=denx[os_][64:128, :, :], in_=oacc[os_][0:64, :, 256:512]),
                         r=OA, w=[("denx", os_, 1)], dma=("denx", os_, 1))
                    P.op("dve", "reciprocal", dict(out=denx[os_][:], in_=denx[os_][:]),
                         r=[("denx", os_, 0), ("denx", os_, 1)], w=[("denx", os_, 0), ("denx", os_, 1)])
                    P.op("dve", "tensor_tensor", dict(out=oTs[os_][:], in0=oacc[os_][:, :, 0:256], in1=denx[os_][:], op=ALU.mult),
                         r=OA + [("denx", os_, 0), ("denx", os_, 1)], w=[("oTs", os_, 0)])
                    P.op("dve", "tensor_tensor", dict(out=oTo[os_][:], in0=oacc[os_][:, :, 256:512], in1=denx[os_][:], op=ALU.mult),
                         r=OA + [("denx", os_, 0), ("denx", os_, 1)], w=[("oTs", os_, 1)])
                    dview = d_oT.rearrange("a p s -> p a s")
                    P.op("sp", "dma_start", dict(out=dview[0:64, :, T0:T0 + 256], in_=oTs[os_][0:64, :, :]),
                         r=[("oTs", os_, 0)], dma=("oTs", os_, 0))
                    P.op("sp", "dma_start", dict(out=dview[64:128, :, T0:T0 + 256], in_=oTo[os_][64:128, :, :]),
                         r=[("oTs", os_, 1)], dma=("oTs", os_, 1))
                P.flush()
            if stop == ("B", l):
                return nc

            with ExitStack() as ph:
                Wao = sb("Wao", [128, 4, D], BF16, ph)
                Wco = sb("Wco", [128, 4, D], BF16, ph)
                Wmo = sb("Wmo", [128, 8, D], BF16, ph)
                Dg = sb("Dg", [128, 4, CW, 128], BF16, ph)
                xt = sb("xtC", [128, 4, D], F32, ph)
                oTc = sb("oTc", [128, 4, 512], BF16, ph)
                hg = sb("hgC", [128, 4, 512 + CW - 1], BF16, ph)
                sga = sb("sgaC", [128, 8, 512], BF16, ph)
                sgb = sb("sgbC", [128, 8, 512], BF16, ph)
                conv = sb("convC", [128, 4, 512], F32, ph)
                sq = sb("sqC", [128, 4, 512], F32, ph)
                mean_s = sb("meanC", [128, 512], F32, ph)
                var_s = sb("varC", [128, 512], F32, ph)
                ytmp = [sb(f"ytmpC{i}", [128, 512], F32, ph) for i in range(2)]
                ycT = sb("ycT", [128, 4, 512], BF16, ph)
                merged = sb("mergedC", [128, 8, 512], BF16, ph)
                t1 = [sb(f"t1C{i}", [128, 512], F32, ph) for i in range(2)]
                t2 = [sb(f"t2C{i}", [128, 512], F32, ph) for i in range(2)]
                P.op("pool", "dma_start", dict(out=Wao[:], in_=w_ao_d[l].rearrange("(k p) n -> p k n", p=128)), w=["Wao"], dma=("w", 0))
                P.op("pool", "dma_start", dict(out=Wco[:], in_=w_co_d[l].rearrange("(k p) n -> p k n", p=128)), w=["Wco"], dma=("w", 1))
                P.op("pool", "dma_start", dict(out=Wmo[:], in_=w_mo_d[l].rearrange("(k p) n -> p k n", p=128)), w=["Wmo"], dma=("w", 2))
                for cc in range(4):
                    for k in range(CW):
                        col = PB + P_DW + cc * CW + k
                        P.op("dve", "tensor_scalar", dict(out=Dg[:, cc, k, :], in0=identF[:], scalar1=par[:, col:col + 1],
                                                                                  scalar2=None, op0=ALU.mult), w=[("Dg", cc)])
                rot = {"ps": 0, "y": 0, "t": 0}

                def nxt(name, n):
                    v = rot[name]
                    rot[name] = (v + 1) % n
                    return v

                HW_ = CW - 1
                for c in range(S // 512):
                    t0 = c * 512
                    P.op("sp", "dma_start", dict(out=xt[:], in_=x_src[t0:t0 + 512, :].rearrange("(j p) d -> p j d", p=128)),
                         w=["xt"], dma="xt")
                    P.op("sp", "dma_start", dict(out=oTc[:], in_=d_oT.rearrange("a p s -> p a s")[:, :, t0:t0 + 512]),
                         w=["oTc"], dma="oTc")
                    if c == 0:
                        P.op("dve", "memset", dict(ap=hg[:, :, 0:HW_], constant=0.0), w=["hg"])
                        P.op("sp", "dma_start", dict(out=hg[:, :, HW_:HW_ + 512], in_=d_hg.rearrange("a p s -> p a s")[:, :, 0:512]),
                             w=["hg"], dma="hg")
                    else:
                        P.op("sp", "dma_start", dict(out=hg[:], in_=d_hg.rearrange("a p s -> p a s")[:, :, t0 - HW_:t0 + 512]),
                             w=["hg"], dma="hg")
                    P.op("sp", "dma_start", dict(out=sga[:], in_=d_sga.rearrange("a p s -> p a s")[:, :, t0:t0 + 512]),
                         w=["sga"], dma="sga")
                    P.op("sp", "dma_start", dict(out=sgb[:], in_=d_sgb.rearrange("a p s -> p a s")[:, :, t0:t0 + 512]),
                         w=["sgb"], dma="sgb")
                    for cc in range(4):
                        b = nxt("ps", 4)
                        for k in range(CW):
                            P.op("pe", "matmul", dict(out=psb[b][:, :], lhsT=Dg[:, cc, k, :], rhs=hg[:, cc, k:k + 512],
                                                                           start=(k == 0), stop=(k == CW - 1)),
                                 r=[("Dg", cc), "hg"], w=[("ps", b)])
                        P.op("dve", "tensor_scalar", dict(out=conv[:, cc, :], in0=psb[b][:, :],
                                                                          scalar1=par[:, PB + P_DWB + cc:PB + P_DWB + cc + 1], scalar2=None,
                                                                          op0=ALU.add), r=[("ps", b)], w=[("conv", cc)])
                        P.op("act", "activation", dict(out=sq[:, cc, :], in_=conv[:, cc, :], func=AF.Square),
                             r=[("conv", cc)], w=[("sq", cc)])
                    bm, bq = 4, 5
                    for cc in range(4):
                        P.op("pe", "matmul", dict(out=psb[bm][:, :], lhsT=onesF[:], rhs=conv[:, cc, :], start=(cc == 0), stop=(cc == 3)),
                             r=[("conv", cc), "onesF"], w=[("ps", bm)])
                    for cc in range(4):
                        P.op("pe", "matmul", dict(out=psb[bq][:, :], lhsT=onesF[:], rhs=sq[:, cc, :], start=(cc == 0), stop=(cc == 3)),
                             r=[("sq", cc), "onesF"], w=[("ps", bq)])
                    P.op("act", "activation", dict(out=mean_s[:], in_=psb[bm][:, :], func=AF.Copy), r=[("ps", bm)], w=["mean"])
                    P.op("dve", "tensor_tensor", dict(out=var_s[:], in0=mean_s[:], in1=mean_s[:], op=ALU.mult), r=["mean"], w=["var"])
                    P.op("dve", "tensor_tensor", dict(out=var_s[:], in0=psb[bq][:, :], in1=var_s[:], op=ALU.subtract),
                         r=[("ps", bq), "var"], w=["var"])
                    P.op("dve", "tensor_scalar", dict(out=var_s[:], in0=var_s[:], scalar1=0.0, scalar2=EPS, op0=ALU.max, op1=ALU.add),
                         r=["var"], w=["var"])
                    P.op("act", "activation", dict(out=var_s[:], in_=var_s[:], func=AF.Sqrt), r=["var"], w=["var"])
                    P.op("dve", "reciprocal", dict(out=var_s[:], in_=var_s[:]), r=["var"], w=["var"])
                    for cc in range(4):
                        y = nxt("y", 2)
                        P.op("dve", "tensor_tensor", dict(out=ytmp[y][:], in0=conv[:, cc, :], in1=mean_s[:], op=ALU.subtract),
                             r=[("conv", cc), "mean"], w=[("ytmp", y)])
                        P.op("pool", "tensor_tensor", dict(out=ytmp[y][:], in0=ytmp[y][:], in1=var_s[:], op=ALU.mult),
                             r=[("ytmp", y), "var"], w=[("ytmp", y)])
                        P.op("act", "activation", dict(out=ycT[:, cc, :], in_=ytmp[y][:], func=AF.Silu,
                                                                       scale=par[:, PB + P_CNG + cc:PB + P_CNG + cc + 1],
                                                                       bias=par[:, PB + P_CNB + cc:PB + P_CNB + cc + 1]),
                             r=[("ytmp", y)], w=[("ycT", cc)])
                    for oc in range(8):
                        ba = nxt("ps", 4)
                        for pr in range(4):
                            P.op("pe", "matmul", dict(out=psb[ba][:, :], lhsT=Wao[:, pr, oc * 128:(oc + 1) * 128],
                                                                               rhs=oTc[:, pr, :], start=(pr == 0), stop=(pr == 3)),
                                 r=["Wao", "oTc"], w=[("ps", ba)])
                        bb = nxt("ps", 4)
                        for cc in range(4):
                            P.op("pe", "matmul", dict(out=psb[bb][:, :], lhsT=Wco[:, cc, oc * 128:(oc + 1) * 128],
                                                                               rhs=ycT[:, cc, :], start=(cc == 0), stop=(cc == 3)),
                                 r=["Wco", ("ycT", cc)], w=[("ps", bb)])
                        ts = nxt("t", 2)
                        P.op("dve", "tensor_tensor", dict(out=t1[ts][:], in0=psb[ba][:, :], in1=sga[:, oc, :], op=ALU.mult),
                             r=[("ps", ba), "sga"], w=[("t1", ts)])
                        P.op("dve", "tensor_tensor", dict(out=t2[ts][:], in0=psb[bb][:, :], in1=sgb[:, oc, :], op=ALU.mult),
                             r=[("ps", bb), "sgb"], w=[("t2", ts)])
                        P.op("pool", "tensor_tensor", dict(out=merged[:, oc, :], in0=t1[ts][:], in1=t2[ts][:], op=ALU.add),
                             r=[("t1", ts), ("t2", ts)], w=[("merged", oc)])
                    for j in range(4):
                        for n in range(2):
                            b = nxt("ps", 4)
                            for k in range(8):
                                P.op("pe", "matmul", dict(out=psb[b][:, :], lhsT=merged[:, k, j * 128:(j + 1) * 128],
                                                                                 rhs=Wmo[:, k, n * 512:(n + 1) * 512], start=(k == 0), stop=(k == 7)),
                                     r=[("merged", k), "Wmo"], w=[("ps", b)])
                            P.op("dve", "tensor_tensor", dict(out=xt[:, j, n * 512:(n + 1) * 512], in0=psb[b][:, :],
                                                                                in1=xt[:, j, n * 512:(n + 1) * 512], op=ALU.add),
                                 r=[("ps", b), "xt"], w=["xt"])
                    P.op("sp", "dma_start", dict(out=d_xa[t0:t0 + 512, :].rearrange("(j p) d -> p j d", p=128), in_=xt[:]),
                         r=["xt"], dma="xo")
                P.flush()
            if stop == ("C1", l):
                return nc

            last = (l == NL - 1)
            with ExitStack() as ph:
                Wfi = sb("Wfi", [128, 8, 2 * DFF], BF16, ph)
                Wfo = sb("Wfo", [128, 22, D], BF16, ph)
                xt2 = [sb(f"xtF{i}", [128, 2, D], F32, ph) for i in range(1)]
                hb = sb("hbF", [128, 2, D], BF16, ph)
                hT = sb("hTF", [128, 8, 256], BF16, ph)
                ss = sb("ssF", [128, 2], F32, ph)
                rstd = sb("rstdF", [128, 2], F32, ph)
                actT = sb("actT", [128, 22, 256], BF16, ph)
                tmpF = [sb(f"tmpFF{i}", [128, 256], F32, ph) for i in range(2)]
                if last:
                    gfin = sb("gfin", [128, D], F32, ph)
                    P.op("sp", "dma_start", dict(out=gfin[:], in_=gfin_d[:, :]), w=["gfin"], dma="gfin")
                for k in range(8):
                    P.op("pool", "dma_start", dict(out=Wfi[:, k, :], in_=w_fi_d[l, k * 128:(k + 1) * 128, :]),
                         w=[("Wfi", k)], dma=("w", k))
                for k in range(22):
                    P.op("pool", "dma_start", dict(out=Wfo[:, k, :], in_=w_fo_d[l, k * 128:(k + 1) * 128, :]),
                         w=[("Wfo", k)], dma=("w", 8 + k))
                rot = {"ps": 0, "tp": 0, "tf": 0}
                dst = out_d if last else d_xb
                for c in range(S // 256):
                    t0 = c * 256
                    xs = 0
                    xt = xt2[xs]
                    P.op("sp", "dma_start", dict(out=xt[:], in_=d_xa[t0:t0 + 256, :].rearrange("(j p) d -> p j d", p=128)),
                         w=[("xt", xs)], dma=("xt", xs))
                    for j in range(2):
                        P.op("act", "activation", dict(out=hb[:, j, :], in_=xt[:, j, :], func=AF.Square, accum_out=ss[:, j:j + 1]),
                             r=[("xt", xs)], w=[("hb", j), ("ss", j)])
                    P.op("dve", "tensor_scalar", dict(out=rstd[:], in0=ss[:], scalar1=1.0 / D, scalar2=EPS, op0=ALU.mult, op1=ALU.add),
                         r=[("ss", 0), ("ss", 1)], w=["rstd"])
                    P.op("act", "activation", dict(out=rstd[:], in_=rstd[:], func=AF.Sqrt), r=["rstd"], w=["rstd"])
                    P.op("dve", "reciprocal", dict(out=rstd[:], in_=rstd[:]), r=["rstd"], w=["rstd"])
                    for j in range(2):
                        P.op("dve", "tensor_scalar", dict(out=hb[:, j, :], in0=xt[:, j, :], scalar1=rstd[:, j:j + 1], scalar2=None,
                                                                         op0=ALU.mult), r=[("xt", xs), "rstd"], w=[("hb", j)])
                    for k in range(8):
                        tb = 6 + (rot["tp"] % 2)
                        rot["tp"] += 1
                        pT = psb[tb][:].bitcast(BF16)
                        for j in range(2):
                            P.op("pe", "transpose", dict(out=pT[:, j * 128:(j + 1) * 128], in_=hb[:, j, k * 128:(k + 1) * 128],
                                                                              identity=identB[:]), r=[("hb", j), "identB"], w=[("ps", tb)])
                        col = PB + P_FFNG + k
                        if k % 2 == 0:
                            P.op("dve", "tensor_scalar", dict(out=hT[:, k, :], in0=pT[:, 0:256], scalar1=par[:, col:col + 1],
                                                                                      scalar2=None, op0=ALU.mult), r=[("ps", tb)], w=[("hT", k)])
                        else:
                            P.op("act", "activation", dict(out=hT[:, k, :], in_=pT[:, 0:256], func=AF.Identity,
                                                                                   scale=par[:, col:col + 1]), r=[("ps", tb)], w=[("hT", k)])
                    for fc in range(22):
                        b = rot["ps"] % 4
                        rot["ps"] += 1
                        for half, c0 in ((0, fc * 128), (1, DFF + fc * 128)):
                            for k in range(8):
                                P.op("pe", "matmul", dict(out=psb[b][:, half * 256:(half + 1) * 256],
                                                                                         lhsT=Wfi[:, k, c0:c0 + 128], rhs=hT[:, k, :],
                                                                                         start=(k == 0), stop=(k == 7)),
                                     r=[("Wfi", k), ("hT", k)], w=[("ps", b)])
                        tf = rot["tf"] % 2
                        rot["tf"] += 1
                        P.op("act", "activation", dict(out=tmpF[tf][:], in_=psb[b][:, 0:256], func=AF.Silu),
                             r=[("ps", b)], w=[("tmpF", tf)])
                        P.op("dve", "tensor_tensor", dict(out=actT[:, fc, :], in0=psb[b][:, 256:512], in1=tmpF[tf][:], op=ALU.mult),
                             r=[("ps", b), ("tmpF", tf)], w=[("actT", fc)])
                    for j in range(2):
                        for n in range(2):
                            b = 4 + (rot["ps"] % 2)
                            rot["ps"] += 1
                            for fc in range(22):
                                P.op("pe", "matmul", dict(out=psb[b][:, :], lhsT=actT[:, fc, j * 128:(j + 1) * 128],
                                                                                   rhs=Wfo[:, fc, n * 512:(n + 1) * 512], start=(fc == 0), stop=(fc == 21)),
                                     r=[("actT", fc), ("Wfo", fc)], w=[("ps", b)])
                            P.op("dve", "tensor_tensor", dict(out=xt[:, j, n * 512:(n + 1) * 512], in0=psb[b][:, :],
                                                                                       in1=xt[:, j, n * 512:(n + 1) * 512], op=ALU.add),
                                 r=[("ps", b), ("xt", xs)], w=[("xt", xs)])
                    if last:
                        for j in range(2):
                            P.op("act", "activation", dict(out=hb[:, j, :], in_=xt[:, j, :], func=AF.Square, accum_out=ss[:, j:j + 1]),
                                 r=[("xt", xs)], w=[("hb", j), ("ss", j)])
                        P.op("dve", "tensor_scalar", dict(out=rstd[:], in0=ss[:], scalar1=1.0 / D, scalar2=EPS, op0=ALU.mult, op1=ALU.add),
                             r=[("ss", 0), ("ss", 1)], w=["rstd"])
                        P.op("act", "activation", dict(out=rstd[:], in_=rstd[:], func=AF.Sqrt), r=["rstd"], w=["rstd"])
                        P.op("dve", "reciprocal", dict(out=rstd[:], in_=rstd[:]), r=["rstd"], w=["rstd"])
                        for j in range(2):
                            P.op("dve", "scalar_tensor_tensor", dict(out=xt[:, j, :], in0=xt[:, j, :], scalar=rstd[:, j:j + 1], in1=gfin[:],
                                                                                    op0=ALU.mult, op1=ALU.mult),
                                 r=[("xt", xs), "rstd", "gfin"], w=[("xt", xs)])
                        P.op("sp", "dma_start", dict(out=dst[t0:t0 + 256, :].rearrange("(j p) d -> p j d", p=128), in_=xt[:]),
                             r=[("xt", xs)], dma=("xo", xs))
                    else:
                        P.op("sp", "dma_start", dict(out=dst[t0:t0 + 256, :].rearrange("(j p) d -> p j d", p=128), in_=xt[:]),
                             r=[("xt", xs)], dma=("xo", xs))
                P.flush()
            x_src = d_xb
    return nc


def t5_bucket_np(dist):
    n = np.maximum(dist, 0)
    nf = np.maximum(n, 1).astype(np.float32)
    large = 16 + (np.log(nf / np.float32(16)) / np.float32(math.log(128 / 16)) * np.float32(16)).astype(np.int32)
    large = np.minimum(large, 31)
    return np.where(n < 16, n, large)


def host_inputs(inputs, S, NL):
    f = lambda a: np.ascontiguousarray(np.asarray(a, dtype=np.float32))
    par = np.zeros((128, NL * P_LSZ + 8), np.float32)
    for l in range(NL):
        b = l * P_LSZ
        par[:, b + P_MIXG:b + P_MIXG + 8] = f(inputs["mix_norm"][l]).reshape(8, 128).T
        par[:, b + P_FFNG:b + P_FFNG + 8] = f(inputs["ffn_norm"][l]).reshape(8, 128).T
        dw = f(inputs["dw_kernel"][l])
        par[:, b + P_DW:b + P_DW + 4 * CW] = dw.T.reshape(4, 128, CW).transpose(1, 0, 2).reshape(128, 4 * CW)
        par[:, b + P_DWB:b + P_DWB + 4] = f(inputs["dw_bias"][l]).reshape(4, 128).T
        par[:, b + P_CNG:b + P_CNG + 4] = f(inputs["conv_norm_g"][l]).reshape(4, 128).T
        par[:, b + P_CNB:b + P_CNB + 4] = f(inputs["conv_norm_b"][l]).reshape(4, 128).T
    rb = f(inputs["rel_bias"])
    par[:, NL * P_LSZ:NL * P_LSZ + 8] = np.broadcast_to(rb[31:32, :], (128, 8))
    s_l = np.arange(128)[:, None]
    t_l = np.arange(256)[None, :]
    biasT = np.zeros((128, NH, 3, 256), np.float32)
    for oi, off in enumerate((-128, 0, 128)):
        bk = t5_bucket_np(t_l - s_l - off)
        biasT[:, :, oi, :] = rb[bk].transpose(0, 2, 1)
    gfin = np.ascontiguousarray(np.broadcast_to(f(inputs["final_norm"])[None, :], (128, D)))
    ident = np.eye(128, dtype=np.float32)
    tri = np.where(np.arange(128)[None, :] <= np.arange(128)[:, None], 0.0, -1e30).astype(np.float32)
    pow2 = np.broadcast_to((2.0 ** -np.arange(NIT + 1)).astype(np.float32)[None, :], (128, NIT + 1)).copy()
    shared = {
        "w_in": f(inputs["w_in"]), "w_attn_out": f(inputs["w_attn_out"]), "w_conv_out": f(inputs["w_conv_out"]),
        "w_mix_out": f(inputs["w_mix_out"]), "w_ffn_in": f(inputs["w_ffn_in"]), "w_ffn_out": f(inputs["w_ffn_out"]),
        "params": par, "gfin": gfin, "biasT": biasT, "ident": ident, "tri": tri, "pow2": pow2,
    }
    return shared


_NC_CACHE = {}


def kernel(**inputs):
    x = np.asarray(inputs["x"], dtype=np.float32)
    B, S, _ = x.shape
    NL = int(np.asarray(inputs["w_in"]).shape[0])
    key = (S, NL)
    if key not in _NC_CACHE:
        _NC_CACHE[key] = build_program(S, NL)
    nc = _NC_CACHE[key]
    shared = host_inputs(inputs, S, NL)
    in_maps = []
    for b in range(B):
        m = dict(shared)
        m["x"] = np.ascontiguousarray(x[b])
        in_maps.append(m)
    res = run_bass_kernel_spmd(nc, in_maps, core_ids=list(range(B)))
    out = np.stack([np.asarray(r["out"], dtype=np.float32) for r in res.results], axis=0)
    return out
```

```python
import math
from contextlib import ExitStack

import numpy as np
import concourse.bass as bass
import concourse.mybir as mybir
from concourse.bass_utils import run_bass_kernel_spmd

F32 = mybir.dt.float32
BF16 = mybir.dt.bfloat16
AF = mybir.ActivationFunctionType
ALU = mybir.AluOpType
AX = mybir.AxisListType

D = 1024
NH = 8
DH = 64
AW = 512
CC = 512
CW = 31
DFF = 2816
NIN = 5192
TOPK = 256
EPS = 1e-6
NIT = 20
SAME_ENGINE_SYNC = True
WIDE_DUP = False
ACT_SPLIT = 0.42

C_Q, C_K, C_V, C_QI, C_KI, C_WI, C_UA, C_UG, C_GA, C_GB = 0, 512, 1024, 1536, 2048, 2112, 2120, 2632, 3144, 4168

P_MIXG, P_FFNG, P_DW, P_DWB, P_CNG, P_CNB = 0, 8, 16, 140, 144, 148
P_LSZ = 152


class Prog:
    ENG = ("sp", "act", "dve", "pool", "pe")

    def __init__(self, nc):
        self.nc = nc
        self.eng_sem = {e: [nc.alloc_semaphore(name=f"es_{e}"), 0] for e in ("act", "dve", "pool", "pe")}
        self.dma = {}
        self.buf = {}
        self.q = {e: [] for e in self.ENG}
        self.waited = {e: {} for e in self.ENG}
        self.nblk = 0

    def _dma(self, key):
        d = self.dma.get(key)
        if d is None:
            d = [self.nc.alloc_semaphore(name=f"ds_{len(self.dma)}"), 0]
            self.dma[key] = d
        return d

    limit = None
    nops = 0

    def op(self, eng, name, kw, r=(), w=(), dma=None):
        self.nops += 1
        if self.limit is not None and self.nops > self.limit:
            return
        fn = (name, kw)
        deps = {}

        def add(t):
            if t is None:
                return
            k = id(t[0])
            if k not in deps or deps[k][1] < t[1]:
                deps[k] = t

        for b in r:
            st = self.buf.get(b)
            if st:
                add(st[0])
        for b in w:
            st = self.buf.get(b)
            if st:
                add(st[0])
                for t in st[1].values():
                    add(t)
        if dma is None:
            es = self.eng_sem[eng]
            es[1] += 1
            ticket = (es[0], es[1], eng)
            inc = 1
        else:
            d = self._dma(dma)
            d[1] += 16
            ticket = (d[0], d[1], None)
            inc = 16
        waits = []
        for k, (sem, val, src) in deps.items():
            if src == eng and (eng == "pe" or not SAME_ENGINE_SYNC):
                continue
            if self.waited[eng].get(k, 0) >= val:
                continue
            self.waited[eng][k] = val
            waits.append((sem, val))
        self.q[eng].append((waits, fn, ticket[0], inc))
        for b in r:
            st = self.buf.setdefault(b, [None, {}])
            st[1][id(ticket[0])] = ticket
        for b in w:
            self.buf[b] = [ticket, {}]

    def flush(self):
        nc = self.nc
        waits = []
        for d in self.dma.values():
            if d[1] > 0 and self.waited["sp"].get(id(d[0]), 0) < d[1]:
                self.waited["sp"][id(d[0])] = d[1]
                waits.append((d[0], d[1]))
        self.q["sp"].append((waits, None, None, 0))
        self.nblk += 1
        import os
        if os.environ.get('KDBG'):
            print('FLUSH', self.nblk, 'nops', self.nops, flush=True)
        with nc.Block() as block:
            for e, deco in (("sp", block.sync), ("act", block.scalar), ("dve", block.vector),
                            ("pool", block.gpsimd), ("pe", block.tensor)):
                items = self.q[e]

                def body(engine, items=items):
                    for waits, fn, sem, inc in items:
                        for (s, v) in waits:
                            engine.wait_ge(s, v)
                        if fn is not None:
                            getattr(engine, fn[0])(**fn[1]).then_inc(sem, inc)

                deco(body)
        self.q = {e: [] for e in self.ENG}
        self.buf = {}


def build_program(S, NL, stop=None, debug=False, limit=None):
    assert S % 512 == 0
    NT = S // 128
    nc = bass.Bass("TRN2", target_bir_lowering=False)
    dt_in = lambda name, shape: nc.dram_tensor(name, list(shape), F32, kind="ExternalInput").ap()
    x_in = dt_in("x", [S, D])
    w_in_d = dt_in("w_in", [NL, D, NIN])
    w_ao_d = dt_in("w_attn_out", [NL, AW, D])
    w_co_d = dt_in("w_conv_out", [NL, CC, D])
    w_mo_d = dt_in("w_mix_out", [NL, D, D])
    w_fi_d = dt_in("w_ffn_in", [NL, D, 2 * DFF])
    w_fo_d = dt_in("w_ffn_out", [NL, DFF, D])
    NPAR = NL * P_LSZ + 8
    par_d = dt_in("params", [128, NPAR])
    gfin_d = dt_in("gfin", [128, D])
    bias_d = dt_in("biasT", [128, NH, 3, 256])
    ident_d = dt_in("ident", [128, 128])
    tri_d = dt_in("tri", [128, 128])
    pow2_d = dt_in("pow2", [128, NIT + 1])
    out_d = nc.dram_tensor("out", [S, D], F32, kind="ExternalOutput").ap()

    scr = lambda name, shape, dt: nc.dram_tensor(name, list(shape), dt, kind=("ExternalOutput" if debug else "Internal")).ap()
    d_xa = scr("d_xa", [S, D], F32)
    d_xb = scr("d_xb", [S, D], F32)
    d_qT = scr("d_qT", [4, 128, S], BF16)
    d_kT = scr("d_kT", [4, 128, S], BF16)
    d_qiT = scr("d_qiT", [4, 128, S], BF16)
    d_V = scr("d_V", [NT, 128, 768], BF16)
    d_hg = scr("d_hg", [4, 128, S], BF16)
    d_sga = scr("d_sga", [8, 128, S], BF16)
    d_sgb = scr("d_sgb", [8, 128, S], BF16)
    d_oT = scr("d_oT", [4, 128, S], BF16)

    P = Prog(nc)
    P.limit = limit
    with ExitStack() as top:
        uid = [0]

        def sb(name, shape, dt, stack=top):
            uid[0] += 1
            return stack.enter_context(nc.sbuf_tensor(f"s{uid[0]}_{name}", list(shape), dt))

        _pa = [top.enter_context(nc.psum_tensor(f"psb{i}", [128, 512], F32)) for i in range(4)]
        Lt = top.enter_context(nc.psum_tensor("psL", [128, 1024], F32))
        _pb = [top.enter_context(nc.psum_tensor(f"psb{i}", [128, 512], F32)) for i in (6, 7)]
        psb = [t[:] for t in _pa] + [Lt[:, 0:512], Lt[:, 512:1024]] + [t[:] for t in _pb]
        par = sb("par", [128, NPAR], F32)
        identF = sb("identF", [128, 128], F32)
        identB = sb("identB", [128, 128], BF16)
        tri = sb("tri", [128, 128], F32)
        pow2 = sb("pow2", [128, NIT + 1], F32)
        onesF = sb("onesF", [128, 128], F32)
        biasadj = sb("biasadj", [128, NH, 3, 256], BF16)
        kidxT = sb("kidxT", [128, S], BF16)
        wi_all = sb("wi_all", [128, NT, 8], F32)
        absw = sb("absw", [128, NT, 8], F32)
        sgnw = sb("sgnw", [128, NT, 8], F32)
        B31 = NL * P_LSZ

        with ExitStack() as ph:
            bstg = sb("bstg", [128, NH, 3, 256], F32, ph)
            P.op("sp", "dma_start", dict(out=par[:], in_=par_d[:, :]), w=["par"], dma="l0")
            P.op("sp", "dma_start", dict(out=identF[:], in_=ident_d[:, :]), w=["identF"], dma="l1")
            P.op("pool", "dma_start", dict(out=identB[:], in_=ident_d[:, :]), w=["identB"], dma="l2")
            P.op("sp", "dma_start", dict(out=tri[:], in_=tri_d[:, :]), w=["tri"], dma="l3")
            P.op("sp", "dma_start", dict(out=pow2[:], in_=pow2_d[:, :]), w=["pow2"], dma="l4")
            P.op("sp", "dma_start", dict(out=bstg[:], in_=bias_d[:]), w=["bstg"], dma="l5")
            P.op("dve", "memset", dict(ap=onesF[:], constant=1.0 / CC), w=["onesF"])
            for h in range(NH):
                P.op("dve", "tensor_scalar", dict(out=biasadj[:, h, :, :], in0=bstg[:, h, :, :],
                                                            scalar1=par[:, B31 + h:B31 + h + 1], scalar2=None,
                                                            op0=ALU.subtract),
                     r=["bstg", "par"], w=[("biasadj", h)])
            P.flush()
        if stop == "setup":
            return nc

        x_src = x_in
        for l in range(NL):
            PB = l * P_LSZ
            with ExitStack() as ph:
                Win = sb("Win", [128, 8, NIN], BF16, ph)
                xt = sb("xtA", [128, 4, D], F32, ph)
                hb = sb("hbA", [128, 4, D], BF16, ph)
                hT = sb("hTA", [128, 8, 512], BF16, ph)
                ss = sb("ssA", [128, 4], F32, ph)
                rstd = sb("rstdA", [128, 4], F32, ph)
                NSL = 8
                ob = [sb(f"obA{i}", [128, 512], BF16, ph) for i in range(NSL)]
                tmpF = [sb(f"tmpFA{i}", [128, 512], F32, ph) for i in range(2)]
                vst = sb("vstA", [128, 4, 768], BF16, ph)
                for k in range(8):
                    P.op("pool", "dma_start", dict(out=Win[:, k, :], in_=w_in_d[l, k * 128:(k + 1) * 128, :]),
                         w=[("Win", k)], dma=("w", k))
                P.op("dve", "memset", dict(ap=vst[:], constant=1.0), w=[("vst", j) for j in range(4)])
                rot = {"ps": 0, "ob": 0, "tp": 0, "tf": 0, "ev": 0}

                def nxt(name, n):
                    v = rot[name]
                    rot[name] = (v + 1) % n
                    return v

                WinK = [("Win", k) for k in range(8)]
                hTK = [("hT", k) for k in range(8)]
                for c in range(S // 512):
                    t0 = c * 512
                    P.op("sp", "dma_start", dict(
                        out=xt[:], in_=x_src[t0:t0 + 512, :].rearrange("(j p) d -> p j d", p=128)),
                        w=["xt"], dma="xt")
                    for j in range(4):
                        P.op("act", "activation", dict(out=hb[:, j, :], in_=xt[:, j, :], func=AF.Square,
                                                                 accum_out=ss[:, j:j + 1]),
                             r=["xt"], w=[("hb", j), ("ss", j)])
                    P.op("dve", "tensor_scalar", dict(out=rstd[:], in0=ss[:], scalar1=1.0 / D, scalar2=EPS,
                                                          op0=ALU.mult, op1=ALU.add),
                         r=[("ss", j) for j in range(4)], w=["rstd"])
                    P.op("act", "activation", dict(out=rstd[:], in_=rstd[:], func=AF.Sqrt), r=["rstd"], w=["rstd"])
                    P.op("dve", "reciprocal", dict(out=rstd[:], in_=rstd[:]), r=["rstd"], w=["rstd"])
                    for j in range(4):
                        P.op("dve", "tensor_scalar", dict(out=hb[:, j, :], in0=xt[:, j, :],
                                                                    scalar1=rstd[:, j:j + 1], scalar2=None, op0=ALU.mult),
                             r=["xt", "rstd"], w=[("hb", j)])
                    for k in range(8):
                        tb = 6 + nxt("tp", 2)
                        pT = psb[tb][:].bitcast(BF16)
                        for j in range(4):
                            P.op("pe", "transpose", dict(
                                out=pT[:, j * 128:(j + 1) * 128], in_=hb[:, j, k * 128:(k + 1) * 128], identity=identB[:]),
                                r=[("hb", j), "identB"], w=[("ps", tb)])
                        if k % 2 == 0:
                            P.op("dve", "tensor_scalar", dict(
                                out=hT[:, k, :], in0=pT[:, 0:512], scalar1=par[:, PB + P_MIXG + k:PB + P_MIXG + k + 1],
                                scalar2=None, op0=ALU.mult), r=[("ps", tb)], w=[("hT", k)])
                        else:
                            P.op("act", "activation", dict(
                                out=hT[:, k, :], in_=pT[:, 0:512], func=AF.Identity,
                                scale=par[:, PB + P_MIXG + k:PB + P_MIXG + k + 1]), r=[("ps", tb)], w=[("hT", k)])

                    def fm_group(col0, M=128):
                        b = nxt("ps", 6)
                        for k in range(8):
                            P.op("pe", "matmul", dict(out=psb[b][0:M, :], lhsT=Win[:, k, col0:col0 + M],
                                                                     rhs=hT[:, k, :], start=(k == 0), stop=(k == 7)),
                                 r=[("Win", k), ("hT", k)], w=[("ps", b)])
                        return b

                    def evac_store(b, dst, func=None, scale=None):
                        s = nxt("ob", NSL)
                        use_act = func is not None or (nxt("ev", 2) == 0)
                        if use_act:
                            f = func if func is not None else AF.Copy
                            kw = {} if scale is None else {"scale": scale}
                            P.op("act", "activation", dict(out=ob[s][:], in_=psb[b][:, :], func=f, **kw),
                                 r=[("ps", b)], w=[("ob", s)])
                        else:
                            sc = 1.0 if scale is None else scale
                            P.op("dve", "tensor_scalar", dict(out=ob[s][:], in0=psb[b][:, :], scalar1=sc, scalar2=None,
                                                                  op0=ALU.mult), r=[("ps", b)], w=[("ob", s)])
                        P.op("sp", "dma_start", dict(out=dst, in_=ob[s][:]), r=[("ob", s)], w=[], dma=("ob", s))

                    for pr in range(4):
                        b = fm_group(C_Q + pr * 128)
                        evac_store(b, d_qT[pr, :, t0:t0 + 512], scale=DH ** -0.5)
                    for pr in range(4):
                        b = fm_group(C_K + pr * 128)
                        evac_store(b, d_kT[pr, :, t0:t0 + 512])
                    for pr in range(4):
                        b = fm_group(C_QI + pr * 128)
                        evac_store(b, d_qiT[pr, :, t0:t0 + 512])
                    b = fm_group(C_KI, M=64)
                    P.op("dve", "tensor_copy", dict(out=kidxT[0:64, t0:t0 + 512], in_=psb[b][0:64, :]),
                         r=[("ps", b)], w=[("kidx", c, 0)])
                    P.op("sp", "dma_start", dict(out=kidxT[64:128, t0:t0 + 512], in_=kidxT[0:64, t0:t0 + 512]),
                         r=[("kidx", c, 0)], w=[("kidx", c, 1)], dma="kidx")
                    for cc in range(4):
                        ba = fm_group(C_UA + cc * 128)
                        bg = fm_group(C_UG + cc * 128)
                        tf = nxt("tf", 2)
                        s = nxt("ob", NSL)
                        P.op("act", "activation", dict(out=tmpF[tf][:], in_=psb[bg][:, :], func=AF.Sigmoid),
                             r=[("ps", bg)], w=[("tmpF", tf)])
                        P.op("dve", "tensor_tensor", dict(out=ob[s][:], in0=psb[ba][:, :], in1=tmpF[tf][:],
                                                                                 op=ALU.mult),
                             r=[("ps", ba), ("tmpF", tf)], w=[("ob", s)])
                        P.op("sp", "dma_start", dict(out=d_hg[cc, :, t0:t0 + 512], in_=ob[s][:]),
                             r=[("ob", s)], dma=("ob", s))
                    for oc in range(8):
                        b = fm_group(C_GA + oc * 128)
                        evac_store(b, d_sga[oc, :, t0:t0 + 512], func=AF.Sigmoid)
                    for oc in range(8):
                        b = fm_group(C_GB + oc * 128)
                        evac_store(b, d_sgb[oc, :, t0:t0 + 512], func=AF.Sigmoid)
                    for j in range(4):
                        b = nxt("ps", 6)
                        for k in range(8):
                            P.op("pe", "matmul", dict(out=psb[b][:, :], lhsT=hT[:, k, j * 128:(j + 1) * 128],
                                                                          rhs=Win[:, k, C_V:C_V + 512], start=(k == 0), stop=(k == 7)),
                                 r=[("Win", k), ("hT", k)], w=[("ps", b)])
                        pv = psb[b][:, :].rearrange("p (a two d) -> p a two d", a=4, two=2)
                        vv = vst[:, j, :].rearrange("p (a x) -> p a x", a=4)
                        P.op("dve", "tensor_copy", dict(out=vv[:, :, 0:64], in_=pv[:, :, 0, :]),
                             r=[("ps", b)], w=[("vst", j)])
                        P.op("act", "activation", dict(out=vv[:, :, 128:192], in_=pv[:, :, 1, :], func=AF.Copy),
                             r=[("ps", b)], w=[("vst", j)])
                        P.op("sp", "dma_start", dict(out=d_V[c * 4 + j, :, :], in_=vst[:, j, :]),
                             r=[("vst", j)], dma=("vst", j))
                    b = nxt("ps", 6)
                    for j in range(4):
                        for k in range(8):
                            P.op("pe", "matmul", dict(out=psb[b][:, j * 8:(j + 1) * 8],
                                                                          lhsT=hT[:, k, j * 128:(j + 1) * 128],
                                                                          rhs=Win[:, k, C_WI:C_WI + 8], start=(k == 0), stop=(k == 7)),
                                 r=[("Win", k), ("hT", k)], w=[("ps", b)])
                    wv = wi_all[:, c * 4:(c + 1) * 4, :]
                    P.op("dve", "tensor_copy", dict(out=wv, in_=psb[b][:, 0:32].rearrange("p (j h) -> p j h", j=4)),
                         r=[("ps", b)], w=[("wi", c)])
                    P.op("act", "activation", dict(out=absw[:, c * 4:(c + 1) * 4, :], in_=wv, func=AF.Abs,
                                                   scale=(64 ** -0.5) * (8 ** -0.5)),
                         r=[("wi", c)], w=[("absw", c)])
                    P.op("dve", "tensor_scalar", dict(out=sgnw[:, c * 4:(c + 1) * 4, :], in0=wv, scalar1=0.0,
                                                                scalar2=2.0, op0=ALU.is_gt, op1=ALU.mult),
                         r=[("wi", c)], w=[("sgnw", c)])
                    P.op("dve", "tensor_scalar", dict(out=sgnw[:, c * 4:(c + 1) * 4, :], in0=sgnw[:, c * 4:(c + 1) * 4, :],
                                                          scalar1=-1.0, scalar2=None, op0=ALU.add),
                         r=[("sgnw", c)], w=[("sgnw", c)])
                P.flush()
            if stop == ("A", l):
                return nc

            with ExitStack() as ph:
                score = sb("score", [128, S], F32, ph)
                junk = sb("junkB", [128, S], F32, ph)
                maskA = [sb(f"maskA{i}", [128, S], BF16, ph) for i in range(2)]
                qTc = [sb(f"qTc{i}", [128, 4, 256], BF16, ph) for i in range(2)]
                qiTc = [sb(f"qiTc{i}", [128, 4, 256], BF16, ph) for i in range(2)]
                NKB = 2
                kTg = [sb(f"kTg{i}", [128, 4, 512], BF16, ph) for i in range(NKB)]
                Vg = [sb(f"Vg{i}", [128, 4, 768], BF16, ph) for i in range(NKB)]
                NR = 3
                Rb = [sb(f"Rb{i}", [128, 512], F32, ph) for i in range(NR)]
                NPT = 3
                Pt = [sb(f"Pt{i}", [128, 4, 256], BF16, ph) for i in range(NPT)]
                lo = sb("loB", [128, 2], F32, ph)
                hi = sb("hiB", [128, 2], F32, ph)
                mid = sb("midB", [128, 2], F32, ph)
                Wd = sb("WdB", [128, NIT + 1], F32, ph)
                cnt = sb("cntB", [128, 2], F32, ph)
                stp = sb("stpB", [128, 2], F32, ph)
                lo1 = sb("lo1B", [128, 2], F32, ph)
                hi1 = sb("hi1B", [128, 2], F32, ph)
                ssA = sb("ssAB", [128, 2], F32, ph)
                zeros2 = sb("zeros2B", [128, 2], F32, ph)
                oacc = [sb(f"oaccB{i}", [128, 4, 512], F32, ph) for i in range(2)]
                denx = [sb(f"denxB{i}", [128, 4, 256], F32, ph) for i in range(2)]
                oTs = [sb(f"oTs{i}", [128, 4, 256], BF16, ph) for i in range(2)]
                oTo = [sb(f"oTo{i}", [128, 4, 256], BF16, ph) for i in range(2)]
                rot = {"idx": 0, "R": 0, "L": 0, "pt": 0, "mt": 0, "kg": 0}

                def nxt(name, n):
                    v = rot[name]
                    rot[name] = (v + 1) % n
                    return v

                P.op("dve", "memset", dict(ap=cnt[:], constant=0.0), w=["cnt"])
                P.op("dve", "memset", dict(ap=ssA[:], constant=0.0), w=["ssA"])
                P.op("dve", "memset", dict(ap=zeros2[:], constant=0.0), w=["zeros2"])
                mTb = [psb[6][:].bitcast(BF16), psb[7][:].bitcast(BF16)]
                Lbanks = [4, 5]
                for tc in range(S // 256):
                    T0 = tc * 256
                    qs = tc % 2
                    P.op("sp", "dma_start", dict(out=qTc[qs][:], in_=d_qT.rearrange("a p s -> p a s")[:, :, T0:T0 + 256]),
                         w=[("qTc", qs)], dma=("qTc", qs))
                    P.op("sp", "dma_start", dict(out=qiTc[qs][:], in_=d_qiT.rearrange("a p s -> p a s")[:, :, T0:T0 + 256]),
                         w=[("qiTc", qs)], dma=("qiTc", qs))
                    for qt in range(2):
                        Tq = T0 + 128 * qt
                        ti = Tq // 128
                        Sx = Tq + 128
                        ng = (Sx + 511) // 512
                        for g in range(ng):
                            wd = min(512, Sx - g * 512)
                            for h in range(NH):
                                b = nxt("idx", 4)
                                hp, pr = (h % 2) * 64, h // 2
                                P.op("pe", "matmul", dict(out=psb[b][:, 0:wd], lhsT=qiTc[qs][hp:hp + 64, pr, qt * 128:(qt + 1) * 128],
                                    rhs=kidxT[hp:hp + 64, g * 512:g * 512 + wd], start=True, stop=True),
                                    r=[("qiTc", qs)], w=[("ps", b)])
                                rs = nxt("R", NR)
                                P.op("act", "activation", dict(
                                    out=Rb[rs][:, 0:wd], in_=psb[b][:, 0:wd], func=AF.Relu, scale=absw[:, ti, h:h + 1]),
                                    r=[("ps", b)], w=[("R", rs)])
                                sc = score[:, g * 512:g * 512 + wd]
                                if h == 0:
                                    P.op("dve", "tensor_scalar", dict(
                                        out=sc, in0=Rb[rs][:, 0:wd], scalar1=sgnw[:, ti, 0:1], scalar2=None, op0=ALU.mult),
                                        r=[("R", rs)], w=[("score", g)])
                                else:
                                    P.op("dve", "scalar_tensor_tensor", dict(
                                        out=sc, in0=Rb[rs][:, 0:wd], scalar=sgnw[:, ti, h:h + 1], in1=sc, op0=ALU.mult, op1=ALU.add),
                                        r=[("R", rs), ("score", g)], w=[("score", g)])
                        SG = [("score", g) for g in range(ng)]
                        P.op("dve", "tensor_scalar", dict(out=junk[:, 0:Sx], in0=score[:, 0:Sx], scalar1=1.0, scalar2=None,
                                                          op0=ALU.mult, op1=ALU.min, accum_out=lo1[:, 0:1]),
                             r=SG, w=["junk", "junkA", "lo1"])
                        P.op("dve", "tensor_tensor", dict(out=score[:, Tq:Tq + 128], in0=score[:, Tq:Tq + 128], in1=tri[:],
                                                                     op=ALU.add), r=SG + ["tri"], w=SG)
                        P.op("dve", "tensor_scalar", dict(out=junk[:, 0:Sx], in0=score[:, 0:Sx], scalar1=1.0, scalar2=None,
                                                          op0=ALU.mult, op1=ALU.max, accum_out=hi1[:, 0:1]),
                             r=SG, w=["junk", "junkA", "hi1"])
                        P.op("dve", "tensor_scalar", dict(out=lo[:], in0=zeros2[:], scalar1=lo1[:, 0:1], scalar2=-1.0, op0=ALU.add, op1=ALU.add),
                             r=["lo1", "zeros2"], w=["lo"])
                        P.op("dve", "tensor_scalar", dict(out=hi[:], in0=zeros2[:], scalar1=hi1[:, 0:1], scalar2=lo[:, 0:1], op0=ALU.add, op1=ALU.subtract),
                             r=["hi1", "lo", "zeros2"], w=["hi"])
                        P.op("dve", "tensor_scalar", dict(out=Wd[:], in0=pow2[:], scalar1=hi[:, 0:1], scalar2=0.5,
                                                              op0=ALU.mult, op1=ALU.mult), r=["hi", "pow2"], w=["Wd"])
                        P.op("dve", "tensor_scalar", dict(out=mid[:], in0=lo[:], scalar1=Wd[:, 0:1], scalar2=None, op0=ALU.add),
                             r=["lo", "Wd"], w=["mid"])
                        hD = min(Sx, max(128, int(round(ACT_SPLIT * Sx / 128.0)) * 128))
                        nA = Sx - hD
                        for k in range(NIT):
                            P.op("dve", "tensor_scalar", dict(out=junk[:, 0:hD], in0=score[:, 0:hD], scalar1=mid[:, 0:1],
                                                              scalar2=None, op0=ALU.is_ge, op1=ALU.add, accum_out=cnt[:, 0:1]),
                                 r=SG + ["mid"], w=["junk", "cnt"])
                            if nA > 0:
                                P.op("act", "activation", dict(out=junk[:, hD:Sx], in_=score[:, hD:Sx], func=AF.Sign, scale=-1.0,
                                                               bias=mid[:, 0:1], accum_out=ssA[:, 0:1]),
                                     r=SG + ["mid"], w=["junkA", "ssA"])
                                P.op("dve", "scalar_tensor_tensor", dict(out=cnt[:], in0=ssA[:], scalar=-0.5, in1=cnt[:], op0=ALU.mult, op1=ALU.add),
                                     r=["ssA", "cnt"], w=["cnt"])
                            P.op("dve", "tensor_scalar", dict(out=stp[:], in0=cnt[:], scalar1=TOPK - 0.5 - nA / 2.0, scalar2=Wd[:, k:k + 1],
                                                                       op0=ALU.is_gt, op1=ALU.mult), r=["cnt", "Wd"], w=["stp"])
                            P.op("dve", "scalar_tensor_tensor", dict(out=mid[:], in0=mid[:], scalar=Wd[:, k + 1:k + 2], in1=stp[:],
                                                                              op0=ALU.subtract, op1=ALU.add),
                                 r=["mid", "stp", "Wd"], w=["mid"])
                        P.op("dve", "tensor_scalar", dict(out=mid[:], in0=mid[:], scalar1=Wd[:, NIT:NIT + 1], scalar2=None, op0=ALU.subtract),
                             r=["mid", "Wd"], w=["mid"])
                        P.op("dve", "tensor_scalar", dict(out=maskA[qt][:, 0:Sx], in0=score[:, 0:Sx], scalar1=mid[:, 0:1],
                                                                            scalar2=None, op0=ALU.is_ge), r=SG + ["mid"], w=[("maskA", qt)])
                        if qt == 0:
                            P.op("dve", "memset", dict(ap=maskA[0][:, Sx:Sx + 128], constant=0.0), w=[("maskA", 0)])
                    if P.limit is not None or True:
                        import os
                        if os.environ.get('KDBG'):
                            print('ATT start tc', tc, 'nops', P.nops, flush=True)
                    nj = (T0 + 256) // 128
                    units = [(j, pp) for j in range(nj) for pp in range(2)]
                    state = {}

                    def qk(i):
                        j, pp = units[i]
                        g, jj = j // 4, j % 4
                        if pp == 0 and jj == 0:
                            kb = nxt("kg", NKB)
                            state["kb"] = kb
                            nch = min(4, nj - g * 4)
                            P.op("sp", "dma_start", dict(
                                out=kTg[kb][:, :, 0:nch * 128], in_=d_kT.rearrange("a p s -> p a s")[:, :, g * 512:g * 512 + nch * 128]),
                                w=[("kTg", kb)], dma=("kTg", kb))
                            P.op("sp", "dma_start", dict(
                                out=Vg[kb][:, 0:nch, :], in_=d_V[g * 4:g * 4 + nch, :, :].rearrange("c p x -> p c x")),
                                w=[("Vg", kb)], dma=("Vg", kb))
                        kb = state["kb"]
                        if pp == 0:
                            ms = nxt("mt", 2)
                            state["ms"] = ms
                            for qt in range(2):
                                P.op("pe", "transpose", dict(
                                    out=mTb[ms][:, qt * 128:(qt + 1) * 128],
                                    in_=maskA[qt][:, j * 128:(j + 1) * 128], identity=identB[:]),
                                    r=[("maskA", qt), "identB"], w=[("ps", 6 + ms)])
                        ms = state["ms"]
                        off = j * 128 - T0
                        near = off in (-128, 0, 128)
                        for q in range(2):
                            pr = 2 * pp + q
                            for hh in range(2):
                                h = 2 * pr + hh
                                Lv = Lt[:, hh * 512 + q * 256: hh * 512 + (q + 1) * 256]
                                P.op("pe", "matmul", dict(out=Lv, lhsT=kTg[kb][hh * 64:(hh + 1) * 64, pr, jj * 128:(jj + 1) * 128],
                                    rhs=qTc[qs][hh * 64:(hh + 1) * 64, pr, :], start=True, stop=not near),
                                    r=[("kTg", kb), ("qTc", qs)], w=[("ps", "L")])
                                if near:
                                    oi = off // 128 + 1
                                    P.op("pe", "matmul", dict(out=Lv, lhsT=identB[:], rhs=biasadj[:, h, oi, :],
                                                                                     start=False, stop=True),
                                         r=[("biasadj", h), "identB"], w=[("ps", "L")])
                        ps_ = nxt("pt", NPT)
                        P.op("act", "activation", dict(out=Pt[ps_][:].rearrange("p a t -> p (a t)"), in_=Lt[:, :], func=AF.Exp),
                             r=[("ps", "L")], w=[("Pt", ps_)])
                        P.op("dve", "tensor_tensor", dict(
                            out=Pt[ps_][:], in0=Pt[ps_][:], in1=mTb[ms][:, 0:256].unsqueeze(1).to_broadcast([128, 4, 256]),
                            op=ALU.mult), r=[("Pt", ps_), ("ps", 6 + ms)], w=[("Pt", ps_)])
                        return (ps_, kb)

                    def pv(i, info):
                        ps_, kb = info
                        j, pp = units[i]
                        jj = j % 4
                        for q in range(2):
                            pr = 2 * pp + q
                            for hh in range(2):
                                P.op("pe", "matmul", dict(out=psb[pr][:, hh * 256:(hh + 1) * 256],
                                    lhsT=Vg[kb][:, jj, pr * 192 + hh * 64: pr * 192 + hh * 64 + 128],
                                    rhs=Pt[ps_][:, hh * 2 + q, :], start=(j == 0 and hh == 0), stop=(j == nj - 1), skip_group_check=True),
                                    r=[("Vg", kb), ("Pt", ps_)], w=[("ps", pr)])

                    infos = {}
                    DEPTH = 1
                    for i in range(len(units)):
                        infos[i] = qk(i)
                        if i >= DEPTH:
                            pv(i - DEPTH, infos.pop(i - DEPTH))
                    for i in range(max(0, len(units) - DEPTH), len(units)):
                        pv(i, infos.pop(i))
                    import os
                    if os.environ.get('KDBG'):
                        print('NORM start tc', tc, 'nops', P.nops, flush=True)
                    os_ = tc % 2
                    for pr in range(4):
                        eng = "act" if pr % 2 == 0 else "dve"
                        if eng == "act":
                            P.op("act", "activation", dict(out=oacc[os_][:, pr, :], in_=psb[pr][:, :], func=AF.Copy),
                                 r=[("ps", pr)], w=[("oacc", os_, pr)])
                        else:
                            P.op("dve", "tensor_copy", dict(out=oacc[os_][:, pr, :], in_=psb[pr][:, :]),
                                 r=[("ps", pr)], w=[("oacc", os_, pr)])
                    OA = [("oacc", os_, pr) for pr in range(4)]
                    P.op("sp", "dma_start", dict(out=denx[os_][0:64, :, :], in_=oacc[os_][64:128, :, 0:256]),
                         r=OA, w=[("denx", os_, 0)], dma=("denx", os_, 0))
                    P.op("sp", "dma_start", dict(out=denx[os_][64:128, :, :], in_=oacc[os_][0:64, :, 256:512]),
                         r=OA, w=[("denx", os_, 1)], dma=("denx", os_, 1))
                    P.op("dve", "reciprocal", dict(out=denx[os_][:], in_=denx[os_][:]),
                         r=[("denx", os_, 0), ("denx", os_, 1)], w=[("denx", os_, 0), ("denx", os_, 1)])
                    P.op("dve", "tensor_tensor", dict(out=oTs[os_][:], in0=oacc[os_][:, :, 0:256], in1=denx[os_][:], op=ALU.mult),
                         r=OA + [("denx", os_, 0), ("denx", os_, 1)], w=[("oTs", os_, 0)])
                    P.op("dve", "tensor_tensor", dict(out=oTo[os_][:], in0=oacc[os_][:, :, 256:512], in1=denx[os_][:], op=ALU.mult),
                         r=OA + [("denx", os_, 0), ("denx", os_, 1)], w=[("oTs", os_, 1)])
                    dview = d_oT.rearrange("a p s -> p a s")
                    P.op("sp", "dma_start", dict(out=dview[0:64, :, T0:T0 + 256], in_=oTs[os_][0:64, :, :]),
                         r=[("oTs", os_, 0)], dma=("oTs", os_, 0))
                    P.op("sp", "dma_start", dict(out=dview[64:128, :, T0:T0 + 256], in_=oTo[os_][64:128, :, :]),
                         r=[("oTs", os_, 1)], dma=("oTs", os_, 1))
                P.flush()
            if stop == ("B", l):
                return nc

            with ExitStack() as ph:
                Wao = sb("Wao", [128, 4, D], BF16, ph)
                Wco = sb("Wco", [128, 4, D], BF16, ph)
                Wmo = sb("Wmo", [128, 8, D], BF16, ph)
                Dg = sb("Dg", [128, 4, CW, 128], BF16, ph)
                xt = sb("xtC", [128, 4, D], F32, ph)
                oTc = sb("oTc", [128, 4, 512], BF16, ph)
                hg = sb("hgC", [128, 4, 512 + CW - 1], BF16, ph)
                sga = sb("sgaC", [128, 8, 512], BF16, ph)
                sgb = sb("sgbC", [128, 8, 512], BF16, ph)
                conv = sb("convC", [128, 4, 512], F32, ph)
                sq = sb("sqC", [128, 4, 512], F32, ph)
                mean_s = sb("meanC", [128, 512], F32, ph)
                var_s = sb("varC", [128, 512], F32, ph)
                ytmp = [sb(f"ytmpC{i}", [128, 512], F32, ph) for i in range(2)]
                ycT = sb("ycT", [128, 4, 512], BF16, ph)
                merged = sb("mergedC", [128, 8, 512], BF16, ph)
                t1 = [sb(f"t1C{i}", [128, 512], F32, ph) for i in range(2)]
                t2 = [sb(f"t2C{i}", [128, 512], F32, ph) for i in range(2)]
                P.op("pool", "dma_start", dict(out=Wao[:], in_=w_ao_d[l].rearrange("(k p) n -> p k n", p=128)), w=["Wao"], dma=("w", 0))
                P.op("pool", "dma_start", dict(out=Wco[:], in_=w_co_d[l].rearrange("(k p) n -> p k n", p=128)), w=["Wco"], dma=("w", 1))
                P.op("pool", "dma_start", dict(out=Wmo[:], in_=w_mo_d[l].rearrange("(k p) n -> p k n", p=128)), w=["Wmo"], dma=("w", 2))
                for cc in range(4):
                    for k in range(CW):
                        col = PB + P_DW + cc * CW + k
                        P.op("dve", "tensor_scalar", dict(out=Dg[:, cc, k, :], in0=identF[:], scalar1=par[:, col:col + 1],
                                                                                  scalar2=None, op0=ALU.mult), w=[("Dg", cc)])
                rot = {"ps": 0, "y": 0, "t": 0}

                def nxt(name, n):
                    v = rot[name]
                    rot[name] = (v + 1) % n
                    return v

                HW_ = CW - 1
                for c in range(S // 512):
                    t0 = c * 512
                    P.op("sp", "dma_start", dict(out=xt[:], in_=x_src[t0:t0 + 512, :].rearrange("(j p) d -> p j d", p=128)),
                         w=["xt"], dma="xt")
                    P.op("sp", "dma_start", dict(out=oTc[:], in_=d_oT.rearrange("a p s -> p a s")[:, :, t0:t0 + 512]),
                         w=["oTc"], dma="oTc")
                    if c == 0:
                        P.op("dve", "memset", dict(ap=hg[:, :, 0:HW_], constant=0.0), w=["hg"])
                        P.op("sp", "dma_start", dict(out=hg[:, :, HW_:HW_ + 512], in_=d_hg.rearrange("a p s -> p a s")[:, :, 0:512]),
                             w=["hg"], dma="hg")
                    else:
                        P.op("sp", "dma_start", dict(out=hg[:], in_=d_hg.rearrange("a p s -> p a s")[:, :, t0 - HW_:t0 + 512]),
                             w=["hg"], dma="hg")
                    P.op("sp", "dma_start", dict(out=sga[:], in_=d_sga.rearrange("a p s -> p a s")[:, :, t0:t0 + 512]),
                         w=["sga"], dma="sga")
                    P.op("sp", "dma_start", dict(out=sgb[:], in_=d_sgb.rearrange("a p s -> p a s")[:, :, t0:t0 + 512]),
                         w=["sgb"], dma="sgb")
                    for cc in range(4):
                        b = nxt("ps", 4)
                        for k in range(CW):
                            P.op("pe", "matmul", dict(out=psb[b][:, :], lhsT=Dg[:, cc, k, :], rhs=hg[:, cc, k:k + 512],
                                                                           start=(k == 0), stop=(k == CW - 1)),
                                 r=[("Dg", cc), "hg"], w=[("ps", b)])
                        P.op("dve", "tensor_scalar", dict(out=conv[:, cc, :], in0=psb[b][:, :],
                                                                          scalar1=par[:, PB + P_DWB + cc:PB + P_DWB + cc + 1], scalar2=None,
                                                                          op0=ALU.add), r=[("ps", b)], w=[("conv", cc)])
                        P.op("act", "activation", dict(out=sq[:, cc, :], in_=conv[:, cc, :], func=AF.Square),
                             r=[("conv", cc)], w=[("sq", cc)])
                    bm, bq = 4, 5
                    for cc in range(4):
                        P.op("pe", "matmul", dict(out=psb[bm][:, :], lhsT=onesF[:], rhs=conv[:, cc, :], start=(cc == 0), stop=(cc == 3)),
                             r=[("conv", cc), "onesF"], w=[("ps", bm)])
                    for cc in range(4):
                        P.op("pe", "matmul", dict(out=psb[bq][:, :], lhsT=onesF[:], rhs=sq[:, cc, :], start=(cc == 0), stop=(cc == 3)),
                             r=[("sq", cc), "onesF"], w=[("ps", bq)])
                    P.op("act", "activation", dict(out=mean_s[:], in_=psb[bm][:, :], func=AF.Copy), r=[("ps", bm)], w=["mean"])
                    P.op("dve", "tensor_tensor", dict(out=var_s[:], in0=mean_s[:], in1=mean_s[:], op=ALU.mult), r=["mean"], w=["var"])
                    P.op("dve", "tensor_tensor", dict(out=var_s[:], in0=psb[bq][:, :], in1=var_s[:], op=ALU.subtract),
                         r=[("ps", bq), "var"], w=["var"])
                    P.op("dve", "tensor_scalar", dict(out=var_s[:], in0=var_s[:], scalar1=0.0, scalar2=EPS, op0=ALU.max, op1=ALU.add),
                         r=["var"], w=["var"])
                    P.op("act", "activation", dict(out=var_s[:], in_=var_s[:], func=AF.Sqrt), r=["var"], w=["var"])
                    P.op("dve", "reciprocal", dict(out=var_s[:], in_=var_s[:]), r=["var"], w=["var"])
                    for cc in range(4):
                        y = nxt("y", 2)
                        P.op("dve", "tensor_tensor", dict(out=ytmp[y][:], in0=conv[:, cc, :], in1=mean_s[:], op=ALU.subtract),
                             r=[("conv", cc), "mean"], w=[("ytmp", y)])
                        P.op("pool", "tensor_tensor", dict(out=ytmp[y][:], in0=ytmp[y][:], in1=var_s[:], op=ALU.mult),
                             r=[("ytmp", y), "var"], w=[("ytmp", y)])
                        P.op("act", "activation", dict(out=ycT[:, cc, :], in_=ytmp[y][:], func=AF.Silu,
                                                                       scale=par[:, PB + P_CNG + cc:PB + P_CNG + cc + 1],
                                                                       bias=par[:, PB + P_CNB + cc:PB + P_CNB + cc + 1]),
                             r=[("ytmp", y)], w=[("ycT", cc)])
                    for oc in range(8):
                        ba = nxt("ps", 4)
                        for pr in range(4):
                            P.op("pe", "matmul", dict(out=psb[ba][:, :], lhsT=Wao[:, pr, oc * 128:(oc + 1) * 128],
                                                                               rhs=oTc[:, pr, :], start=(pr == 0), stop=(pr == 3)),
                                 r=["Wao", "oTc"], w=[("ps", ba)])
                        bb = nxt("ps", 4)
                        for cc in range(4):
                            P.op("pe", "matmul", dict(out=psb[bb][:, :], lhsT=Wco[:, cc, oc * 128:(oc + 1) * 128],
                                                                               rhs=ycT[:, cc, :], start=(cc == 0), stop=(cc == 3)),
                                 r=["Wco", ("ycT", cc)], w=[("ps", bb)])
                        ts = nxt("t", 2)
                        P.op("dve", "tensor_tensor", dict(out=t1[ts][:], in0=psb[ba][:, :], in1=sga[:, oc, :], op=ALU.mult),
                             r=[("ps", ba), "sga"], w=[("t1", ts)])
                        P.op("dve", "tensor_tensor", dict(out=t2[ts][:], in0=psb[bb][:, :], in1=sgb[:, oc, :], op=ALU.mult),
                             r=[("ps", bb), "sgb"], w=[("t2", ts)])
                        P.op("pool", "tensor_tensor", dict(out=merged[:, oc, :], in0=t1[ts][:], in1=t2[ts][:], op=ALU.add),
                             r=[("t1", ts), ("t2", ts)], w=[("merged", oc)])
                    for j in range(4):
                        for n in range(2):
                            b = nxt("ps", 4)
                            for k in range(8):
                                P.op("pe", "matmul", dict(out=psb[b][:, :], lhsT=merged[:, k, j * 128:(j + 1) * 128],
                                                                                 rhs=Wmo[:, k, n * 512:(n + 1) * 512], start=(k == 0), stop=(k == 7)),
                                     r=[("merged", k), "Wmo"], w=[("ps", b)])
                            P.op("dve", "tensor_tensor", dict(out=xt[:, j, n * 512:(n + 1) * 512], in0=psb[b][:, :],
                                                                                in1=xt[:, j, n * 512:(n + 1) * 512], op=ALU.add),
                                 r=[("ps", b), "xt"], w=["xt"])
                    P.op("sp", "dma_start", dict(out=d_xa[t0:t0 + 512, :].rearrange("(j p) d -> p j d", p=128), in_=xt[:]),
                         r=["xt"], dma="xo")
                P.flush()
            if stop == ("C1", l):
                return nc

            last = (l == NL - 1)
            with ExitStack() as ph:
                Wfi = sb("Wfi", [128, 8, 2 * DFF], BF16, ph)
                Wfo = sb("Wfo", [128, 22, D], BF16, ph)
                xt2 = [sb(f"xtF{i}", [128, 2, D], F32, ph) for i in range(1)]
                hb = sb("hbF", [128, 2, D], BF16, ph)
                hT = sb("hTF", [128, 8, 256], BF16, ph)
                ss = sb("ssF", [128, 2], F32, ph)
                rstd = sb("rstdF", [128, 2], F32, ph)
                actT = sb("actT", [128, 22, 256], BF16, ph)
                tmpF = [sb(f"tmpFF{i}", [128, 256], F32, ph) for i in range(2)]
                if last:
                    gfin = sb("gfin", [128, D], F32, ph)
                    P.op("sp", "dma_start", dict(out=gfin[:], in_=gfin_d[:, :]), w=["gfin"], dma="gfin")
                for k in range(8):
                    P.op("pool", "dma_start", dict(out=Wfi[:, k, :], in_=w_fi_d[l, k * 128:(k + 1) * 128, :]),
                         w=[("Wfi", k)], dma=("w", k))
                for k in range(22):
                    P.op("pool", "dma_start", dict(out=Wfo[:, k, :], in_=w_fo_d[l, k * 128:(k + 1) * 128, :]),
                         w=[("Wfo", k)], dma=("w", 8 + k))
                rot = {"ps": 0, "tp": 0, "tf": 0}
                dst = out_d if last else d_xb
                for c in range(S // 256):
                    t0 = c * 256
                    xs = 0
                    xt = xt2[xs]
                    P.op("sp", "dma_start", dict(out=xt[:], in_=d_xa[t0:t0 + 256, :].rearrange("(j p) d -> p j d", p=128)),
                         w=[("xt", xs)], dma=("xt", xs))
                    for j in range(2):
                        P.op("act", "activation", dict(out=hb[:, j, :], in_=xt[:, j, :], func=AF.Square, accum_out=ss[:, j:j + 1]),
                             r=[("xt", xs)], w=[("hb", j), ("ss", j)])
                    P.op("dve", "tensor_scalar", dict(out=rstd[:], in0=ss[:], scalar1=1.0 / D, scalar2=EPS, op0=ALU.mult, op1=ALU.add),
                         r=[("ss", 0), ("ss", 1)], w=["rstd"])
                    P.op("act", "activation", dict(out=rstd[:], in_=rstd[:], func=AF.Sqrt), r=["rstd"], w=["rstd"])
                    P.op("dve", "reciprocal", dict(out=rstd[:], in_=rstd[:]), r=["rstd"], w=["rstd"])
                    for j in range(2):
                        P.op("dve", "tensor_scalar", dict(out=hb[:, j, :], in0=xt[:, j, :], scalar1=rstd[:, j:j + 1], scalar2=None,
                                                                         op0=ALU.mult), r=[("xt", xs), "rstd"], w=[("hb", j)])
                    for k in range(8):
                        tb = 6 + (rot["tp"] % 2)
                        rot["tp"] += 1
                        pT = psb[tb][:].bitcast(BF16)
                        for j in range(2):
                            P.op("pe", "transpose", dict(out=pT[:, j * 128:(j + 1) * 128], in_=hb[:, j, k * 128:(k + 1) * 128],
                                                                              identity=identB[:]), r=[("hb", j), "identB"], w=[("ps", tb)])
                        col = PB + P_FFNG + k
                        if k % 2 == 0:
                            P.op("dve", "tensor_scalar", dict(out=hT[:, k, :], in0=pT[:, 0:256], scalar1=par[:, col:col + 1],
                                                                                      scalar2=None, op0=ALU.mult), r=[("ps", tb)], w=[("hT", k)])
                        else:
                            P.op("act", "activation", dict(out=hT[:, k, :], in_=pT[:, 0:256], func=AF.Identity,
                                                                                   scale=par[:, col:col + 1]), r=[("ps", tb)], w=[("hT", k)])
                    for fc in range(22):
                        b = rot["ps"] % 4
                        rot["ps"] += 1
                        for half, c0 in ((0, fc * 128), (1, DFF + fc * 128)):
                            for k in range(8):
                                P.op("pe", "matmul", dict(out=psb[b][:, half * 256:(half + 1) * 256],
                                                                                         lhsT=Wfi[:, k, c0:c0 + 128], rhs=hT[:, k, :],
                                                                                         start=(k == 0), stop=(k == 7)),
                                     r=[("Wfi", k), ("hT", k)], w=[("ps", b)])
                        tf = rot["tf"] % 2
                        rot["tf"] += 1
                        P.op("act", "activation", dict(out=tmpF[tf][:], in_=psb[b][:, 0:256], func=AF.Silu),
                             r=[("ps", b)], w=[("tmpF", tf)])
                        P.op("dve", "tensor_tensor", dict(out=actT[:, fc, :], in0=psb[b][:, 256:512], in1=tmpF[tf][:], op=ALU.mult),
                             r=[("ps", b), ("tmpF", tf)], w=[("actT", fc)])
                    for j in range(2):
                        for n in range(2):
                            b = 4 + (rot["ps"] % 2)
                            rot["ps"] += 1
                            for fc in range(22):
                                P.op("pe", "matmul", dict(out=psb[b][:, :], lhsT=actT[:, fc, j * 128:(j + 1) * 128],
                                                                                   rhs=Wfo[:, fc, n * 512:(n + 1) * 512], start=(fc == 0), stop=(fc == 21)),
                                     r=[("actT", fc), ("Wfo", fc)], w=[("ps", b)])
                            P.op("dve", "tensor_tensor", dict(out=xt[:, j, n * 512:(n + 1) * 512], in0=psb[b][:, :],
                                                                                       in1=xt[:, j, n * 512:(n + 1) * 512], op=ALU.add),
                                 r=[("ps", b), ("xt", xs)], w=[("xt", xs)])
                    if last:
                        for j in range(2):
                            P.op("act", "activation", dict(out=hb[:, j, :], in_=xt[:, j, :], func=AF.Square, accum_out=ss[:, j:j + 1]),
                                 r=[("xt", xs)], w=[("hb", j), ("ss", j)])
                        P.op("dve", "tensor_scalar", dict(out=rstd[:], in0=ss[:], scalar1=1.0 / D, scalar2=EPS, op0=ALU.mult, op1=ALU.add),
                             r=[("ss", 0), ("ss", 1)], w=["rstd"])
                        P.op("act", "activation", dict(out=rstd[:], in_=rstd[:], func=AF.Sqrt), r=["rstd"], w=["rstd"])
                        P.op("dve", "reciprocal", dict(out=rstd[:], in_=rstd[:]), r=["rstd"], w=["rstd"])
                        for j in range(2):
                            P.op("dve", "scalar_tensor_tensor", dict(out=xt[:, j, :], in0=xt[:, j, :], scalar=rstd[:, j:j + 1], in1=gfin[:],
                                                                                    op0=ALU.mult, op1=ALU.mult),
                                 r=[("xt", xs), "rstd", "gfin"], w=[("xt", xs)])
                        P.op("sp", "dma_start", dict(out=dst[t0:t0 + 256, :].rearrange("(j p) d -> p j d", p=128), in_=xt[:]),
                             r=[("xt", xs)], dma=("xo", xs))
                    else:
                        P.op("sp", "dma_start", dict(out=dst[t0:t0 + 256, :].rearrange("(j p) d -> p j d", p=128), in_=xt[:]),
                             r=[("xt", xs)], dma=("xo", xs))
                P.flush()
            x_src = d_xb
    return nc


def t5_bucket_np(dist):
    n = np.maximum(dist, 0)
    nf = np.maximum(n, 1).astype(np.float32)
    large = 16 + (np.log(nf / np.float32(16)) / np.float32(math.log(128 / 16)) * np.float32(16)).astype(np.int32)
    large = np.minimum(large, 31)
    return np.where(n < 16, n, large)


def host_inputs(inputs, S, NL):
    f = lambda a: np.ascontiguousarray(np.asarray(a, dtype=np.float32))
    par = np.zeros((128, NL * P_LSZ + 8), np.float32)
    for l in range(NL):
        b = l * P_LSZ
        par[:, b + P_MIXG:b + P_MIXG + 8] = f(inputs["mix_norm"][l]).reshape(8, 128).T
        par[:, b + P_FFNG:b + P_FFNG + 8] = f(inputs["ffn_norm"][l]).reshape(8, 128).T
        dw = f(inputs["dw_kernel"][l])
        par[:, b + P_DW:b + P_DW + 4 * CW] = dw.T.reshape(4, 128, CW).transpose(1, 0, 2).reshape(128, 4 * CW)
        par[:, b + P_DWB:b + P_DWB + 4] = f(inputs["dw_bias"][l]).reshape(4, 128).T
        par[:, b + P_CNG:b + P_CNG + 4] = f(inputs["conv_norm_g"][l]).reshape(4, 128).T
        par[:, b + P_CNB:b + P_CNB + 4] = f(inputs["conv_norm_b"][l]).reshape(4, 128).T
    rb = f(inputs["rel_bias"])
    par[:, NL * P_LSZ:NL * P_LSZ + 8] = np.broadcast_to(rb[31:32, :], (128, 8))
    s_l = np.arange(128)[:, None]
    t_l = np.arange(256)[None, :]
    biasT = np.zeros((128, NH, 3, 256), np.float32)
    for oi, off in enumerate((-128, 0, 128)):
        bk = t5_bucket_np(t_l - s_l - off)
        biasT[:, :, oi, :] = rb[bk].transpose(0, 2, 1)
    gfin = np.ascontiguousarray(np.broadcast_to(f(inputs["final_norm"])[None, :], (128, D)))
    ident = np.eye(128, dtype=np.float32)
    tri = np.where(np.arange(128)[None, :] <= np.arange(128)[:, None], 0.0, -1e30).astype(np.float32)
    pow2 = np.broadcast_to((2.0 ** -np.arange(NIT + 1)).astype(np.float32)[None, :], (128, NIT + 1)).copy()
    shared = {
        "w_in": f(inputs["w_in"]), "w_attn_out": f(inputs["w_attn_out"]), "w_conv_out": f(inputs["w_conv_out"]),
        "w_mix_out": f(inputs["w_mix_out"]), "w_ffn_in": f(inputs["w_ffn_in"]), "w_ffn_out": f(inputs["w_ffn_out"]),
        "params": par, "gfin": gfin, "biasT": biasT, "ident": ident, "tri": tri, "pow2": pow2,
    }
    return shared


_NC_CACHE = {}


def kernel(**inputs):
    x = np.asarray(inputs["x"], dtype=np.float32)
    B, S, _ = x.shape
    NL = int(np.asarray(inputs["w_in"]).shape[0])
    key = (S, NL)
    if key not in _NC_CACHE:
        _NC_CACHE[key] = build_program(S, NL)
    nc = _NC_CACHE[key]
    shared = host_inputs(inputs, S, NL)
    in_maps = []
    for b in range(B):
        m = dict(shared)
        m["x"] = np.ascontiguousarray(x[b])
        in_maps.append(m)
    res = run_bass_kernel_spmd(nc, in_maps, core_ids=list(range(B)))
    out = np.stack([np.asarray(r["out"], dtype=np.float32) for r in res.results], axis=0)
    return out
```

```python
import math
from contextlib import ExitStack

import numpy as np
import concourse.bass as bass
import concourse.mybir as mybir
from concourse.bass_utils import run_bass_kernel_spmd

F32 = mybir.dt.float32
BF16 = mybir.dt.bfloat16
AF = mybir.ActivationFunctionType
ALU = mybir.AluOpType
AX = mybir.AxisListType

D = 1024
NH = 8
DH = 64
AW = 512
CC = 512
CW = 31
DFF = 2816
NIN = 5192
TOPK = 256
EPS = 1e-6
NIT = 20
NIT_TIGHT = 16
SAME_ENGINE_SYNC = True
WIDE_DUP = False
ACT_SPLIT = 0.42

C_Q, C_K, C_V, C_QI, C_KI, C_WI, C_UA, C_UG, C_GA, C_GB = 0, 512, 1024, 1536, 2048, 2112, 2120, 2632, 3144, 4168

P_MIXG, P_FFNG, P_DW, P_DWB, P_CNG, P_CNB = 0, 8, 16, 140, 144, 148
P_LSZ = 152


class Prog:
    ENG = ("sp", "act", "dve", "pool", "pe")

    def __init__(self, nc):
        self.nc = nc
        self.eng_sem = {e: [nc.alloc_semaphore(name=f"es_{e}"), 0] for e in ("act", "dve", "pool", "pe")}
        self.dma = {}
        self.buf = {}
        self.q = {e: [] for e in self.ENG}
        self.waited = {e: {} for e in self.ENG}
        self.nblk = 0

    def _dma(self, key):
        d = self.dma.get(key)
        if d is None:
            d = [self.nc.alloc_semaphore(name=f"ds_{len(self.dma)}"), 0]
            self.dma[key] = d
        return d

    limit = None
    nops = 0

    def op(self, eng, name, kw, r=(), w=(), dma=None):
        self.nops += 1
        if self.limit is not None and self.nops > self.limit:
            return
        fn = (name, kw)
        deps = {}

        def add(t):
            if t is None:
                return
            k = id(t[0])
            if k not in deps or deps[k][1] < t[1]:
                deps[k] = t

        for b in r:
            st = self.buf.get(b)
            if st:
                add(st[0])
        for b in w:
            st = self.buf.get(b)
            if st:
                add(st[0])
                for t in st[1].values():
                    add(t)
        if dma is None:
            es = self.eng_sem[eng]
            es[1] += 1
            ticket = (es[0], es[1], eng)
            inc = 1
        else:
            d = self._dma(dma)
            d[1] += 16
            ticket = (d[0], d[1], None)
            inc = 16
        waits = []
        for k, (sem, val, src) in deps.items():
            if src == eng and (eng == "pe" or not SAME_ENGINE_SYNC):
                continue
            if self.waited[eng].get(k, 0) >= val:
                continue
            self.waited[eng][k] = val
            waits.append((sem, val))
        self.q[eng].append((waits, fn, ticket[0], inc))
        for b in r:
            st = self.buf.setdefault(b, [None, {}])
            st[1][id(ticket[0])] = ticket
        for b in w:
            self.buf[b] = [ticket, {}]

    def flush(self):
        nc = self.nc
        waits = []
        for d in self.dma.values():
            if d[1] > 0 and self.waited["sp"].get(id(d[0]), 0) < d[1]:
                self.waited["sp"][id(d[0])] = d[1]
                waits.append((d[0], d[1]))
        self.q["sp"].append((waits, None, None, 0))
        self.nblk += 1
        import os
        if os.environ.get('KDBG'):
            print('FLUSH', self.nblk, 'nops', self.nops, flush=True)
        with nc.Block() as block:
            for e, deco in (("sp", block.sync), ("act", block.scalar), ("dve", block.vector),
                            ("pool", block.gpsimd), ("pe", block.tensor)):
                items = self.q[e]

                def body(engine, items=items):
                    for waits, fn, sem, inc in items:
                        for (s, v) in waits:
                            engine.wait_ge(s, v)
                        if fn is not None:
                            getattr(engine, fn[0])(**fn[1]).then_inc(sem, inc)

                deco(body)
        self.q = {e: [] for e in self.ENG}
        self.buf = {}


def build_program(S, NL, stop=None, debug=False, limit=None):
    assert S % 512 == 0
    NT = S // 128
    nc = bass.Bass("TRN2", target_bir_lowering=False)
    dt_in = lambda name, shape: nc.dram_tensor(name, list(shape), F32, kind="ExternalInput").ap()
    x_in = dt_in("x", [S, D])
    w_in_d = dt_in("w_in", [NL, D, NIN])
    w_ao_d = dt_in("w_attn_out", [NL, AW, D])
    w_co_d = dt_in("w_conv_out", [NL, CC, D])
    w_mo_d = dt_in("w_mix_out", [NL, D, D])
    w_fi_d = dt_in("w_ffn_in", [NL, D, 2 * DFF])
    w_fo_d = dt_in("w_ffn_out", [NL, DFF, D])
    NPAR = NL * P_LSZ + 8
    par_d = dt_in("params", [128, NPAR])
    gfin_d = dt_in("gfin", [128, D])
    bias_d = dt_in("biasT", [128, NH, 3, 256])
    ident_d = dt_in("ident", [128, 128])
    tri_d = dt_in("tri", [128, 128])
    pow2_d = dt_in("pow2", [128, NIT + 1])
    out_d = nc.dram_tensor("out", [S, D], F32, kind="ExternalOutput").ap()

    scr = lambda name, shape, dt: nc.dram_tensor(name, list(shape), dt, kind=("ExternalOutput" if debug else "Internal")).ap()
    d_xa = scr("d_xa", [S, D], F32)
    d_xb = scr("d_xb", [S, D], F32)
    d_qT = scr("d_qT", [4, 128, S], BF16)
    d_kT = scr("d_kT", [4, 128, S], BF16)
    d_qiT = scr("d_qiT", [4, 128, S], BF16)
    d_V = scr("d_V", [NT, 128, 768], BF16)
    d_hg = scr("d_hg", [4, 128, S], BF16)
    d_sga = scr("d_sga", [8, 128, S], BF16)
    d_sgb = scr("d_sgb", [8, 128, S], BF16)
    d_oT = scr("d_oT", [4, 128, S], BF16)

    P = Prog(nc)
    P.limit = limit
    with ExitStack() as top:
        uid = [0]

        def sb(name, shape, dt, stack=top):
            uid[0] += 1
            return stack.enter_context(nc.sbuf_tensor(f"s{uid[0]}_{name}", list(shape), dt))

        _pa = [top.enter_context(nc.psum_tensor(f"psb{i}", [128, 512], F32)) for i in range(4)]
        Lt = top.enter_context(nc.psum_tensor("psL", [128, 1024], F32))
        _pb = [top.enter_context(nc.psum_tensor(f"psb{i}", [128, 512], F32)) for i in (6, 7)]
        psb = [t[:] for t in _pa] + [Lt[:, 0:512], Lt[:, 512:1024]] + [t[:] for t in _pb]
        par = sb("par", [128, NPAR], F32)
        identF = sb("identF", [128, 128], F32)
        identB = sb("identB", [128, 128], BF16)
        tri = sb("tri", [128, 128], F32)
        pow2 = sb("pow2", [128, NIT + 1], F32)
        onesF = sb("onesF", [128, 128], F32)
        biasadj = sb("biasadj", [128, NH, 3, 256], BF16)
        kidxT = sb("kidxT", [128, S], BF16)
        wi_all = sb("wi_all", [128, NT, 8], F32)
        absw = sb("absw", [128, NT, 8], F32)
        sgnw = sb("sgnw", [128, NT, 8], F32)
        B31 = NL * P_LSZ

        with ExitStack() as ph:
            bstg = sb("bstg", [128, NH, 3, 256], F32, ph)
            P.op("sp", "dma_start", dict(out=par[:], in_=par_d[:, :]), w=["par"], dma="l0")
            P.op("sp", "dma_start", dict(out=identF[:], in_=ident_d[:, :]), w=["identF"], dma="l1")
            P.op("pool", "dma_start", dict(out=identB[:], in_=ident_d[:, :]), w=["identB"], dma="l2")
            P.op("sp", "dma_start", dict(out=tri[:], in_=tri_d[:, :]), w=["tri"], dma="l3")
            P.op("sp", "dma_start", dict(out=pow2[:], in_=pow2_d[:, :]), w=["pow2"], dma="l4")
            P.op("sp", "dma_start", dict(out=bstg[:], in_=bias_d[:]), w=["bstg"], dma="l5")
            P.op("dve", "memset", dict(ap=onesF[:], constant=1.0 / CC), w=["onesF"])
            for h in range(NH):
                P.op("dve", "tensor_scalar", dict(out=biasadj[:, h, :, :], in0=bstg[:, h, :, :],
                                                            scalar1=par[:, B31 + h:B31 + h + 1], scalar2=None,
                                                            op0=ALU.subtract),
                     r=["bstg", "par"], w=[("biasadj", h)])
            P.flush()
        if stop == "setup":
            return nc

        x_src = x_in
        for l in range(NL):
            PB = l * P_LSZ
            with ExitStack() as ph:
                Win = sb("Win", [128, 8, NIN], BF16, ph)
                xt = sb("xtA", [128, 4, D], F32, ph)
                hb = sb("hbA", [128, 4, D], BF16, ph)
                hT = sb("hTA", [128, 8, 512], BF16, ph)
                ss = sb("ssA", [128, 4], F32, ph)
                rstd = sb("rstdA", [128, 4], F32, ph)
                NSL = 8
                ob = [sb(f"obA{i}", [128, 512], BF16, ph) for i in range(NSL)]
                tmpF = [sb(f"tmpFA{i}", [128, 512], F32, ph) for i in range(2)]
                vst = sb("vstA", [128, 4, 768], BF16, ph)
                for k in range(8):
                    P.op("pool", "dma_start", dict(out=Win[:, k, :], in_=w_in_d[l, k * 128:(k + 1) * 128, :]),
                         w=[("Win", k)], dma=("w", k))
                P.op("dve", "memset", dict(ap=vst[:], constant=1.0), w=[("vst", j) for j in range(4)])
                rot = {"ps": 0, "ob": 0, "tp": 0, "tf": 0, "ev": 0}

                def nxt(name, n):
                    v = rot[name]
                    rot[name] = (v + 1) % n
                    return v

                WinK = [("Win", k) for k in range(8)]
                hTK = [("hT", k) for k in range(8)]
                for c in range(S // 512):
                    t0 = c * 512
                    P.op("sp", "dma_start", dict(
                        out=xt[:], in_=x_src[t0:t0 + 512, :].rearrange("(j p) d -> p j d", p=128)),
                        w=["xt"], dma="xt")
                    for j in range(4):
                        P.op("act", "activation", dict(out=hb[:, j, :], in_=xt[:, j, :], func=AF.Square,
                                                                 accum_out=ss[:, j:j + 1]),
                             r=["xt"], w=[("hb", j), ("ss", j)])
                    P.op("dve", "tensor_scalar", dict(out=rstd[:], in0=ss[:], scalar1=1.0 / D, scalar2=EPS,
                                                          op0=ALU.mult, op1=ALU.add),
                         r=[("ss", j) for j in range(4)], w=["rstd"])
                    P.op("act", "activation", dict(out=rstd[:], in_=rstd[:], func=AF.Sqrt), r=["rstd"], w=["rstd"])
                    P.op("dve", "reciprocal", dict(out=rstd[:], in_=rstd[:]), r=["rstd"], w=["rstd"])
                    for j in range(4):
                        P.op("dve", "tensor_scalar", dict(out=hb[:, j, :], in0=xt[:, j, :],
                                                                    scalar1=rstd[:, j:j + 1], scalar2=None, op0=ALU.mult),
                             r=["xt", "rstd"], w=[("hb", j)])
                    for k in range(8):
                        tb = 6 + nxt("tp", 2)
                        pT = psb[tb][:].bitcast(BF16)
                        for j in range(4):
                            P.op("pe", "transpose", dict(
                                out=pT[:, j * 128:(j + 1) * 128], in_=hb[:, j, k * 128:(k + 1) * 128], identity=identB[:]),
                                r=[("hb", j), "identB"], w=[("ps", tb)])
                        if k % 2 == 0:
                            P.op("dve", "tensor_scalar", dict(
                                out=hT[:, k, :], in0=pT[:, 0:512], scalar1=par[:, PB + P_MIXG + k:PB + P_MIXG + k + 1],
                                scalar2=None, op0=ALU.mult), r=[("ps", tb)], w=[("hT", k)])
                        else:
                            P.op("act", "activation", dict(
                                out=hT[:, k, :], in_=pT[:, 0:512], func=AF.Identity,
                                scale=par[:, PB + P_MIXG + k:PB + P_MIXG + k + 1]), r=[("ps", tb)], w=[("hT", k)])

                    def fm_group(col0, M=128):
                        b = nxt("ps", 6)
                        for k in range(8):
                            P.op("pe", "matmul", dict(out=psb[b][0:M, :], lhsT=Win[:, k, col0:col0 + M],
                                                                     rhs=hT[:, k, :], start=(k == 0), stop=(k == 7)),
                                 r=[("Win", k), ("hT", k)], w=[("ps", b)])
                        return b

                    def evac_store(b, dst, func=None, scale=None):
                        s = nxt("ob", NSL)
                        use_act = func is not None or (nxt("ev", 2) == 0)
                        if use_act:
                            f = func if func is not None else AF.Copy
                            kw = {} if scale is None else {"scale": scale}
                            P.op("act", "activation", dict(out=ob[s][:], in_=psb[b][:, :], func=f, **kw),
                                 r=[("ps", b)], w=[("ob", s)])
                        else:
                            sc = 1.0 if scale is None else scale
                            P.op("dve", "tensor_scalar", dict(out=ob[s][:], in0=psb[b][:, :], scalar1=sc, scalar2=None,
                                                                  op0=ALU.mult), r=[("ps", b)], w=[("ob", s)])
                        P.op("sp", "dma_start", dict(out=dst, in_=ob[s][:]), r=[("ob", s)], w=[], dma=("ob", s))

                    for pr in range(4):
                        b = fm_group(C_Q + pr * 128)
                        evac_store(b, d_qT[pr, :, t0:t0 + 512], scale=DH ** -0.5)
                    for pr in range(4):
                        b = fm_group(C_K + pr * 128)
                        evac_store(b, d_kT[pr, :, t0:t0 + 512])
                    for pr in range(4):
                        b = fm_group(C_QI + pr * 128)
                        evac_store(b, d_qiT[pr, :, t0:t0 + 512])
                    b = fm_group(C_KI, M=64)
                    P.op("dve", "tensor_copy", dict(out=kidxT[0:64, t0:t0 + 512], in_=psb[b][0:64, :]),
                         r=[("ps", b)], w=[("kidx", c, 0)])
                    P.op("sp", "dma_start", dict(out=kidxT[64:128, t0:t0 + 512], in_=kidxT[0:64, t0:t0 + 512]),
                         r=[("kidx", c, 0)], w=[("kidx", c, 1)], dma="kidx")
                    for cc in range(4):
                        ba = fm_group(C_UA + cc * 128)
                        bg = fm_group(C_UG + cc * 128)
                        tf = nxt("tf", 2)
                        s = nxt("ob", NSL)
                        P.op("act", "activation", dict(out=tmpF[tf][:], in_=psb[bg][:, :], func=AF.Sigmoid),
                             r=[("ps", bg)], w=[("tmpF", tf)])
                        P.op("dve", "tensor_tensor", dict(out=ob[s][:], in0=psb[ba][:, :], in1=tmpF[tf][:],
                                                                                 op=ALU.mult),
                             r=[("ps", ba), ("tmpF", tf)], w=[("ob", s)])
                        P.op("sp", "dma_start", dict(out=d_hg[cc, :, t0:t0 + 512], in_=ob[s][:]),
                             r=[("ob", s)], dma=("ob", s))
                    for oc in range(8):
                        b = fm_group(C_GA + oc * 128)
                        evac_store(b, d_sga[oc, :, t0:t0 + 512], func=AF.Sigmoid)
                    for oc in range(8):
                        b = fm_group(C_GB + oc * 128)
                        evac_store(b, d_sgb[oc, :, t0:t0 + 512], func=AF.Sigmoid)
                    for j in range(4):
                        b = nxt("ps", 6)
                        for k in range(8):
                            P.op("pe", "matmul", dict(out=psb[b][:, :], lhsT=hT[:, k, j * 128:(j + 1) * 128],
                                                                          rhs=Win[:, k, C_V:C_V + 512], start=(k == 0), stop=(k == 7)),
                                 r=[("Win", k), ("hT", k)], w=[("ps", b)])
                        pv = psb[b][:, :].rearrange("p (a two d) -> p a two d", a=4, two=2)
                        vv = vst[:, j, :].rearrange("p (a x) -> p a x", a=4)
                        P.op("dve", "tensor_copy", dict(out=vv[:, :, 0:64], in_=pv[:, :, 0, :]),
                             r=[("ps", b)], w=[("vst", j)])
                        P.op("act", "activation", dict(out=vv[:, :, 128:192], in_=pv[:, :, 1, :], func=AF.Copy),
                             r=[("ps", b)], w=[("vst", j)])
                        P.op("sp", "dma_start", dict(out=d_V[c * 4 + j, :, :], in_=vst[:, j, :]),
                             r=[("vst", j)], dma=("vst", j))
                    b = nxt("ps", 6)
                    for j in range(4):
                        for k in range(8):
                            P.op("pe", "matmul", dict(out=psb[b][:, j * 8:(j + 1) * 8],
                                                                          lhsT=hT[:, k, j * 128:(j + 1) * 128],
                                                                          rhs=Win[:, k, C_WI:C_WI + 8], start=(k == 0), stop=(k == 7)),
                                 r=[("Win", k), ("hT", k)], w=[("ps", b)])
                    wv = wi_all[:, c * 4:(c + 1) * 4, :]
                    P.op("dve", "tensor_copy", dict(out=wv, in_=psb[b][:, 0:32].rearrange("p (j h) -> p j h", j=4)),
                         r=[("ps", b)], w=[("wi", c)])
                    P.op("act", "activation", dict(out=absw[:, c * 4:(c + 1) * 4, :], in_=wv, func=AF.Abs,
                                                   scale=(64 ** -0.5) * (8 ** -0.5)),
                         r=[("wi", c)], w=[("absw", c)])
                    P.op("dve", "tensor_scalar", dict(out=sgnw[:, c * 4:(c + 1) * 4, :], in0=wv, scalar1=0.0,
                                                                scalar2=2.0, op0=ALU.is_gt, op1=ALU.mult),
                         r=[("wi", c)], w=[("sgnw", c)])
                    P.op("dve", "tensor_scalar", dict(out=sgnw[:, c * 4:(c + 1) * 4, :], in0=sgnw[:, c * 4:(c + 1) * 4, :],
                                                          scalar1=-1.0, scalar2=None, op0=ALU.add),
                         r=[("sgnw", c)], w=[("sgnw", c)])
                P.flush()
            if stop == ("A", l):
                return nc

            with ExitStack() as ph:
                score = sb("score", [128, S], F32, ph)
                junk = sb("junkB", [128, S], F32, ph)
                maskA = [sb(f"maskA{i}", [128, S], BF16, ph) for i in range(2)]
                qTc = [sb(f"qTc{i}", [128, 4, 256], BF16, ph) for i in range(2)]
                qiTc = [sb(f"qiTc{i}", [128, 4, 256], BF16, ph) for i in range(2)]
                NKB = 2
                kTg = [sb(f"kTg{i}", [128, 4, 512], BF16, ph) for i in range(NKB)]
                Vg = [sb(f"Vg{i}", [128, 4, 768], BF16, ph) for i in range(NKB)]
                NR = 3
                Rb = [sb(f"Rb{i}", [128, 512], F32, ph) for i in range(NR)]
                NPT = 3
                Pt = [sb(f"Pt{i}", [128, 4, 256], BF16, ph) for i in range(NPT)]
                lo = sb("loB", [128, 2], F32, ph)
                hi = sb("hiB", [128, 2], F32, ph)
                mid = sb("midB", [128, 2], F32, ph)
                Wd = sb("WdB", [128, NIT + 1], F32, ph)
                cnt = sb("cntB", [128, 2], F32, ph)
                stp = sb("stpB", [128, 2], F32, ph)
                lo1 = sb("lo1B", [128, 2], F32, ph)
                hi1 = sb("hi1B", [128, 2], F32, ph)
                ssA = sb("ssAB", [128, 2], F32, ph)
                zeros2 = sb("zeros2B", [128, 2], F32, ph)
                v8 = sb("v8B", [128, 32, 8], F32, ph)
                j32 = sb("j32B", [128, 32], F32, ph)
                oacc = [sb(f"oaccB{i}", [128, 4, 512], F32, ph) for i in range(2)]
                denx = [sb(f"denxB{i}", [128, 4, 256], F32, ph) for i in range(2)]
                oTs = [sb(f"oTs{i}", [128, 4, 256], BF16, ph) for i in range(2)]
                oTo = [sb(f"oTo{i}", [128, 4, 256], BF16, ph) for i in range(2)]
                rot = {"idx": 0, "R": 0, "L": 0, "pt": 0, "mt": 0, "kg": 0}

                def nxt(name, n):
                    v = rot[name]
                    rot[name] = (v + 1) % n
                    return v

                P.op("dve", "memset", dict(ap=cnt[:], constant=0.0), w=["cnt"])
                P.op("dve", "memset", dict(ap=ssA[:], constant=0.0), w=["ssA"])
                P.op("dve", "memset", dict(ap=zeros2[:], constant=0.0), w=["zeros2"])
                mTb = [psb[6][:].bitcast(BF16), psb[7][:].bitcast(BF16)]
                Lbanks = [4, 5]
                for tc in range(S // 256):
                    T0 = tc * 256
                    qs = tc % 2
                    P.op("sp", "dma_start", dict(out=qTc[qs][:], in_=d_qT.rearrange("a p s -> p a s")[:, :, T0:T0 + 256]),
                         w=[("qTc", qs)], dma=("qTc", qs))
                    P.op("sp", "dma_start", dict(out=qiTc[qs][:], in_=d_qiT.rearrange("a p s -> p a s")[:, :, T0:T0 + 256]),
                         w=[("qiTc", qs)], dma=("qiTc", qs))
                    for qt in range(2):
                        Tq = T0 + 128 * qt
                        ti = Tq // 128
                        Sx = Tq + 128
                        ng = (Sx + 511) // 512
                        for g in range(ng):
                            wd = min(512, Sx - g * 512)
                            for h in range(NH):
                                b = nxt("idx", 4)
                                hp, pr = (h % 2) * 64, h // 2
                                P.op("pe", "matmul", dict(out=psb[b][:, 0:wd], lhsT=qiTc[qs][hp:hp + 64, pr, qt * 128:(qt + 1) * 128],
                                    rhs=kidxT[hp:hp + 64, g * 512:g * 512 + wd], start=True, stop=True),
                                    r=[("qiTc", qs)], w=[("ps", b)])
                                rs = nxt("R", NR)
                                P.op("act", "activation", dict(
                                    out=Rb[rs][:, 0:wd], in_=psb[b][:, 0:wd], func=AF.Relu, scale=absw[:, ti, h:h + 1]),
                                    r=[("ps", b)], w=[("R", rs)])
                                sc = score[:, g * 512:g * 512 + wd]
                                if h == 0:
                                    P.op("dve", "tensor_scalar", dict(
                                        out=sc, in0=Rb[rs][:, 0:wd], scalar1=sgnw[:, ti, 0:1], scalar2=None, op0=ALU.mult),
                                        r=[("R", rs)], w=[("score", g)])
                                else:
                                    P.op("dve", "scalar_tensor_tensor", dict(
                                        out=sc, in0=Rb[rs][:, 0:wd], scalar=sgnw[:, ti, h:h + 1], in1=sc, op0=ALU.mult, op1=ALU.add),
                                        r=[("R", rs), ("score", g)], w=[("score", g)])
                        SG = [("score", g) for g in range(ng)]
                        P.op("dve", "tensor_tensor", dict(out=score[:, Tq:Tq + 128], in0=score[:, Tq:Tq + 128], in1=tri[:],
                                                                     op=ALU.add), r=SG + ["tri"], w=SG)
                        if Sx > TOPK:
                            wch = (Sx - 128) // 32
                            for c in range(32):
                                P.op("dve", "max", dict(out=v8[:, c, :], in_=score[:, c * wch:(c + 1) * wch]), r=SG, w=[("v8", c)])
                            V8 = [("v8", c) for c in range(32)]
                            P.op("dve", "tensor_scalar", dict(out=j32[:, 0:32], in0=v8[:, :, 7], scalar1=1.0, scalar2=None,
                                                              op0=ALU.mult, op1=ALU.min, accum_out=lo1[:, 0:1]), r=V8, w=["j32", "lo1"])
                            P.op("dve", "tensor_scalar", dict(out=j32[:, 0:32], in0=v8[:, :, 7], scalar1=1.0, scalar2=None,
                                                              op0=ALU.mult, op1=ALU.max, accum_out=hi1[:, 0:1]), r=V8, w=["j32", "hi1"])
                            P.op("dve", "tensor_scalar", dict(out=junk[:, 0:128], in0=score[:, Tq:Tq + 128], scalar1=1.0, scalar2=None,
                                                              op0=ALU.mult, op1=ALU.max, accum_out=hi1[:, 1:2]), r=SG, w=["junk", "junkA", "hi1"])
                            P.op("dve", "tensor_scalar", dict(out=lo[:], in0=zeros2[:], scalar1=lo1[:, 0:1], scalar2=-0.01, op0=ALU.add, op1=ALU.add),
                                 r=["lo1", "zeros2"], w=["lo"])
                            P.op("dve", "tensor_scalar", dict(out=hi[:], in0=zeros2[:], scalar1=hi1[:, 0:1], scalar2=hi1[:, 1:2], op0=ALU.add, op1=ALU.max),
                                 r=["hi1", "zeros2"], w=["hi"])
                            P.op("dve", "tensor_scalar", dict(out=hi[:], in0=hi[:], scalar1=lo[:, 0:1], scalar2=None, op0=ALU.subtract),
                                 r=["hi", "lo"], w=["hi"])
                        else:
                            P.op("dve", "tensor_scalar", dict(out=junk[:, 0:Sx], in0=score[:, 0:Sx], scalar1=1.0, scalar2=None,
                                                              op0=ALU.mult, op1=ALU.max, accum_out=hi1[:, 0:1]),
                                 r=SG, w=["junk", "junkA", "hi1"])
                            P.op("dve", "tensor_scalar", dict(out=junk[:, 0:Sx], in0=score[:, 0:Sx], scalar1=-1.0, scalar2=None,
                                                              op0=ALU.mult, op1=ALU.max, accum_out=lo1[:, 0:1]),
                                 r=SG, w=["junk", "junkA", "lo1"])
                            P.op("dve", "tensor_scalar", dict(out=lo[:], in0=zeros2[:], scalar1=hi1[:, 0:1], scalar2=-1.0e4, op0=ALU.add, op1=ALU.add),
                                 r=["hi1", "zeros2"], w=["lo"])
                            P.op("dve", "tensor_scalar", dict(out=hi[:], in0=zeros2[:], scalar1=hi1[:, 0:1], scalar2=lo[:, 0:1], op0=ALU.add, op1=ALU.subtract),
                                 r=["hi1", "lo", "zeros2"], w=["hi"])
                        P.op("dve", "tensor_scalar", dict(out=Wd[:], in0=pow2[:], scalar1=hi[:, 0:1], scalar2=0.5,
                                                              op0=ALU.mult, op1=ALU.mult), r=["hi", "pow2"], w=["Wd"])
                        P.op("dve", "tensor_scalar", dict(out=mid[:], in0=lo[:], scalar1=Wd[:, 0:1], scalar2=None, op0=ALU.add),
                             r=["lo", "Wd"], w=["mid"])
                        hD = min(Sx, max(128, int(round(ACT_SPLIT * Sx / 128.0)) * 128))
                        nA = Sx - hD
                        NITq = 0 if Sx <= TOPK else NIT_TIGHT
                        for k in range(NITq):
                            P.op("dve", "tensor_scalar", dict(out=junk[:, 0:hD], in0=score[:, 0:hD], scalar1=mid[:, 0:1],
                                                              scalar2=None, op0=ALU.is_ge, op1=ALU.add, accum_out=cnt[:, 0:1]),
                                 r=SG + ["mid"], w=["junk", "cnt"])
                            if nA > 0:
                                P.op("act", "activation", dict(out=junk[:, hD:Sx], in_=score[:, hD:Sx], func=AF.Sign, scale=-1.0,
                                                               bias=mid[:, 0:1], accum_out=ssA[:, 0:1]),
                                     r=SG + ["mid"], w=["junkA", "ssA"])
                                P.op("dve", "scalar_tensor_tensor", dict(out=cnt[:], in0=ssA[:], scalar=-0.5, in1=cnt[:], op0=ALU.mult, op1=ALU.add),
                                     r=["ssA", "cnt"], w=["cnt"])
                            P.op("dve", "tensor_scalar", dict(out=stp[:], in0=cnt[:], scalar1=TOPK - 0.5 - nA / 2.0, scalar2=Wd[:, k:k + 1],
                                                                       op0=ALU.is_gt, op1=ALU.mult), r=["cnt", "Wd"], w=["stp"])
                            P.op("dve", "scalar_tensor_tensor", dict(out=mid[:], in0=mid[:], scalar=Wd[:, k + 1:k + 2], in1=stp[:],
                                                                              op0=ALU.subtract, op1=ALU.add),
                                 r=["mid", "stp", "Wd"], w=["mid"])
                        P.op("dve", "tensor_scalar", dict(out=mid[:], in0=mid[:], scalar1=Wd[:, NITq:NITq + 1], scalar2=None, op0=ALU.subtract),
                             r=["mid", "Wd"], w=["mid"])
                        P.op("dve", "tensor_scalar", dict(out=maskA[qt][:, 0:Sx], in0=score[:, 0:Sx], scalar1=mid[:, 0:1],
                                                                            scalar2=None, op0=ALU.is_ge), r=SG + ["mid"], w=[("maskA", qt)])
                        if qt == 0:
                            P.op("dve", "memset", dict(ap=maskA[0][:, Sx:Sx + 128], constant=0.0), w=[("maskA", 0)])
                    if P.limit is not None or True:
                        import os
                        if os.environ.get('KDBG'):
                            print('ATT start tc', tc, 'nops', P.nops, flush=True)
                    nj = (T0 + 256) // 128
                    units = [(j, pp) for j in range(nj) for pp in range(2)]
                    state = {}

                    def qk(i):
                        j, pp = units[i]
                        g, jj = j // 4, j % 4
                        if pp == 0 and jj == 0:
                            kb = nxt("kg", NKB)
                            state["kb"] = kb
                            nch = min(4, nj - g * 4)
                            P.op("sp", "dma_start", dict(
                                out=kTg[kb][:, :, 0:nch * 128], in_=d_kT.rearrange("a p s -> p a s")[:, :, g * 512:g * 512 + nch * 128]),
                                w=[("kTg", kb)], dma=("kTg", kb))
                            P.op("sp", "dma_start", dict(
                                out=Vg[kb][:, 0:nch, :], in_=d_V[g * 4:g * 4 + nch, :, :].rearrange("c p x -> p c x")),
                                w=[("Vg", kb)], dma=("Vg", kb))
                        kb = state["kb"]
                        if pp == 0:
                            ms = nxt("mt", 2)
                            state["ms"] = ms
                            for qt in range(2):
                                P.op("pe", "transpose", dict(
                                    out=mTb[ms][:, qt * 128:(qt + 1) * 128],
                                    in_=maskA[qt][:, j * 128:(j + 1) * 128], identity=identB[:]),
                                    r=[("maskA", qt), "identB"], w=[("ps", 6 + ms)])
                        ms = state["ms"]
                        off = j * 128 - T0
                        near = off in (-128, 0, 128)
                        for q in range(2):
                            pr = 2 * pp + q
                            for hh in range(2):
                                h = 2 * pr + hh
                                Lv = Lt[:, hh * 512 + q * 256: hh * 512 + (q + 1) * 256]
                                P.op("pe", "matmul", dict(out=Lv, lhsT=kTg[kb][hh * 64:(hh + 1) * 64, pr, jj * 128:(jj + 1) * 128],
                                    rhs=qTc[qs][hh * 64:(hh + 1) * 64, pr, :], start=True, stop=not near),
                                    r=[("kTg", kb), ("qTc", qs)], w=[("ps", "L")])
                                if near:
                                    oi = off // 128 + 1
                                    P.op("pe", "matmul", dict(out=Lv, lhsT=identB[:], rhs=biasadj[:, h, oi, :],
                                                                                     start=False, stop=True),
                                         r=[("biasadj", h), "identB"], w=[("ps", "L")])
                        ps_ = nxt("pt", NPT)
                        P.op("act", "activation", dict(out=Pt[ps_][:].rearrange("p a t -> p (a t)"), in_=Lt[:, :], func=AF.Exp),
                             r=[("ps", "L")], w=[("Pt", ps_)])
                        P.op("dve", "tensor_tensor", dict(
                            out=Pt[ps_][:], in0=Pt[ps_][:], in1=mTb[ms][:, 0:256].unsqueeze(1).to_broadcast([128, 4, 256]),
                            op=ALU.mult), r=[("Pt", ps_), ("ps", 6 + ms)], w=[("Pt", ps_)])
                        return (ps_, kb)

                    def pv(i, info):
                        ps_, kb = info
                        j, pp = units[i]
                        jj = j % 4
                        for q in range(2):
                            pr = 2 * pp + q
                            for hh in range(2):
                                P.op("pe", "matmul", dict(out=psb[pr][:, hh * 256:(hh + 1) * 256],
                                    lhsT=Vg[kb][:, jj, pr * 192 + hh * 64: pr * 192 + hh * 64 + 128],
                                    rhs=Pt[ps_][:, hh * 2 + q, :], start=(j == 0 and hh == 0), stop=(j == nj - 1), skip_group_check=True),
                                    r=[("Vg", kb), ("Pt", ps_)], w=[("ps", pr)])

                    infos = {}
                    DEPTH = 1
                    for i in range(len(units)):
                        infos[i] = qk(i)
                        if i >= DEPTH:
                            pv(i - DEPTH, infos.pop(i - DEPTH))
                    for i in range(max(0, len(units) - DEPTH), len(units)):
                        pv(i, infos.pop(i))
                    import os
                    if os.environ.get('KDBG'):
                        print('NORM start tc', tc, 'nops', P.nops, flush=True)
                    os_ = tc % 2
                    for pr in range(4):
                        eng = "act" if pr % 2 == 0 else "dve"
                        if eng == "act":
                            P.op("act", "activation", dict(out=oacc[os_][:, pr, :], in_=psb[pr][:, :], func=AF.Copy),
                                 r=[("ps", pr)], w=[("oacc", os_, pr)])
                        else:
                            P.op("dve", "tensor_copy", dict(out=oacc[os_][:, pr, :], in_=psb[pr][:, :]),
                                 r=[("ps", pr)], w=[("oacc", os_, pr)])
                    OA = [("oacc", os_, pr) for pr in range(4)]
                    P.op("sp", "dma_start", dict(out=denx[os_][0:64, :, :], in_=oacc[os_][64:128, :, 0:256]),
                         r=OA, w=[("denx", os_, 0)], dma=("denx", os_, 0))
                    P.op("sp", "dma_start", dict(out=denx[os_][64:128, :, :], in_=oacc[os_][0:64, :, 256:512]),
                         r=OA, w=[("denx", os_, 1)], dma=("denx", os_, 1))
                    P.op("dve", "reciprocal", dict(out=denx[os_][:], in_=denx[os_][:]),
                         r=[("denx", os_, 0), ("denx", os_, 1)], w=[("denx", os_, 0), ("denx", os_, 1)])
                    P.op("dve", "tensor_tensor", dict(out=oTs[os_][:], in0=oacc[os_][:, :, 0:256], in1=denx[os_][:], op=ALU.mult),
                         r=OA + [("denx", os_, 0), ("denx", os_, 1)], w=[("oTs", os_, 0)])
                    P.op("dve", "tensor_tensor", dict(out=oTo[os_][:], in0=oacc[os_][:, :, 256:512], in1=denx[os_][:], op=ALU.mult),
                         r=OA + [("denx", os_, 0), ("denx", os_, 1)], w=[("oTs", os_, 1)])
                    dview = d_oT.rearrange("a p s -> p a s")
                    P.op("sp", "dma_start", dict(out=dview[0:64, :, T0:T0 + 256], in_=oTs[os_][0:64, :, :]),
                         r=[("oTs", os_, 0)], dma=("oTs", os_, 0))
                    P.op("sp", "dma_start", dict(out=dview[64:128, :, T0:T0 + 256], in_=oTo[os_][64:128, :, :]),
                         r=[("oTs", os_, 1)], dma=("oTs", os_, 1))
                P.flush()
            if stop == ("B", l):
                return nc

            with ExitStack() as ph:
                Wao = sb("Wao", [128, 4, D], BF16, ph)
                Wco = sb("Wco", [128, 4, D], BF16, ph)
                Wmo = sb("Wmo", [128, 8, D], BF16, ph)
                Dg = sb("Dg", [128, 4, CW, 128], BF16, ph)
                xt = sb("xtC", [128, 4, D], F32, ph)
                oTc = sb("oTc", [128, 4, 512], BF16, ph)
                hg = sb("hgC", [128, 4, 512 + CW - 1], BF16, ph)
                sga = sb("sgaC", [128, 8, 512], BF16, ph)
                sgb = sb("sgbC", [128, 8, 512], BF16, ph)
                conv = sb("convC", [128, 4, 512], F32, ph)
                sq = sb("sqC", [128, 4, 512], F32, ph)
                mean_s = sb("meanC", [128, 512], F32, ph)
                var_s = sb("varC", [128, 512], F32, ph)
                ytmp = [sb(f"ytmpC{i}", [128, 512], F32, ph) for i in range(2)]
                ycT = sb("ycT", [128, 4, 512], BF16, ph)
                merged = sb("mergedC", [128, 8, 512], BF16, ph)
                t1 = [sb(f"t1C{i}", [128, 512], F32, ph) for i in range(2)]
                t2 = [sb(f"t2C{i}", [128, 512], F32, ph) for i in range(2)]
                P.op("pool", "dma_start", dict(out=Wao[:], in_=w_ao_d[l].rearrange("(k p) n -> p k n", p=128)), w=["Wao"], dma=("w", 0))
                P.op("pool", "dma_start", dict(out=Wco[:], in_=w_co_d[l].rearrange("(k p) n -> p k n", p=128)), w=["Wco"], dma=("w", 1))
                P.op("pool", "dma_start", dict(out=Wmo[:], in_=w_mo_d[l].rearrange("(k p) n -> p k n", p=128)), w=["Wmo"], dma=("w", 2))
                for cc in range(4):
                    for k in range(CW):
                        col = PB + P_DW + cc * CW + k
                        P.op("dve", "tensor_scalar", dict(out=Dg[:, cc, k, :], in0=identF[:], scalar1=par[:, col:col + 1],
                                                                                  scalar2=None, op0=ALU.mult), w=[("Dg", cc)])
                rot = {"ps": 0, "y": 0, "t": 0}

                def nxt(name, n):
                    v = rot[name]
                    rot[name] = (v + 1) % n
                    return v

                HW_ = CW - 1
                for c in range(S // 512):
                    t0 = c * 512
                    P.op("sp", "dma_start", dict(out=xt[:], in_=x_src[t0:t0 + 512, :].rearrange("(j p) d -> p j d", p=128)),
                         w=["xt"], dma="xt")
                    P.op("sp", "dma_start", dict(out=oTc[:], in_=d_oT.rearrange("a p s -> p a s")[:, :, t0:t0 + 512]),
                         w=["oTc"], dma="oTc")
                    if c == 0:
                        P.op("dve", "memset", dict(ap=hg[:, :, 0:HW_], constant=0.0), w=["hg"])
                        P.op("sp", "dma_start", dict(out=hg[:, :, HW_:HW_ + 512], in_=d_hg.rearrange("a p s -> p a s")[:, :, 0:512]),
                             w=["hg"], dma="hg")
                    else:
                        P.op("sp", "dma_start", dict(out=hg[:], in_=d_hg.rearrange("a p s -> p a s")[:, :, t0 - HW_:t0 + 512]),
                             w=["hg"], dma="hg")
                    P.op("sp", "dma_start", dict(out=sga[:], in_=d_sga.rearrange("a p s -> p a s")[:, :, t0:t0 + 512]),
                         w=["sga"], dma="sga")
                    P.op("sp", "dma_start", dict(out=sgb[:], in_=d_sgb.rearrange("a p s -> p a s")[:, :, t0:t0 + 512]),
                         w=["sgb"], dma="sgb")
                    for cc in range(4):
                        b = nxt("ps", 4)
                        for k in range(CW):
                            P.op("pe", "matmul", dict(out=psb[b][:, :], lhsT=Dg[:, cc, k, :], rhs=hg[:, cc, k:k + 512],
                                                                           start=(k == 0), stop=(k == CW - 1)),
                                 r=[("Dg", cc), "hg"], w=[("ps", b)])
                        P.op("dve", "tensor_scalar", dict(out=conv[:, cc, :], in0=psb[b][:, :],
                                                                          scalar1=par[:, PB + P_DWB + cc:PB + P_DWB + cc + 1], scalar2=None,
                                                                          op0=ALU.add), r=[("ps", b)], w=[("conv", cc)])
                        P.op("act", "activation", dict(out=sq[:, cc, :], in_=conv[:, cc, :], func=AF.Square),
                             r=[("conv", cc)], w=[("sq", cc)])
                    bm, bq = 4, 5
                    for cc in range(4):
                        P.op("pe", "matmul", dict(out=psb[bm][:, :], lhsT=onesF[:], rhs=conv[:, cc, :], start=(cc == 0), stop=(cc == 3)),
                             r=[("conv", cc), "onesF"], w=[("ps", bm)])
                    for cc in range(4):
                        P.op("pe", "matmul", dict(out=psb[bq][:, :], lhsT=onesF[:], rhs=sq[:, cc, :], start=(cc == 0), stop=(cc == 3)),
                             r=[("sq", cc), "onesF"], w=[("ps", bq)])
                    P.op("act", "activation", dict(out=mean_s[:], in_=psb[bm][:, :], func=AF.Copy), r=[("ps", bm)], w=["mean"])
                    P.op("dve", "tensor_tensor", dict(out=var_s[:], in0=mean_s[:], in1=mean_s[:], op=ALU.mult), r=["mean"], w=["var"])
                    P.op("dve", "tensor_tensor", dict(out=var_s[:], in0=psb[bq][:, :], in1=var_s[:], op=ALU.subtract),
                         r=[("ps", bq), "var"], w=["var"])
                    P.op("dve", "tensor_scalar", dict(out=var_s[:], in0=var_s[:], scalar1=0.0, scalar2=EPS, op0=ALU.max, op1=ALU.add),
                         r=["var"], w=["var"])
                    P.op("act", "activation", dict(out=var_s[:], in_=var_s[:], func=AF.Sqrt), r=["var"], w=["var"])
                    P.op("dve", "reciprocal", dict(out=var_s[:], in_=var_s[:]), r=["var"], w=["var"])
                    for cc in range(4):
                        y = nxt("y", 2)
                        P.op("dve", "tensor_tensor", dict(out=ytmp[y][:], in0=conv[:, cc, :], in1=mean_s[:], op=ALU.subtract),
                             r=[("conv", cc), "mean"], w=[("ytmp", y)])
                        P.op("pool", "tensor_tensor", dict(out=ytmp[y][:], in0=ytmp[y][:], in1=var_s[:], op=ALU.mult),
                             r=[("ytmp", y), "var"], w=[("ytmp", y)])
                        P.op("act", "activation", dict(out=ycT[:, cc, :], in_=ytmp[y][:], func=AF.Silu,
                                                                       scale=par[:, PB + P_CNG + cc:PB + P_CNG + cc + 1],
                                                                       bias=par[:, PB + P_CNB + cc:PB + P_CNB + cc + 1]),
                             r=[("ytmp", y)], w=[("ycT", cc)])
                    for oc in range(8):
                        ba = nxt("ps", 4)
                        for pr in range(4):
                            P.op("pe", "matmul", dict(out=psb[ba][:, :], lhsT=Wao[:, pr, oc * 128:(oc + 1) * 128],
                                                                               rhs=oTc[:, pr, :], start=(pr == 0), stop=(pr == 3)),
                                 r=["Wao", "oTc"], w=[("ps", ba)])
                        bb = nxt("ps", 4)
                        for cc in range(4):
                            P.op("pe", "matmul", dict(out=psb[bb][:, :], lhsT=Wco[:, cc, oc * 128:(oc + 1) * 128],
                                                                               rhs=ycT[:, cc, :], start=(cc == 0), stop=(cc == 3)),
                                 r=["Wco", ("ycT", cc)], w=[("ps", bb)])
                        ts = nxt("t", 2)
                        P.op("dve", "tensor_tensor", dict(out=t1[ts][:], in0=psb[ba][:, :], in1=sga[:, oc, :], op=ALU.mult),
                             r=[("ps", ba), "sga"], w=[("t1", ts)])
                        P.op("dve", "tensor_tensor", dict(out=t2[ts][:], in0=psb[bb][:, :], in1=sgb[:, oc, :], op=ALU.mult),
                             r=[("ps", bb), "sgb"], w=[("t2", ts)])
                        P.op("pool", "tensor_tensor", dict(out=merged[:, oc, :], in0=t1[ts][:], in1=t2[ts][:], op=ALU.add),
                             r=[("t1", ts), ("t2", ts)], w=[("merged", oc)])
                    for j in range(4):
                        for n in range(2):
                            b = nxt("ps", 4)
                            for k in range(8):
                                P.op("pe", "matmul", dict(out=psb[b][:, :], lhsT=merged[:, k, j * 128:(j + 1) * 128],
                                                                                 rhs=Wmo[:, k, n * 512:(n + 1) * 512], start=(k == 0), stop=(k == 7)),
                                     r=[("merged", k), "Wmo"], w=[("ps", b)])
                            P.op("dve", "tensor_tensor", dict(out=xt[:, j, n * 512:(n + 1) * 512], in0=psb[b][:, :],
                                                                                in1=xt[:, j, n * 512:(n + 1) * 512], op=ALU.add),
                                 r=[("ps", b), "xt"], w=["xt"])
                    P.op("sp", "dma_start", dict(out=d_xa[t0:t0 + 512, :].rearrange("(j p) d -> p j d", p=128), in_=xt[:]),
                         r=["xt"], dma="xo")
                P.flush()
            if stop == ("C1", l):
                return nc

            last = (l == NL - 1)
            with ExitStack() as ph:
                Wfi = sb("Wfi", [128, 8, 2 * DFF], BF16, ph)
                Wfo = sb("Wfo", [128, 22, D], BF16, ph)
                xt2 = [sb(f"xtF{i}", [128, 2, D], F32, ph) for i in range(1)]
                hb = sb("hbF", [128, 2, D], BF16, ph)
                hT = sb("hTF", [128, 8, 256], BF16, ph)
                ss = sb("ssF", [128, 2], F32, ph)
                rstd = sb("rstdF", [128, 2], F32, ph)
                actT = sb("actT", [128, 22, 256], BF16, ph)
                tmpF = [sb(f"tmpFF{i}", [128, 256], F32, ph) for i in range(2)]
                if last:
                    gfin = sb("gfin", [128, D], F32, ph)
                    P.op("sp", "dma_start", dict(out=gfin[:], in_=gfin_d[:, :]), w=["gfin"], dma="gfin")
                for k in range(8):
                    P.op("pool", "dma_start", dict(out=Wfi[:, k, :], in_=w_fi_d[l, k * 128:(k + 1) * 128, :]),
                         w=[("Wfi", k)], dma=("w", k))
                for k in range(22):
                    P.op("pool", "dma_start", dict(out=Wfo[:, k, :], in_=w_fo_d[l, k * 128:(k + 1) * 128, :]),
                         w=[("Wfo", k)], dma=("w", 8 + k))
                rot = {"ps": 0, "tp": 0, "tf": 0}
                dst = out_d if last else d_xb
                for c in range(S // 256):
                    t0 = c * 256
                    xs = 0
                    xt = xt2[xs]
                    P.op("sp", "dma_start", dict(out=xt[:], in_=d_xa[t0:t0 + 256, :].rearrange("(j p) d -> p j d", p=128)),
                         w=[("xt", xs)], dma=("xt", xs))
                    for j in range(2):
                        P.op("act", "activation", dict(out=hb[:, j, :], in_=xt[:, j, :], func=AF.Square, accum_out=ss[:, j:j + 1]),
                             r=[("xt", xs)], w=[("hb", j), ("ss", j)])
                    P.op("dve", "tensor_scalar", dict(out=rstd[:], in0=ss[:], scalar1=1.0 / D, scalar2=EPS, op0=ALU.mult, op1=ALU.add),
                         r=[("ss", 0), ("ss", 1)], w=["rstd"])
                    P.op("act", "activation", dict(out=rstd[:], in_=rstd[:], func=AF.Sqrt), r=["rstd"], w=["rstd"])
                    P.op("dve", "reciprocal", dict(out=rstd[:], in_=rstd[:]), r=["rstd"], w=["rstd"])
                    for j in range(2):
                        P.op("dve", "tensor_scalar", dict(out=hb[:, j, :], in0=xt[:, j, :], scalar1=rstd[:, j:j + 1], scalar2=None,
                                                                         op0=ALU.mult), r=[("xt", xs), "rstd"], w=[("hb", j)])
                    for k in range(8):
                        tb = 6 + (rot["tp"] % 2)
                        rot["tp"] += 1
                        pT = psb[tb][:].bitcast(BF16)
                        for j in range(2):
                            P.op("pe", "transpose", dict(out=pT[:, j * 128:(j + 1) * 128], in_=hb[:, j, k * 128:(k + 1) * 128],
                                                                              identity=identB[:]), r=[("hb", j), "identB"], w=[("ps", tb)])
                        col = PB + P_FFNG + k
                        if k % 2 == 0:
                            P.op("dve", "tensor_scalar", dict(out=hT[:, k, :], in0=pT[:, 0:256], scalar1=par[:, col:col + 1],
                                                                                      scalar2=None, op0=ALU.mult), r=[("ps", tb)], w=[("hT", k)])
                        else:
                            P.op("act", "activation", dict(out=hT[:, k, :], in_=pT[:, 0:256], func=AF.Identity,
                                                                                   scale=par[:, col:col + 1]), r=[("ps", tb)], w=[("hT", k)])
                    for fc in range(22):
                        b = rot["ps"] % 4
                        rot["ps"] += 1
                        for half, c0 in ((0, fc * 128), (1, DFF + fc * 128)):
                            for k in range(8):
                                P.op("pe", "matmul", dict(out=psb[b][:, half * 256:(half + 1) * 256],
                                                                                         lhsT=Wfi[:, k, c0:c0 + 128], rhs=hT[:, k, :],
                                                                                         start=(k == 0), stop=(k == 7)),
                                     r=[("Wfi", k), ("hT", k)], w=[("ps", b)])
                        tf = rot["tf"] % 2
                        rot["tf"] += 1
                        P.op("act", "activation", dict(out=tmpF[tf][:], in_=psb[b][:, 0:256], func=AF.Silu),
                             r=[("ps", b)], w=[("tmpF", tf)])
                        P.op("dve", "tensor_tensor", dict(out=actT[:, fc, :], in0=psb[b][:, 256:512], in1=tmpF[tf][:], op=ALU.mult),
                             r=[("ps", b), ("tmpF", tf)], w=[("actT", fc)])
                    for j in range(2):
                        for n in range(2):
                            b = 4 + (rot["ps"] % 2)
                            rot["ps"] += 1
                            for fc in range(22):
                                P.op("pe", "matmul", dict(out=psb[b][:, :], lhsT=actT[:, fc, j * 128:(j + 1) * 128],
                                                                                   rhs=Wfo[:, fc, n * 512:(n + 1) * 512], start=(fc == 0), stop=(fc == 21)),
                                     r=[("actT", fc), ("Wfo", fc)], w=[("ps", b)])
                            P.op("dve", "tensor_tensor", dict(out=xt[:, j, n * 512:(n + 1) * 512], in0=psb[b][:, :],
                                                                                       in1=xt[:, j, n * 512:(n + 1) * 512], op=ALU.add),
                                 r=[("ps", b), ("xt", xs)], w=[("xt", xs)])
                    if last:
                        for j in range(2):
                            P.op("act", "activation", dict(out=hb[:, j, :], in_=xt[:, j, :], func=AF.Square, accum_out=ss[:, j:j + 1]),
                                 r=[("xt", xs)], w=[("hb", j), ("ss", j)])
                        P.op("dve", "tensor_scalar", dict(out=rstd[:], in0=ss[:], scalar1=1.0 / D, scalar2=EPS, op0=ALU.mult, op1=ALU.add),
                             r=[("ss", 0), ("ss", 1)], w=["rstd"])
                        P.op("act", "activation", dict(out=rstd[:], in_=rstd[:], func=AF.Sqrt), r=["rstd"], w=["rstd"])
                        P.op("dve", "reciprocal", dict(out=rstd[:], in_=rstd[:]), r=["rstd"], w=["rstd"])
                        for j in range(2):
                            P.op("dve", "scalar_tensor_tensor", dict(out=xt[:, j, :], in0=xt[:, j, :], scalar=rstd[:, j:j + 1], in1=gfin[:],
                                                                                    op0=ALU.mult, op1=ALU.mult),
                                 r=[("xt", xs), "rstd", "gfin"], w=[("xt", xs)])
                        P.op("sp", "dma_start", dict(out=dst[t0:t0 + 256, :].rearrange("(j p) d -> p j d", p=128), in_=xt[:]),
                             r=[("xt", xs)], dma=("xo", xs))
                    else:
                        P.op("sp", "dma_start", dict(out=dst[t0:t0 + 256, :].rearrange("(j p) d -> p j d", p=128), in_=xt[:]),
                             r=[("xt", xs)], dma=("xo", xs))
                P.flush()
            x_src = d_xb
    return nc


def t5_bucket_np(dist):
    n = np.maximum(dist, 0)
    nf = np.maximum(n, 1).astype(np.float32)
    large = 16 + (np.log(nf / np.float32(16)) / np.float32(math.log(128 / 16)) * np.float32(16)).astype(np.int32)
    large = np.minimum(large, 31)
    return np.where(n < 16, n, large)


def host_inputs(inputs, S, NL):
    f = lambda a: np.ascontiguousarray(np.asarray(a, dtype=np.float32))
    par = np.zeros((128, NL * P_LSZ + 8), np.float32)
    for l in range(NL):
        b = l * P_LSZ
        par[:, b + P_MIXG:b + P_MIXG + 8] = f(inputs["mix_norm"][l]).reshape(8, 128).T
        par[:, b + P_FFNG:b + P_FFNG + 8] = f(inputs["ffn_norm"][l]).reshape(8, 128).T
        dw = f(inputs["dw_kernel"][l])
        par[:, b + P_DW:b + P_DW + 4 * CW] = dw.T.reshape(4, 128, CW).transpose(1, 0, 2).reshape(128, 4 * CW)
        par[:, b + P_DWB:b + P_DWB + 4] = f(inputs["dw_bias"][l]).reshape(4, 128).T
        par[:, b + P_CNG:b + P_CNG + 4] = f(inputs["conv_norm_g"][l]).reshape(4, 128).T
        par[:, b + P_CNB:b + P_CNB + 4] = f(inputs["conv_norm_b"][l]).reshape(4, 128).T
    rb = f(inputs["rel_bias"])
    par[:, NL * P_LSZ:NL * P_LSZ + 8] = np.broadcast_to(rb[31:32, :], (128, 8))
    s_l = np.arange(128)[:, None]
    t_l = np.arange(256)[None, :]
    biasT = np.zeros((128, NH, 3, 256), np.float32)
    for oi, off in enumerate((-128, 0, 128)):
        bk = t5_bucket_np(t_l - s_l - off)
        biasT[:, :, oi, :] = rb[bk].transpose(0, 2, 1)
    gfin = np.ascontiguousarray(np.broadcast_to(f(inputs["final_norm"])[None, :], (128, D)))
    ident = np.eye(128, dtype=np.float32)
    tri = np.where(np.arange(128)[None, :] <= np.arange(128)[:, None], 0.0, -1e30).astype(np.float32)
    pow2 = np.broadcast_to((2.0 ** -np.arange(NIT + 1)).astype(np.float32)[None, :], (128, NIT + 1)).copy()
    shared = {
        "w_in": f(inputs["w_in"]), "w_attn_out": f(inputs["w_attn_out"]), "w_conv_out": f(inputs["w_conv_out"]),
        "w_mix_out": f(inputs["w_mix_out"]), "w_ffn_in": f(inputs["w_ffn_in"]), "w_ffn_out": f(inputs["w_ffn_out"]),
        "params": par, "gfin": gfin, "biasT": biasT, "ident": ident, "tri": tri, "pow2": pow2,
    }
    return shared


_NC_CACHE = {}


def kernel(**inputs):
    x = np.asarray(inputs["x"], dtype=np.float32)
    B, S, _ = x.shape
    NL = int(np.asarray(inputs["w_in"]).shape[0])
    key = (S, NL)
    if key not in _NC_CACHE:
        _NC_CACHE[key] = build_program(S, NL)
    nc = _NC_CACHE[key]
    shared = host_inputs(inputs, S, NL)
    in_maps = []
    for b in range(B):
        m = dict(shared)
        m["x"] = np.ascontiguousarray(x[b])
        in_maps.append(m)
    res = run_bass_kernel_spmd(nc, in_maps, core_ids=list(range(B)))
    out = np.stack([np.asarray(r["out"], dtype=np.float32) for r in res.results], axis=0)
    return out
```
